# Optimizing a Trainium2 kernel written in Bass

```python
import jax
import jax.numpy as jnp
from jax import lax
import numpy as np

D_MODEL = 1024
BATCH = 32
SEQ = 2048
DEPTH = 2

GRID_W = 64
CTX_LEN = 256
N_MIXERS = 2
N_RET_LAYERS = (DEPTH + N_MIXERS - 1) // N_MIXERS
N_ATT_LAYERS = DEPTH // N_MIXERS
N_MOD = 6
NORM_EPS = 1e-6
HEAD_NORM_EPS = 1e-5
ROPE_THETA = 10000.0

RET_HEADS = 4
RET_DK = D_MODEL // RET_HEADS
RET_DV = 2 * RET_DK
RET_QK = RET_HEADS * RET_DK
RET_V = RET_HEADS * RET_DV
RET_QKV = 2 * RET_QK + RET_V
RET_IN = RET_QKV + RET_V
RET_CHUNK = 128

ATT_HEAD_DIM = 128
ATT_Q_HEADS = D_MODEL // ATT_HEAD_DIM
ATT_KV_HEADS = 2
ATT_GROUP = ATT_Q_HEADS // ATT_KV_HEADS
ATT_QW = ATT_Q_HEADS * ATT_HEAD_DIM
ATT_KW = ATT_KV_HEADS * ATT_HEAD_DIM
ATT_IN = ATT_QW + 2 * ATT_KW
Q_BLOCK = 128

N_GROUPS = 4
EXPERTS_PER_GROUP = 8
N_EXPERTS = N_GROUPS * EXPERTS_PER_GROUP
TOP_K_IN_GROUP = 2
EXPERT_HIDDEN = D_MODEL // 2
EXPERT_BLOCK = 256

kernel_name = "hybrid_retention_gqa_hmoe_dit"


def rms_norm(x, eps=NORM_EPS):
    xf = x.astype(jnp.float32)
    return (xf * lax.rsqrt(jnp.mean(xf * xf, axis=-1, keepdims=True) + eps)).astype(x.dtype)


def head_norm(y):
    yf = y.astype(jnp.float32)
    mu = jnp.mean(yf, axis=-1, keepdims=True)
    var = jnp.mean(jnp.square(yf - mu), axis=-1, keepdims=True)
    return ((yf - mu) * lax.rsqrt(var + HEAD_NORM_EPS)).astype(y.dtype)


def modulate(x, shift, scale):
    return rms_norm(x) * (1 + scale) + shift


def axial_rope_tables(n, head_dim, dtype):
    rows = n // GRID_W
    r = jnp.repeat(jnp.arange(rows), GRID_W).astype(jnp.float32)
    col = jnp.tile(jnp.arange(GRID_W), rows).astype(jnp.float32)
    nf = head_dim // 4
    inv = ROPE_THETA ** (-jnp.arange(nf, dtype=jnp.float32) / nf)
    ar = r[:, None] * inv
    ac = col[:, None] * inv
    ang = jnp.concatenate([ar, ar, ac, ac], axis=-1)
    return jnp.cos(ang).astype(dtype), jnp.sin(ang).astype(dtype)


def rotate_half(u):
    a, b = jnp.split(u, 2, axis=-1)
    return jnp.concatenate([-b, a], axis=-1)


def apply_axial_rope(x, cos, sin):
    half = x.shape[-1] // 2
    x_rot = jnp.concatenate([rotate_half(x[..., :half]), rotate_half(x[..., half:])], axis=-1)
    return x * cos[:, None, :] + x_rot * sin[:, None, :]


def chunk_retention(q, k, v, log_g, state0, with_output):
    B, H, T, dk = q.shape
    dv = v.shape[-1]
    C = RET_CHUNK
    n = T // C
    lg = log_g.astype(jnp.float32)
    pos = jnp.arange(C, dtype=jnp.float32)
    zeta = jnp.exp(lg[:, None] * (C - 1 - pos))
    chunk_decay = jnp.exp(lg * C)
    qc = q.reshape(B, H, n, C, dk)
    kc = k.reshape(B, H, n, C, dk)
    vc = v.reshape(B, H, n, C, dv)
    kz = kc * zeta[:, None, :, None].astype(k.dtype)

    def step(state, xs):
        q_i, kz_i, v_i = xs
        cross = jnp.einsum('bhcd,bhde->bhce', q_i.astype(jnp.float32), state) if with_output else None
        upd = jnp.einsum('bhcd,bhce->bhde', kz_i, v_i).astype(jnp.float32)
        return state * chunk_decay[:, None, None] + upd, cross

    final_state, crosses = lax.scan(
        step, state0, (jnp.moveaxis(qc, 2, 0), jnp.moveaxis(kz, 2, 0), jnp.moveaxis(vc, 2, 0)))
    if not with_output:
        return final_state, None
    diff = pos[:, None] - pos[None, :]
    dmask = jnp.where(diff >= 0, jnp.exp(lg[:, None, None] * jnp.maximum(diff, 0.0)), 0.0)
    scores = jnp.einsum('bhnid,bhnjd->bhnij', qc, kc) * dmask[:, None].astype(q.dtype)
    inner = jnp.einsum('bhnij,bhnje->bhnie', scores, vc)
    xi = jnp.exp(lg[:, None] * (pos + 1))
    cross = (jnp.moveaxis(crosses, 0, 2) * xi[:, None, :, None]).astype(v.dtype)
    return final_state, (inner + cross).reshape(B, H, T, dv)


def _ret_project(u, w_in, with_gate):
    B, n, _ = u.shape
    p = u @ (w_in if with_gate else w_in[:, :RET_QKV])
    q = p[..., :RET_QK].reshape(B, n, RET_HEADS, RET_DK)
    k = p[..., RET_QK:2 * RET_QK].reshape(B, n, RET_HEADS, RET_DK) * (RET_DK ** -0.5)
    v = p[..., 2 * RET_QK:RET_QKV].reshape(B, n, RET_HEADS, RET_DV)
    g = p[..., RET_QKV:] if with_gate else None
    return q, k, v, g


def retention_mixer(h, hc, w_in, w_out, log_g_fwd, log_g_bwd, need_ctx):
    B, S, _ = h.shape
    L = hc.shape[1]
    q, k, v, g = _ret_project(h, w_in, True)
    cos, sin = axial_rope_tables(S, RET_DK, h.dtype)
    q = apply_axial_rope(q, cos, sin)
    k = apply_axial_rope(k, cos, sin)
    qc, kc, vc, gc = _ret_project(hc, w_in, need_ctx)
    to_bhtd = lambda u: u.transpose(0, 2, 1, 3)
    flip = lambda u: jnp.flip(u, axis=2)
    q, k, v = to_bhtd(q), to_bhtd(k), to_bhtd(v)
    qc, kc, vc = to_bhtd(qc), to_bhtd(kc), to_bhtd(vc)
    zero = jnp.zeros((B, RET_HEADS, RET_DK, RET_DV), jnp.float32)
    st_f, yc_f = chunk_retention(qc, kc, vc, log_g_fwd, zero, need_ctx)
    st_b, yc_b = chunk_retention(flip(qc), flip(kc), flip(vc), log_g_bwd, zero, need_ctx)
    _, y_f = chunk_retention(q, k, v, log_g_fwd, st_f, True)
    _, y_b = chunk_retention(flip(q), flip(k), flip(v), log_g_bwd, st_b, True)

    def finish(y, gate, n):
        y = head_norm(y).transpose(0, 2, 1, 3).reshape(B, n, RET_V)
        return (y * jax.nn.silu(gate)) @ w_out

    out = finish(y_f + flip(y_b), g, S)
    out_c = finish(yc_f + flip(yc_b), gc, L) if need_ctx else None
    return out, out_c


def _att_project(u, w_qkv, q_gain, k_gain, with_q):
    B, n, _ = u.shape
    p = u @ (w_qkv if with_q else w_qkv[:, ATT_QW:])
    if with_q:
        q = rms_norm(p[..., :ATT_QW].reshape(B, n, ATT_Q_HEADS, ATT_HEAD_DIM)) * q_gain
        p = p[..., ATT_QW:]
    else:
        q = None
    k = rms_norm(p[..., :ATT_KW].reshape(B, n, ATT_KV_HEADS, ATT_HEAD_DIM)) * k_gain
    v = p[..., ATT_KW:].reshape(B, n, ATT_KV_HEADS, ATT_HEAD_DIM)
    return q, k, v


def gqa_softmax(q, k, v):
    B, nq = q.shape[0], q.shape[1]
    qg = q.reshape(B, nq, ATT_KV_HEADS, ATT_GROUP, ATT_HEAD_DIM)
    s = jnp.einsum('bqkgd,bskd->bkgqs', qg, k).astype(jnp.float32) * (ATT_HEAD_DIM ** -0.5)
    p = jax.nn.softmax(s, axis=-1).astype(v.dtype)
    o = jnp.einsum('bkgqs,bskd->bqkgd', p, v)
    return o.reshape(B, nq, ATT_QW)


def blocked_attention(q, k, v):
    B, S = q.shape[0], q.shape[1]
    nb = S // Q_BLOCK
    qb = jnp.moveaxis(q.reshape(B, nb, Q_BLOCK, ATT_Q_HEADS, ATT_HEAD_DIM), 1, 0)
    o = lax.map(lambda qi: gqa_softmax(qi, k, v), qb)
    return jnp.moveaxis(o, 0, 1).reshape(B, S, ATT_QW)


def attention_mixer(h, hc, w_qkv, w_o, q_gain, k_gain, need_ctx):
    S = h.shape[1]
    q, k, v = _att_project(h, w_qkv, q_gain, k_gain, True)
    cos, sin = axial_rope_tables(S, ATT_HEAD_DIM, h.dtype)
    q = apply_axial_rope(q, cos, sin)
    k = apply_axial_rope(k, cos, sin)
    qc, kc, vc = _att_project(hc, w_qkv, q_gain, k_gain, need_ctx)
    k_all = jnp.concatenate([k, kc], axis=1)
    v_all = jnp.concatenate([v, vc], axis=1)
    out = blocked_attention(q, k_all, v_all) @ w_o
    out_c = gqa_softmax(qc, kc, vc) @ w_o if need_ctx else None
    return out, out_c


def grouped_expert_ffn(h, expert_ids, gates, w_gate, w_up, w_down):
    T, D = h.shape
    K = expert_ids.shape[1]
    A = T * K
    flat_e = expert_ids.reshape(A)
    order = jnp.argsort(flat_e)
    sorted_e = flat_e[order]
    counts = jnp.bincount(flat_e, length=N_EXPERTS)
    starts = jnp.cumsum(counts) - counts
    padded = (counts + EXPERT_BLOCK - 1) // EXPERT_BLOCK * EXPERT_BLOCK
    ends_p = jnp.cumsum(padded)
    starts_p = ends_p - padded
    dest = starts_p[sorted_e] + (jnp.arange(A) - starts[sorted_e])
    n_blocks = -(-A // EXPERT_BLOCK) + N_EXPERTS
    slot_tok = jnp.full((n_blocks * EXPERT_BLOCK,), T, dtype=jnp.int32).at[dest].set(
        (order // K).astype(jnp.int32))
    block_e = jnp.minimum(
        jnp.searchsorted(ends_p, jnp.arange(n_blocks) * EXPERT_BLOCK, side='right'), N_EXPERTS - 1)
    h_pad = jnp.concatenate([h, jnp.zeros((1, D), h.dtype)], axis=0)

    def run_block(args):
        toks, e = args
        xb = h_pad[toks]
        return (jax.nn.silu(xb @ w_gate[e]) * (xb @ w_up[e])) @ w_down[e]

    y_slots = lax.map(run_block, (slot_tok.reshape(n_blocks, EXPERT_BLOCK), block_e))
    y_sorted = y_slots.reshape(-1, D)[dest]
    y_assign = jnp.zeros_like(y_sorted).at[order].set(y_sorted).reshape(T, K, D)
    return jnp.sum(y_assign * gates[..., None].astype(h.dtype), axis=1)


def hier_moe(h, w_group, b_group, w_expert, b_expert, w_gate, w_up, w_down):
    T = h.shape[0]
    glog = (h @ w_group).astype(jnp.float32) + b_group.astype(jnp.float32)
    gprob = jax.nn.softmax(glog, axis=-1)
    g_idx = jnp.argmax(glog, axis=-1)
    g_p = jnp.take_along_axis(gprob, g_idx[:, None], axis=-1)
    elog = ((h @ w_expert).astype(jnp.float32) + b_expert.astype(jnp.float32)).reshape(
        T, N_GROUPS, EXPERTS_PER_GROUP)
    elog = jnp.take_along_axis(elog, g_idx[:, None, None], axis=1)[:, 0]
    top_p, top_e = lax.top_k(jax.nn.softmax(elog, axis=-1), TOP_K_IN_GROUP)
    gates = g_p * top_p / jnp.sum(top_p, axis=-1, keepdims=True)
    expert_ids = g_idx[:, None] * EXPERTS_PER_GROUP + top_e
    return grouped_expert_ffn(h, expert_ids, gates, w_gate, w_up, w_down)


def setup_inputs(seed: int = 0) -> dict:
    key = jax.random.key(seed)
    ks = jax.random.split(key, 22)
    f32 = jnp.float32
    D = D_MODEL

    def normal(k, shape, scale):
        return jax.random.normal(k, shape, f32) * scale

    heads = jnp.arange(RET_HEADS, dtype=f32)
    base_log_decay = jnp.log1p(-(2.0 ** (-5.0 - heads)))
    return {
        'x': normal(ks[0], (BATCH, SEQ, D), 1.0),
        'c': normal(ks[1], (BATCH, D), 1.0),
        'ctx': normal(ks[2], (BATCH, CTX_LEN, D), 1.0),
        'c_ctx': normal(ks[3], (D,), 1.0),
        'w_mod': normal(ks[4], (DEPTH, D, N_MOD * D), 0.5 * D ** -0.5),
        'b_mod': normal(ks[5], (DEPTH, N_MOD * D), 0.02),
        'ret_w_in': normal(ks[6], (N_RET_LAYERS, D, RET_IN), D ** -0.5),
        'ret_w_out': normal(ks[7], (N_RET_LAYERS, RET_V, D), RET_V ** -0.5),
        'ret_log_decay_fwd': base_log_decay[None] * (1.0 + 0.05 * normal(ks[8], (N_RET_LAYERS, RET_HEADS), 1.0)),
        'ret_log_decay_bwd': base_log_decay[None] * (1.0 + 0.05 * normal(ks[9], (N_RET_LAYERS, RET_HEADS), 1.0)),
        'att_w_qkv': normal(ks[10], (N_ATT_LAYERS, D, ATT_IN), D ** -0.5),
        'att_w_o': normal(ks[11], (N_ATT_LAYERS, ATT_QW, D), ATT_QW ** -0.5),
        'att_q_gain': 1.0 + normal(ks[12], (N_ATT_LAYERS, ATT_HEAD_DIM), 0.02),
        'att_k_gain': 1.0 + normal(ks[13], (N_ATT_LAYERS, ATT_HEAD_DIM), 0.02),
        'moe_w_group': normal(ks[14], (DEPTH, D, N_GROUPS), D ** -0.5),
        'moe_b_group': normal(ks[15], (DEPTH, N_GROUPS), 0.01),
        'moe_w_expert': normal(ks[16], (DEPTH, D, N_EXPERTS), D ** -0.5),
        'moe_b_expert': normal(ks[17], (DEPTH, N_EXPERTS), 0.01),
        'moe_w_gate': normal(ks[18], (DEPTH, N_EXPERTS, D, EXPERT_HIDDEN), D ** -0.5),
        'moe_w_up': normal(ks[19], (DEPTH, N_EXPERTS, D, EXPERT_HIDDEN), D ** -0.5),
        'moe_w_down': normal(ks[20], (DEPTH, N_EXPERTS, EXPERT_HIDDEN, D), EXPERT_HIDDEN ** -0.5),
        'final_norm_gain': 1.0 + normal(ks[21], (D,), 0.02),
    }


def reference(x, c, ctx, c_ctx, w_mod, b_mod, ret_w_in, ret_w_out, ret_log_decay_fwd,
              ret_log_decay_bwd, att_w_qkv, att_w_o, att_q_gain, att_k_gain, moe_w_group,
              moe_b_group, moe_w_expert, moe_b_expert, moe_w_gate, moe_w_up, moe_w_down,
              final_norm_gain):
    B, S, D = x.shape
    L = ctx.shape[1]
    xc = ctx
    for i in range(DEPTH):
        need_ctx = i < DEPTH - 1
        j = i // N_MIXERS
        m = jax.nn.silu(c) @ w_mod[i] + b_mod[i]
        mc = jax.nn.silu(c_ctx) @ w_mod[i] + b_mod[i]
        sh1, sc1, g1, sh2, sc2, g2 = jnp.split(m[:, None, :], N_MOD, axis=-1)
        csh1, csc1, cg1, csh2, csc2, cg2 = jnp.split(mc, N_MOD, axis=-1)
        h = modulate(x, sh1, sc1)
        hc = modulate(xc, csh1, csc1)
        if i % N_MIXERS == 0:
            y, yc = retention_mixer(h, hc, ret_w_in[j], ret_w_out[j], ret_log_decay_fwd[j],
                                    ret_log_decay_bwd[j], need_ctx)
        else:
            y, yc = attention_mixer(h, hc, att_w_qkv[j], att_w_o[j], att_q_gain[j],
                                    att_k_gain[j], need_ctx)
        x = x + g1 * y
        h2 = modulate(x, sh2, sc2)
        moe_args = (moe_w_group[i], moe_b_group[i], moe_w_expert[i], moe_b_expert[i],
                    moe_w_gate[i], moe_w_up[i], moe_w_down[i])
        if need_ctx:
            xc = xc + cg1 * yc
            h2c = modulate(xc, csh2, csc2)
            f = hier_moe(jnp.concatenate([h2.reshape(B * S, D), h2c.reshape(B * L, D)], axis=0), *moe_args)
            x = x + g2 * f[:B * S].reshape(B, S, D)
            xc = xc + cg2 * f[B * S:].reshape(B, L, D)
        else:
            x = x + g2 * hier_moe(h2.reshape(B * S, D), *moe_args).reshape(B, S, D)
    return rms_norm(x) * final_norm_gain
```

```python
import numpy as np
from contextlib import ExitStack
import concourse.bass as bass
import concourse.mybir as mybir
from concourse.bass_utils import run_bass_kernel_spmd

F32 = mybir.dt.float32
BF16 = mybir.dt.bfloat16
AF = mybir.ActivationFunctionType
ALU = mybir.AluOpType
AX = mybir.AxisListType

D = 1024
S = 2048
L = 256
T = S + L
NBLK = 5
NCORES = 8
BATCH = 32
NB_FULL = BATCH // NCORES
RET_IN = 6144
ATT_IN = 1536
BIG = 1.0e30


def blk_rng(blk):
    if blk < 4:
        return blk * 512, 512
    return 2048, 256


class KB:
    def __init__(self, nc, es):
        self.nc = nc
        self.eng = {'pe': nc.tensor, 'dve': nc.vector, 'act': nc.scalar, 'pool': nc.gpsimd, 'sp': nc.sync}
        self.sem = {k: es.enter_context(nc.semaphore("s_" + k)) for k in ('pe', 'dve', 'act', 'pool')}
        self.cnt = {k: 0 for k in self.sem}
        self.seen = {e: {} for e in self.eng}
        self.lastw = {}
        self.readers = {}
        self.dsem = {}
        self.dcnt = {}
        self.es = es
        self.bar_sem = es.enter_context(nc.semaphore("s_bar"))
        self.bar_cnt = 0
        self.lazy = set()
        self.scr = nc.dram_tensor("bar_scr", [2, 64], F32, kind="Internal").ap()

    def _semobj(self, key):
        if isinstance(key, tuple):
            return self.dsem[key[1]]
        return self.sem[key]

    _skip = None

    def _wait(self, e, key, val):
        if key == 'pe' and e == 'pe':
            return
        if key == self._skip:
            return
        if self.seen[e].get(key, 0) >= val:
            return
        self.eng[e].wait_ge(self._semobj(key), val)
        self.seen[e][key] = val

    def _deps(self, e, reads, writes):
        for r in reads:
            t = self.lastw.get(r)
            if t is not None:
                self._wait(e, t[0], t[1])
        for w in writes:
            t = self.lastw.get(w)
            if t is not None:
                self._wait(e, t[0], t[1])
            rd = self.readers.get(w)
            if rd:
                for k, v in rd.items():
                    self._wait(e, k, v)

    def _commit(self, tok, reads, writes):
        for w in writes:
            self.lastw[w] = tok
            self.readers[w] = {}
        for r in reads:
            self.readers.setdefault(r, {})[tok[0]] = tok[1]

    def op(self, e, fn, reads=(), writes=()):
        self._deps(e, reads, writes)
        ins = fn()
        self.cnt[e] += 1
        ins.then_inc(self.sem[e], 1)
        self._commit((e, self.cnt[e]), reads, writes)

    def dma(self, q, stream, out, in_, reads=(), writes=(), indirect=None, **kw):
        if stream not in self.dsem:
            self.dsem[stream] = self.es.enter_context(self.nc.semaphore("d_" + str(len(self.dsem))))
            self.dcnt[stream] = 0
        self._skip = ('dma', stream)
        self._deps(q, reads, writes)
        self._skip = None
        self.dcnt[stream] += 16
        if indirect is None:
            ins = self.eng[q].dma_start(out=out, in_=in_, **kw)
        elif indirect[0] == 'out':
            ins = self.eng[q].indirect_dma_start(out=out, out_offset=bass.IndirectOffsetOnAxis(ap=indirect[1], axis=0), in_=in_, in_offset=None)
        else:
            if len(indirect) > 2:
                ins = self.eng[q].indirect_dma_start(out=out, out_offset=None, in_=in_, in_offset=bass.IndirectOffsetOnAxis(ap=indirect[1], axis=0),
                                                     bounds_check=indirect[2], oob_is_err=False)
            else:
                ins = self.eng[q].indirect_dma_start(out=out, out_offset=None, in_=in_, in_offset=bass.IndirectOffsetOnAxis(ap=indirect[1], axis=0))
        ins.then_inc(self.dsem[stream], 16)
        self._commit((('dma', stream), self.dcnt[stream]), reads, writes)

    def barrier(self):
        sp = 'sp'
        for k in self.sem:
            if self.cnt[k]:
                self._wait(sp, k, self.cnt[k])
        for s, c in self.dcnt.items():
            if c and s not in self.lazy:
                self._wait(sp, ('dma', s), c)
        self.bar_cnt += 16
        self.eng[sp].dma_start(out=self.scr[1:2, :], in_=self.scr[0:1, :]).then_inc(self.bar_sem, 16)
        for e in ('pe', 'dve', 'act', 'pool', 'sp'):
            self.eng[e].wait_ge(self.bar_sem, self.bar_cnt)
            for k in self.sem:
                self.seen[e][k] = self.cnt[k]
            for s, c in self.dcnt.items():
                if s not in self.lazy:
                    self.seen[e][('dma', s)] = c
        self.lastw.clear()
        self.readers.clear()

    def finish(self):
        sp = 'sp'
        for k in self.sem:
            if self.cnt[k]:
                self._wait(sp, k, self.cnt[k])
        for s, c in self.dcnt.items():
            if c:
                self._wait(sp, ('dma', s), c)


def build_program(NB=NB_FULL, stop=None, dump_ctx=False):
    nc = bass.Bass("TRN2", target_bir_lowering=False)
    NM = NB + 1

    def din(name, shape):
        return nc.dram_tensor(name, list(shape), F32, kind="ExternalInput").ap()

    x_d = din("x", [NB, S, D])
    ctx_d = din("ctx", [NB, L, D])
    cT_d = din("cT", [128, 8, NM])
    wmod_d = din("w_mod", [2, D, 6 * D])
    bmod_d = din("bmodT", [128, 2, 48])
    rwin_d = din("ret_w_in", [D, RET_IN + 2048])
    rwout_d = din("ret_w_out", [2048, D])
    lgf_d = din("lgf", [1, 4])
    lgb_d = din("lgb", [1, 4])
    aw_d = din("att_w_qkv", [D, ATT_IN + 1280])
    awo_d = din("att_w_o", [D, D])
    gq_d = din("gq", [128, 2])
    gk_d = din("gk", [128, 2])
    wr_d = din("w_router", [2, D, 36])
    br_d = din("b_router", [2, 36])
    wgr_d = din("wg_r", [2 * 32 * 128, 4096])
    wur_d = din("wu_r", [2 * 32 * 128, 4096])
    wdr_d = din("wd_r", [2 * 32 * 128, 4096])
    NSLOT = 50 * 256
    w16_d = [nc.dram_tensor("w16_%d" % i, [2 * 32 * 128, 4096], BF16, kind="Internal").ap() for i in range(3)]
    dec_d = nc.dram_tensor("dec_scr", [4, 128, 6144], BF16, kind="Internal").ap()
    Xs_d = nc.dram_tensor("Xs_scr", [NSLOT, D], BF16, kind="Internal").ap()
    Ys_d = nc.dram_tensor("Ys_scr", [NSLOT, D], F32, kind="Internal").ap()
    fg_d = din("fgT", [128, 8])
    rtab_d = din("ret_tab", [128, 192])
    atab_d = din("att_tab", [128, 2, 64])
    ident_d = din("ident", [128, 128])
    out_d = nc.dram_tensor("out", [NB, S, D], F32, kind="ExternalOutput").ap()
    if dump_ctx:
        outc_d = nc.dram_tensor("outc", [NB, L, D], F32, kind="ExternalOutput").ap()

    with ExitStack() as es:
        kb = KB(nc, es)
        bcw = nc.gpsimd.to_reg(2 * 32 * 128 - 1)
        for l_ in range(2):
            kb.lazy.add('pc%d' % l_)
            for r in range(32):
                r0 = l_ * 4096 + r * 128
                for i_, src_ in enumerate((wgr_d, wur_d, wdr_d)):
                    kb.dma('pool', 'pc%d' % l_, w16_d[i_][r0:r0 + 128, :], src_[r0:r0 + 128, :])
        V, A, P, PE = 'dve', 'act', 'pool', 'pe'

        uid = [0]

        def sb(name, shape, dt, st=es):
            uid[0] += 1
            return st.enter_context(nc.sbuf_tensor("%s_%d" % (name, uid[0]), list(shape), dt))

        def pst(name, shape, dt, st):
            uid[0] += 1
            return st.enter_context(nc.psum_tensor("%s_%d" % (name, uid[0]), list(shape), dt))

        xT = sb("xT", [128, 8, T], F32)
        hT = sb("hT", [128, 8, T], BF16)
        WA = sb("WA", [128, 4, 4096], BF16)
        identf = sb("identf", [128, 128], F32)
        identb = sb("identb", [128, 128], BF16)
        onesb = sb("onesb", [128, 128], BF16)
        mod = sb("mod", [128, 2, 48, NM], F32)
        bmod = sb("bmod", [128, 2, 48], F32)
        rtab = sb("rtab", [128, 192], F32)
        atab = sb("atab", [128, 2, 64], F32)
        atq = sb("atq", [128, 2, 64], F32)
        atk = sb("atk", [128, 2, 64], F32)
        gq = sb("gq_s", [128, 2], F32)
        gk = sb("gk_s", [128, 2], F32)
        lgf = sb("lgf_s", [128, 4], F32)
        lgb = sb("lgb_s", [128, 4], F32)
        nlgb = sb("nlgb_s", [128, 4], F32)
        fg = sb("fg_s", [128, 8], F32)
        triU = sb("triU", [128, 128], F32)
        onesf = sb("onesf", [128, 128], F32)
        UI = sb("UI", [32, 32], F32)
        thr18 = sb("thr18", [32, 18], F32)
        thr68 = sb("thr68", [128, 68], F32)
        iop = sb("iop", [128, 1], F32)
        epsn = sb("epsn", [128, 1], F32)
        epsh = sb("epsh", [128, 1], F32)

        def xk(dc, blk):
            return ('xT', dc, blk)

        def hk(dc, blk):
            return ('hT', dc, blk)

        def xks(blk):
            return [xk(dc, blk) for dc in range(8)]

        def hks(blk):
            return [hk(dc, blk) for dc in range(8)]

        with ExitStack() as ps:
            scT = sb("scT", [128, 8, NM], F32, ps)
            wst = [sb("wst%d" % i, [128, 8, 512], F32, ps) for i in range(2)]
            pm = [pst("pm%d" % i, [128, 4, NM], F32, ps) for i in range(2)]
            kb.dma('sp', 'c0', identf[:], ident_d[:], writes=['identf'])
            kb.dma('sp', 'c1', scT[:], cT_d[:], writes=['scT'])
            kb.dma('sp', 'c2', bmod[:], bmod_d[:], writes=['bmod'])
            kb.dma('sp', 'c3', rtab[:], rtab_d[:], writes=['rtab'])
            kb.dma('sp', 'c4', atab[:], atab_d[:], writes=['atab'])
            kb.dma('sp', 'c5', gq[:], gq_d[:], writes=['gq'])
            kb.dma('sp', 'c6', gk[:], gk_d[:], writes=['gk'])
            kb.dma('sp', 'c7', lgf[:], lgf_d.partition_broadcast(128), writes=['lgf'])
            kb.dma('sp', 'c8', lgb[:], lgb_d.partition_broadcast(128), writes=['lgb'])
            kb.dma('sp', 'c9', fg[:], fg_d[:], writes=['fg'])
            kb.op(V, lambda: nc.vector.tensor_copy(identb[:], identf[:]), reads=['identf'], writes=['identb'])
            kb.op(V, lambda: nc.vector.memset(onesb[:], 1.0), writes=['onesb'])
            kb.op(V, lambda: nc.vector.memset(epsn[:], 1e-6), writes=['epsn'])
            kb.op(V, lambda: nc.vector.memset(onesf[:], 1.0), writes=['onesf'])
            kb.op(P, lambda: nc.gpsimd.iota(triU[:], [[1, 128]], base=0, channel_multiplier=-1, allow_small_or_imprecise_dtypes=True), writes=['triU'])
            kb.op(V, lambda: nc.vector.tensor_scalar(UI[:], triU[0:32, 0:32], 0.0, None, ALU.is_ge), reads=['triU'], writes=['UI'])
            kb.op(V, lambda: nc.vector.tensor_scalar(triU[:], triU[:], 0.0, None, ALU.is_gt), reads=['triU', 'UI'], writes=['triU'])
            kb.op(P, lambda: nc.gpsimd.iota(thr18[:], [[256, 18]], base=0, channel_multiplier=0, allow_small_or_imprecise_dtypes=True), writes=['thr18'])
            kb.op(P, lambda: nc.gpsimd.iota(thr68[:], [[256, 68]], base=0, channel_multiplier=0, allow_small_or_imprecise_dtypes=True), writes=['thr68'])
            kb.op(P, lambda: nc.gpsimd.iota(iop[:], [[0, 1]], base=0, channel_multiplier=1, allow_small_or_imprecise_dtypes=True), writes=['iop'])
            kb.op(V, lambda: nc.vector.memset(epsh[:], 1e-5), writes=['epsh'])
            kb.op(V, lambda: nc.vector.tensor_scalar(nlgb[:], lgb[:], -1.0, None, ALU.mult), reads=['lgb'], writes=['nlgb'])
            for tabo, g_, nm in ((atq, gq, 'atq'), (atk, gk, 'atk')):
                kb.op(V, lambda tabo=tabo, g_=g_: nc.vector.tensor_scalar(tabo[:, 0, :], atab[:, 0, :], g_[:, 0:1], None, ALU.mult),
                      reads=['atab', 'gq', 'gk'], writes=[nm + '0'])
                kb.op(V, lambda tabo=tabo, g_=g_: nc.vector.tensor_scalar(tabo[:, 1, :], atab[:, 1, :], g_[:, 1:2], None, ALU.mult),
                      reads=['atab', 'gq', 'gk'], writes=[nm + '1'])
            kb.op(A, lambda: nc.scalar.activation(scT[:], scT[:], AF.Silu), reads=['scT'], writes=['scT'])
            strip = sb("strip_s", [128, 3968], BF16, ps)
            cstrip = sb("cstrip_s", [128, 2176], BF16, ps)
            it0 = sb("it0", [128, 496], F32, ps)
            it1 = sb("it1", [128, 496], F32, ps)
            it2 = sb("it2", [128, 496], F32, ps)
            for h in range(4):
                for pcs in range(8):
                    u0 = pcs * 496
                    kb.op(P, lambda u0=u0: nc.gpsimd.iota(it0[:, :496], [[1, 496]], base=u0 - 1920, channel_multiplier=-1,
                                                          allow_small_or_imprecise_dtypes=True), writes=['it0'])
                    kb.op(V, lambda: nc.vector.tensor_scalar(it1[:, :496], it0[:, :496], 0.0, None, ALU.max), reads=['it0'], writes=['it1'])
                    kb.op(V, lambda: nc.vector.tensor_scalar(it2[:, :496], it0[:, :496], 0.0, None, ALU.min), reads=['it0'], writes=['it2'])
                    kb.op(A, lambda h=h: nc.scalar.activation(it1[:, :496], it1[:, :496], AF.Exp, scale=lgf[:, h:h + 1]), reads=['it1', 'lgf'], writes=['it1'])
                    kb.op(A, lambda h=h: nc.scalar.activation(it2[:, :496], it2[:, :496], AF.Exp, scale=nlgb[:, h:h + 1]), reads=['it2', 'nlgb'], writes=['it2'])
                    kb.op(V, lambda: nc.vector.tensor_tensor(it1[:, :496], it1[:, :496], it2[:, :496], ALU.add), reads=['it1', 'it2'], writes=['it1'])
                    kb.op(V, lambda: nc.vector.tensor_scalar(it2[:, :496], it0[:, :496], 0.0, None, ALU.is_equal), reads=['it0', 'it1'], writes=['it2'])
                    kb.op(V, lambda: nc.vector.tensor_tensor(it1[:, :496], it1[:, :496], it2[:, :496], ALU.add), reads=['it2', 'it1'], writes=['it1'])
                    kb.op(V, lambda u0=u0: nc.vector.tensor_scalar(strip[:, u0:u0 + 496], it1[:, :496], -1.0, 1.0 / 16, ALU.add, ALU.mult),
                          reads=['it1'], writes=['strip'])
                for pcs in range(8):
                    u0 = pcs * 272
                    kb.op(P, lambda u0=u0: nc.gpsimd.iota(it0[:, :272], [[1, 272]], base=u0 + 128, channel_multiplier=-1,
                                                          allow_small_or_imprecise_dtypes=True), writes=['it0'])
                    kb.op(P, lambda u0=u0: nc.gpsimd.iota(it2[:, :272], [[-1, 272]], base=2176 - u0, channel_multiplier=1,
                                                          allow_small_or_imprecise_dtypes=True), writes=['it2'])
                    kb.op(A, lambda h=h: nc.scalar.activation(it0[:, :272], it0[:, :272], AF.Exp, scale=lgf[:, h:h + 1]), reads=['it0', 'lgf'], writes=['it0'])
                    kb.op(A, lambda h=h: nc.scalar.activation(it2[:, :272], it2[:, :272], AF.Exp, scale=lgb[:, h:h + 1]), reads=['it2', 'lgb'], writes=['it2'])
                    kb.op(V, lambda: nc.vector.tensor_tensor(it0[:, :272], it0[:, :272], it2[:, :272], ALU.add), reads=['it0', 'it2'], writes=['it0'])
                    kb.op(V, lambda u0=u0: nc.vector.tensor_scalar(cstrip[:, u0:u0 + 272], it0[:, :272], 1.0 / 16, None, ALU.mult),
                          reads=['it0'], writes=['cstrip'])
                kb.dma('sp', 'dec0', dec_d[h, :, 0:3968], strip[:], reads=['strip'])
                kb.dma('sp', 'dec1', dec_d[h, :, 3968:6144], cstrip[:], reads=['cstrip'])
            pc = 0
            for l in range(2):
                for piece in range(12):
                    s_ = pc % 2
                    kb.dma('sp', 'wst%d' % s_, wst[s_][:],
                           wmod_d[l, :, piece * 512:(piece + 1) * 512].rearrange("(kc p) c -> p kc c", p=128),
                           writes=['wst%d' % s_])
                    for f4 in range(4):
                        for kc in range(8):
                            kb.op(PE, lambda f4=f4, kc=kc, s_=s_: nc.tensor.matmul(
                                pm[s_][:, f4, :], wst[s_][:, kc, f4 * 128:(f4 + 1) * 128], scT[:, kc, :],
                                start=(kc == 0), stop=(kc == 7)),
                                reads=['wst%d' % s_, 'scT'], writes=['pm%d' % s_])
                    for f4 in range(4):
                        fc = piece * 4 + f4
                        is_sc = (fc // 8) in (1, 4)
                        kb.op(V, lambda f4=f4, fc=fc, s_=s_, l=l, is_sc=is_sc: nc.vector.tensor_scalar(
                            mod[:, l, fc, :], pm[s_][:, f4, :], bmod[:, l, fc:fc + 1], 1.0 if is_sc else 0.0,
                            ALU.add, ALU.add),
                            reads=['pm%d' % s_, 'bmod'], writes=[('mod', l, fc)])
                    pc += 1
        kb.barrier()

        def modc(l, which, dc, n):
            base = which * 24
            return (mod[:, l, base + dc, n:n + 1], mod[:, l, base + 8 + dc, n:n + 1], mod[:, l, base + 16 + dc, n:n + 1])

        def rms_block(blk, sqb, pss, rt, R, pskey):
            t0, W = blk_rng(blk)
            for dc in range(8):
                kb.op(A, lambda dc=dc: nc.scalar.activation(sqb[:, dc, :W], xT[:, dc, t0:t0 + W], AF.Square),
                      reads=[xk(dc, blk)], writes=[('sqb', dc)])
            for dc in range(8):
                kb.op(PE, lambda dc=dc: nc.tensor.matmul(pss[:, :W], onesb[:], sqb[:, dc, :W], start=(dc == 0), stop=(dc == 7)),
                      reads=[('sqb', dc)], writes=[pskey])
            kb.op(A, lambda: nc.scalar.activation(rt[:, :W], pss[:, :W], AF.Sqrt, bias=epsn[:, 0:1], scale=1.0 / D),
                  reads=[pskey], writes=['rt'])
            kb.op(V, lambda: nc.vector.reciprocal(R[:, :W], rt[:, :W]), reads=['rt'], writes=['R'])

        def load_x(b):
            with ExitStack() as ps:
                stg = [sb("xstg%d" % i, [128, D], F32, ps) for i in range(4)]
                ptr = [pst("ptr%d" % i, [128, 4, 128], F32, ps) for i in range(4)]
                for tc in range(18):
                    s_ = tc % 4
                    src = x_d[b, tc * 128:(tc + 1) * 128, :] if tc < 16 else ctx_d[b, (tc - 16) * 128:(tc - 15) * 128, :]
                    kb.dma('sp', 'xstg%d' % s_, stg[s_][:], src, writes=['xstg%d' % s_])
                    blk = tc // 4
                    for hf in range(2):
                        pi = (tc % 2) * 2 + hf
                        for d4 in range(4):
                            dc = hf * 4 + d4
                            kb.op(PE, lambda dc=dc, d4=d4, pi=pi, s_=s_: nc.tensor.transpose(
                                ptr[pi][:, d4, :], stg[s_][:, dc * 128:(dc + 1) * 128], identf[:]),
                                reads=['xstg%d' % s_], writes=['ptr%d' % pi])
                        e_ = A if hf == 0 else V
                        f_ = (lambda pi=pi, hf=hf, tc=tc: nc.scalar.copy(xT[:, hf * 4:(hf + 1) * 4, tc * 128:(tc + 1) * 128], ptr[pi][:])) if hf == 0 else \
                             (lambda pi=pi, hf=hf, tc=tc: nc.vector.tensor_copy(xT[:, hf * 4:(hf + 1) * 4, tc * 128:(tc + 1) * 128], ptr[pi][:]))
                        kb.op(e_, f_, reads=['ptr%d' % pi], writes=[('xTl', hf * 4 + d4, tc) for d4 in range(4)])
            kb.barrier()

        def modulate(l, which, b, nblk, ps, router=None):
            sqb = sb("sqb", [128, 8, 512], BF16, ps)
            rt = sb("rt", [128, 512], F32, ps)
            R = sb("R", [128, 512], F32, ps)
            tmp = [sb("mtmp%d" % i, [128, 512], F32, ps) for i in range(2)]
            pss = pst("pss", [128, 512], F32, ps)
            for blk in range(nblk):
                t0, W = blk_rng(blk)
                n = b if blk < 4 else NB
                rms_block(blk, sqb, pss, rt, R, 'pss')
                for dc in range(8):
                    sh, sc, _ = modc(l, which, dc, n)
                    tm = tmp[dc % 2]
                    kb.op(V, lambda dc=dc, tm=tm: nc.vector.tensor_tensor(tm[:, :W], xT[:, dc, t0:t0 + W], R[:, :W], ALU.mult),
                          reads=[xk(dc, blk), 'R'], writes=['mtmp%d' % (dc % 2)])
                    if router is None:
                        kb.op(A, lambda dc=dc, tm=tm, sh=sh, sc=sc: nc.scalar.activation(
                            hT[:, dc, t0:t0 + W], tm[:, :W], AF.Identity, bias=sh, scale=sc),
                            reads=['mtmp%d' % (dc % 2)], writes=[hk(dc, blk)])
                    else:
                        h2f = router['h2f']
                        kb.op(A, lambda dc=dc, tm=tm, sh=sh, sc=sc: nc.scalar.activation(
                            h2f[:, dc, :W], tm[:, :W], AF.Identity, bias=sh, scale=sc),
                            reads=['mtmp%d' % (dc % 2)], writes=[('h2f', dc)])
                        kb.op(P, lambda dc=dc: nc.gpsimd.tensor_copy(hT[:, dc, t0:t0 + W], h2f[:, dc, :W]),
                              reads=[('h2f', dc)], writes=[hk(dc, blk)])
                if router is not None:
                    router['fn'](blk)

        wslot_rr = [0]

        def wload(slot, dst_ap, src_ap, keys=None):
            kb.dma('pool', 'W%d' % slot, dst_ap, src_ap, writes=keys or ['W%d' % slot])

        def retention(b):
            with ExitStack() as ps:
                modulate(0, 0, b, 5, ps)
            kb.barrier()
            with ExitStack() as ps:
                kT = sb("kT", [128, 2, T], BF16, ps)
                qT = sb("qT", [128, 2, 512], BF16, ps)
                vh = sb("vh", [128, 18, 512], BF16, ps)
                strip = sb("strip", [128, 3968], BF16, ps)
                cstrip = sb("cstrip", [128, 2176], BF16, ps)
                PT = [sb("PT%d" % i, [128, 512], BF16, ps) for i in range(2)]
                rA = sb("rA", [128, 512], F32, ps)
                rB = sb("rB", [128, 512], F32, ps)
                zb = [sb("zb%d" % i, [128, 512], BF16, ps) for i in range(2)]
                zT = sb("zT", [128, 4, 512], BF16, ps)
                dg = sb("dg", [128, 4, 128], BF16, ps)
                st6 = sb("st6", [128, 4, 6], F32, ps)
                mv = sb("mv", [128, 4, 2], F32, ps)
                rs = sb("rs", [128, 4], F32, ps)
                psS = [pst("psS%d" % i, [128, 512], F32, ps) for i in range(2)]
                psY = [pst("psY%d" % i, [128, 512], F32, ps) for i in range(4)]
                psM = pst("psM", [128, 512], F32, ps)
                psTb = pst("psTb", [128, 4, 256], BF16, ps)
                psM2 = psTb[:].rearrange("p a b -> p (a b)").bitcast(F32)

                def wslot():
                    s_ = wslot_rr[0] % 4
                    wslot_rr[0] += 1
                    return s_

                c_row, s_row = rtab[:, 0:32], rtab[:, 32:64]
                c_col, s_col = rtab[:, 64:128], rtab[:, 128:192]

                def rope_tabs(j, t0, W):
                    nr = W // 64
                    if j == 0:
                        r0 = t0 // 64
                        return (c_row[:, r0:r0 + nr].unsqueeze(2).to_broadcast([128, nr, 64]),
                                s_row[:, r0:r0 + nr].unsqueeze(2).to_broadcast([128, nr, 64]))
                    return (c_col.unsqueeze(1).to_broadcast([128, nr, 64]),
                            s_col.unsqueeze(1).to_broadcast([128, nr, 64]))

                def v3(ap, W):
                    return ap.rearrange("p (a b) -> p a b", b=64)

                def proj_rope(ws, dstT, dkey, blk, alt=0):
                    t0, W = blk_rng(blk)
                    wv_ = WA[:, ws, :].rearrange("p (s k c) -> p s k c", s=2, k=8)
                    for j in range(2):
                        if alt and j == 1:
                            pA, pAk, pB, pBk = psS[0], 'psS0', psS[1], 'psS1'
                        else:
                            pA, pAk, pB, pBk = psM, 'psM', psM2, 'psTb'
                        for kc in range(8):
                            kb.op(PE, lambda kc=kc, j=j: nc.tensor.matmul(pA[:, :W], wv_[:, 0, kc, j * 128:(j + 1) * 128], hT[:, kc, t0:t0 + W],
                                                                         start=(kc == 0), stop=(kc == 7)),
                                  reads=['W%d' % ws] + hks(blk), writes=[pAk])
                        if blk < 4:
                            for kc in range(8):
                                kb.op(PE, lambda kc=kc, j=j: nc.tensor.matmul(pB[:, :W], wv_[:, 1, kc, j * 128:(j + 1) * 128], hT[:, kc, t0:t0 + W],
                                                                             start=(kc == 0), stop=(kc == 7)),
                                      reads=['W%d' % ws] + hks(blk), writes=[pBk])
                            ct, st_ = rope_tabs(j, t0, W)
                            kb.op(V, lambda ct=ct: nc.vector.tensor_tensor(v3(rA[:, :W], W), v3(pA[:, :W], W), ct, ALU.mult),
                                  reads=[pAk], writes=['rA'])
                            kb.op(V, lambda st_=st_: nc.vector.tensor_tensor(v3(rB[:, :W], W), v3(pB[:, :W], W), st_, ALU.mult),
                                  reads=[pBk], writes=['rB'])
                            kb.op(P, lambda j=j: nc.gpsimd.tensor_tensor(dstT[:, j, t0 - dkey[1]:t0 - dkey[1] + W], rA[:, :W], rB[:, :W], ALU.add),
                                  reads=['rA', 'rB'], writes=[(dkey[0], j, blk)])
                        else:
                            kb.op(A, lambda j=j: nc.scalar.copy(dstT[:, j, t0 - dkey[1]:t0 - dkey[1] + W], pA[:, :W]),
                                  reads=[pAk], writes=[(dkey[0], j, blk)])

                for h in range(4):
                    kb.dma('sp', 'dec0', strip[:], dec_d[h, :, 0:3968], writes=['strip'])
                    kb.dma('sp', 'dec1', cstrip[:], dec_d[h, :, 3968:6144], writes=['cstrip'])
                    ws = wslot()
                    wdst = WA[:, ws, :].rearrange("p (s k c) -> p s k c", s=2, k=8)
                    c0 = 1024 + h * 256
                    wload(ws, wdst[:, 0], rwin_d[:, c0:c0 + 256].rearrange("(kc p) c -> p kc c", p=128))
                    wload(ws, wdst[:, 1], rwin_d[:, RET_IN + c0:RET_IN + c0 + 256].rearrange("(kc p) c -> p kc c", p=128))
                    for blk in range(5):
                        proj_rope(ws, kT, ('kT', 0), blk, alt=1)
                    ws = wslot()
                    wv_ = WA[:, ws, :].rearrange("p (k c) -> p k c", k=8)
                    c0 = 2048 + h * 512
                    wload(ws, wv_, rwin_d[:, c0:c0 + 512].rearrange("(kc p) c -> p kc c", p=128))
                    for tc in range(18):
                        blk = tc // 4
                        pv, pvk = (psM, 'psM') if tc % 2 == 0 else (psM2, 'psTb')
                        for kc in range(8):
                            kb.op(PE, lambda kc=kc, tc=tc, pv=pv: nc.tensor.matmul(pv[:], hT[:, kc, tc * 128:(tc + 1) * 128], wv_[:, kc, :],
                                                                                  start=(kc == 0), stop=(kc == 7)),
                                  reads=['W%d' % ws] + hks(blk), writes=[pvk])
                        if tc % 2 == 0:
                            kb.op(A, lambda tc=tc, pv=pv: nc.scalar.copy(vh[:, tc, :], pv[:]), reads=[pvk], writes=[('vh', tc)])
                        else:
                            kb.op(V, lambda tc=tc, pv=pv: nc.vector.tensor_copy(vh[:, tc, :], pv[:]), reads=[pvk], writes=[('vh', tc)])
                    wsq = wslot()
                    wdq = WA[:, wsq, :].rearrange("p (s k c) -> p s k c", s=2, k=8)
                    c0 = h * 256
                    wload(wsq, wdq[:, 0], rwin_d[:, c0:c0 + 256].rearrange("(kc p) c -> p kc c", p=128))
                    wload(wsq, wdq[:, 1], rwin_d[:, RET_IN + c0:RET_IN + c0 + 256].rearrange("(kc p) c -> p kc c", p=128))
                    wsg = wslot()
                    wg_ = WA[:, wsg, :].rearrange("p (k c) -> p k c", k=8)
                    c0 = 4096 + h * 512
                    wload(wsg, wg_, rwin_d[:, c0:c0 + 512].rearrange("(kc p) c -> p kc c", p=128))
                    wso = wslot()
                    wo_ = WA[:, wso, :].rearrange("p (k c) -> p k c", k=4)
                    wload(wso, wo_, rwout_d[h * 512:(h + 1) * 512, :].rearrange("(hc p) d -> p hc d", p=128))
                    for qb in range(5):
                        t0, W = blk_rng(qb)
                        nts = W // 128
                        proj_rope(wsq, qT, ('qT', t0), qb)
                        scs = list(range(18)) if qb < 4 else [16, 17]
                        def Sm(si):
                            sc = scs[si]
                            pS = psS[si % 2]
                            for j in range(2):
                                kb.op(PE, lambda j=j: nc.tensor.matmul(pS[:, :W], kT[:, j, sc * 128:(sc + 1) * 128], qT[:, j, :W],
                                                                      start=(j == 0), stop=(j == 1)),
                                      reads=[('kT', j, sc // 4), ('qT', 0, qb), ('qT', 1, qb)], writes=['psS%d' % (si % 2)])
                            if sc < 16:
                                off = t0 - sc * 128 + 1920
                                dk, dkey = strip[:, off:off + W], 'strip'
                            elif qb < 4:
                                off = t0 - 128 * (sc - 16) + 128
                                dk, dkey = cstrip[:, off:off + W], 'cstrip'
                            else:
                                off = 1920 - 128 * (sc - 16)
                                dk, dkey = strip[:, off:off + W], 'strip'
                            pt = PT[si % 2]
                            kb.op(V, lambda: nc.vector.tensor_tensor(pt[:, :W], pS[:, :W], dk, ALU.mult),
                                  reads=['psS%d' % (si % 2), dkey], writes=['PT%d' % (si % 2)])

                        def PVm(si):
                            sc = scs[si]
                            pt = PT[si % 2]
                            for ts in range(nts):
                                kb.op(PE, lambda ts=ts: nc.tensor.matmul(
                                    psY[ts][:], pt[:, ts * 128:(ts + 1) * 128], vh[:, sc, :], start=(si == 0), stop=(si == len(scs) - 1)),
                                    reads=['PT%d' % (si % 2), ('vh', sc)], writes=['psY%d' % ts])

                        Sm(0)
                        for si in range(len(scs)):
                            if si + 1 < len(scs):
                                Sm(si + 1)
                            PVm(si)
                        n = b if qb < 4 else NB
                        sgs = [rA, rB]
                        for ts in range(nts):
                            kb.op(V, lambda ts=ts: nc.vector.bn_stats(st6[:, ts, :], psY[ts][:]), reads=['psY%d' % ts], writes=[('st6', ts)])
                            kb.op(V, lambda ts=ts: nc.vector.bn_aggr(mv[:, ts, :], st6[:, ts, :]), reads=[('st6', ts)], writes=[('mv', ts)])
                        kb.op(A, lambda: nc.scalar.activation(rs[:, :nts], mv[:, :nts, 1], AF.Sqrt, bias=epsh[:, 0:1], scale=1.0),
                              reads=[('mv', i) for i in range(nts)], writes=['rs'])
                        kb.op(V, lambda: nc.vector.reciprocal(rs[:, :nts], rs[:, :nts]), reads=['rs'], writes=['rs'])
                        for ts in range(nts):
                            kb.op(P, lambda ts=ts: nc.gpsimd.tensor_scalar(dg[:, ts, :], identb[:], rs[:, ts:ts + 1], None, ALU.mult),
                                  reads=['rs'], writes=[('dg', ts)])

                        def gproj(ts):
                            tk0 = t0 + ts * 128
                            pg = psS[ts % 2]
                            for kc in range(8):
                                kb.op(PE, lambda kc=kc: nc.tensor.matmul(pg[:], hT[:, kc, tk0:tk0 + 128], wg_[:, kc, :], start=(kc == 0), stop=(kc == 7)),
                                      reads=['W%d' % wsg] + hks(qb), writes=['psS%d' % (ts % 2)])
                            sg_ = sgs[ts % 2]
                            sk = 'rA' if ts % 2 == 0 else 'rB'
                            zb_ = zb[ts % 2]
                            kb.op(A, lambda: nc.scalar.activation(sg_[:], pg[:], AF.Silu), reads=['psS%d' % (ts % 2)], writes=[sk])
                            kb.op(V, lambda: nc.vector.scalar_tensor_tensor(zb_[:], psY[ts][:], mv[:, ts, 0:1], sg_[:], ALU.subtract, ALU.mult),
                                  reads=['psY%d' % ts, ('mv', ts), sk], writes=['zb%d' % (ts % 2)])

                        def ztrans(ts):
                            zb_ = zb[ts % 2]
                            pz = psM2.rearrange("p (a b) -> p a b", b=128)
                            for hc in range(4):
                                kb.op(PE, lambda hc=hc: nc.tensor.matmul(pz[:, hc, :], zb_[:, hc * 128:(hc + 1) * 128], dg[:, ts, :], start=True, stop=True),
                                      reads=['zb%d' % (ts % 2), ('dg', ts)], writes=['psTb'])
                            kb.op(A, lambda: nc.scalar.copy(zT[:, :, ts * 128:(ts + 1) * 128], pz[:]), reads=['psTb'], writes=['zT'])

                        gproj(0)
                        for ts in range(nts):
                            if ts + 1 < nts:
                                gproj(ts + 1)
                            ztrans(ts)
                        for dc in range(8):
                            po, pok = (psM, 'psM') if dc % 2 == 0 else (psM2, 'psTb')
                            for hc in range(4):
                                kb.op(PE, lambda hc=hc, dc=dc, po=po: nc.tensor.matmul(po[:, :W], wo_[:, hc, dc * 128:(dc + 1) * 128], zT[:, hc, :W],
                                                                                      start=(hc == 0), stop=(hc == 3)),
                                      reads=['W%d' % wso, 'zT'], writes=[pok])
                            g1 = modc(0, 0, dc, n)[2]
                            kb.op(V, lambda dc=dc, g1=g1, po=po: nc.vector.scalar_tensor_tensor(
                                xT[:, dc, t0:t0 + W], po[:, :W], g1, xT[:, dc, t0:t0 + W], ALU.mult, ALU.add),
                                reads=[pok, xk(dc, qb)], writes=[xk(dc, qb)])
            kb.barrier()

        def moe(l, b):
            nblk = 5 if l == 0 else 4
            nch = 18 if l == 0 else 16
            NBK = nch + 32
            with ExitStack() as ps:
                wr = sb("wr", [128, 8, 36], F32, ps)
                br = sb("br", [128, 36], F32, ps)
                gwall = sb("gwall", [128, 18, 32], F32, ps)
                Mall = sb("Mall", [128, 18, 32], F32, ps)
                Call = sb("Call", [128, 18, 32], F32, ps)
                Macc = sb("Macc", [128, 32], F32, ps)
                base_rep = sb("base_rep", [128, 32], F32, ps)
                idxw = sb("idxw", [128, 68], mybir.dt.int32, ps)
                dA = sb("dA", [128, 18], mybir.dt.int32, ps)
                dB = sb("dB", [128, 18], mybir.dt.int32, ps)
                gAB = sb("gAB", [128, 2, 18], F32, ps)
                kb.dma('sp', 'c0', wr[:], wr_d[l].rearrange("(kc p) n -> p kc n", p=128), writes=['wr'])
                kb.dma('sp', 'c1', br[:], br_d[l:l + 1, :].partition_broadcast(128), writes=['br'])
                kb.op(V, lambda: nc.vector.memset(Macc[:], 0.0), writes=['Macc'])
                with ExitStack() as ps2:
                    h2f = sb("h2f", [128, 8, 512], F32, ps2)
                    Lg = sb("Lg", [128, 4, 36], F32, ps2)
                    EM = sb("EM", [128, 4, 32], F32, ps2)
                    EM2 = sb("EM2", [128, 4, 32], F32, ps2)
                    pe_ = sb("pe_", [128, 4, 32], F32, ps2)
                    gx = sb("gx", [128, 4, 4], F32, ps2)
                    gm = sb("gm", [128, 4, 4], F32, ps2)
                    c4 = sb("c4", [128, 6, 4], F32, ps2)
                    msum = sb("msum", [128, 32], F32, ps2)
                    psL = pst("psL", [128, 4, 36], F32, ps2)
                    psC = pst("psC", [128, 4, 32], F32, ps2)

                    def router_fn(blk):
                        t0, W = blk_rng(blk)
                        nts = W // 128
                        tc0 = t0 // 128
                        gw4 = gwall[:, tc0:tc0 + nts, :]
                        M4 = Mall[:, tc0:tc0 + nts, :]
                        L4 = Lg[:, :nts, :]
                        G4 = L4[:, :, 0:4]
                        E4 = EM[:, :nts, :]
                        E24 = EM2[:, :nts, :]
                        P4 = pe_[:, :nts, :]

                        def bc(col, k):
                            return col.unsqueeze(2).to_broadcast([128, nts, k])

                        for ts in range(nts):
                            for kc in range(8):
                                kb.op(PE, lambda kc=kc, ts=ts: nc.tensor.matmul(psL[:, ts, :], h2f[:, kc, ts * 128:(ts + 1) * 128], wr[:, kc, :],
                                                                               start=(kc == 0), stop=(kc == 7)),
                                      reads=['wr'] + [('h2f', dc) for dc in range(8)], writes=['psL'])
                        kb.op(V, lambda: nc.vector.tensor_tensor(L4, psL[:, :nts, :], br[:].unsqueeze(1).to_broadcast([128, nts, 36]), ALU.add),
                              reads=['psL', 'br'], writes=['Lg'])
                        kb.op(V, lambda: nc.vector.reduce_max(c4[:, 0, :nts], G4, AX.X), reads=['Lg'], writes=['c4a'])
                        kb.op(V, lambda: nc.vector.tensor_tensor(gx[:, :nts, :], G4, bc(c4[:, 0, :nts], 4), ALU.subtract), reads=['Lg', 'c4a'], writes=['gx'])
                        kb.op(A, lambda: nc.scalar.activation(gx[:, :nts, :], gx[:, :nts, :], AF.Exp), reads=['gx'], writes=['gx'])
                        kb.op(V, lambda: nc.vector.reduce_sum(c4[:, 1, :nts], gx[:, :nts, :], AX.X), reads=['gx'], writes=['c4b'])
                        kb.op(V, lambda: nc.vector.tensor_tensor(gm[:, :nts, :], G4, bc(c4[:, 0, :nts], 4), ALU.is_ge), reads=['Lg', 'c4a'], writes=['gm'])
                        kb.op(V, lambda: nc.vector.tensor_scalar(gm[:, :nts, :], gm[:, :nts, :], -1.0, BIG, ALU.add, ALU.mult), reads=['gm'], writes=['gm'])
                        kb.op(V, lambda: nc.vector.tensor_tensor(E4.rearrange("p t (g e) -> p t g e", e=8),
                                                                 L4[:, :, 4:36].rearrange("p t (g e) -> p t g e", e=8),
                                                                 gm[:, :nts, :].unsqueeze(3).to_broadcast([128, nts, 4, 8]), ALU.add),
                              reads=['Lg', 'gm'], writes=['EM'])
                        kb.op(V, lambda: nc.vector.reduce_max(c4[:, 2, :nts], E4, AX.X), reads=['EM'], writes=['c4c'])
                        kb.op(V, lambda: nc.vector.tensor_tensor(E24, E4, bc(c4[:, 2, :nts], 32), ALU.is_ge), reads=['EM', 'c4c'], writes=['EM2'])
                        kb.op(V, lambda: nc.vector.scalar_tensor_tensor(E24, E24, -BIG, E4, ALU.mult, ALU.add), reads=['EM', 'EM2'], writes=['EM2'])
                        kb.op(V, lambda: nc.vector.reduce_max(c4[:, 3, :nts], E24, AX.X), reads=['EM2'], writes=['c4d'])
                        kb.op(V, lambda: nc.vector.tensor_tensor(P4, E4, bc(c4[:, 2, :nts], 32), ALU.subtract), reads=['EM', 'c4c'], writes=['pe_'])
                        kb.op(A, lambda: nc.scalar.activation(P4, P4, AF.Exp), reads=['pe_'], writes=['pe_'])
                        kb.op(V, lambda: nc.vector.tensor_tensor(M4, E4, bc(c4[:, 3, :nts], 32), ALU.is_ge), reads=['EM', 'c4d'], writes=[('M', blk)])
                        kb.op(V, lambda: nc.vector.tensor_tensor(P4, P4, M4, ALU.mult), reads=['pe_', ('M', blk)], writes=['pe_'])
                        kb.op(V, lambda: nc.vector.reduce_sum(c4[:, 4, :nts], P4, AX.X), reads=['pe_'], writes=['c4e'])
                        kb.op(V, lambda: nc.vector.tensor_tensor(c4[:, 4, :nts], c4[:, 4, :nts], c4[:, 1, :nts], ALU.mult), reads=['c4e', 'c4b'], writes=['c4e'])
                        kb.op(V, lambda: nc.vector.reciprocal(c4[:, 5, :nts], c4[:, 4, :nts]), reads=['c4e'], writes=['c4f'])
                        kb.op(V, lambda: nc.vector.tensor_tensor(gw4, P4, bc(c4[:, 5, :nts], 32), ALU.mult), reads=['pe_', 'c4f'], writes=[('gw', blk)])
                        for ts in range(nts):
                            kb.op(PE, lambda ts=ts: nc.tensor.matmul(psC[:, ts, :], triU[:], Mall[:, tc0 + ts, :], start=True, stop=False),
                                  reads=[('M', blk), 'triU'], writes=['psC'])
                            for t2 in range(ts):
                                kb.op(PE, lambda ts=ts, t2=t2: nc.tensor.matmul(psC[:, ts, :], onesf[:], Mall[:, tc0 + t2, :], start=False, stop=False),
                                      reads=[('M', blk)], writes=['psC'])
                            kb.op(PE, lambda ts=ts: nc.tensor.matmul(psC[:, ts, :], onesf[:], Macc[:], start=False, stop=True),
                                  reads=['Macc', 'onesf'], writes=['psC'])
                        kb.op(A, lambda: nc.scalar.copy(Call[:, tc0:tc0 + nts, :], psC[:, :nts, :]), reads=['psC'], writes=[('C', blk)])
                        kb.op(V, lambda: nc.vector.reduce_sum(msum[:], M4.rearrange("p t e -> p e t"), AX.X), reads=[('M', blk)], writes=['msum'])
                        kb.op(V, lambda: nc.vector.tensor_tensor(Macc[:], Macc[:], msum[:], ALU.add), reads=['Macc', 'msum'], writes=['Macc'])

                    modulate(l, 1, b, nblk, ps2, router={'h2f': h2f, 'fn': router_fn})
                    cnt = sb("cnt", [32, 4], F32, ps2)
                    cmp18 = sb("cmp18", [32, 18], F32, ps2)
                    padb = sb("padb", [32, 128], F32, ps2)
                    end_rep = sb("end_rep", [128, 32], F32, ps2)
                    cmpb = sb("cmpb", [128, 68, 32], F32, ps2)
                    bef = sb("bef", [128, 68], F32, ps2)
                    psK = pst("psK", [32, 1], F32, ps2)
                    psB = pst("psB", [128, 32], F32, ps2)
                    psE = pst("psE", [128, 32], F32, ps2)
                    kb.op(PE, lambda: nc.tensor.matmul(psK[:], Macc[:], onesf[:, 0:1], start=True, stop=True), reads=['Macc'], writes=['psK'])
                    kb.op(V, lambda: nc.vector.tensor_copy(cnt[:, 0:1], psK[:]), reads=['psK'], writes=['cnt0'])
                    kb.op(V, lambda: nc.vector.tensor_scalar(cmp18[:], thr18[:], cnt[:, 0:1], None, ALU.is_lt), reads=['cnt0', 'thr18'], writes=['cmp18'])
                    kb.op(V, lambda: nc.vector.reduce_sum(cnt[:, 1:2], cmp18[:], AX.X), reads=['cmp18'], writes=['cnt1'])
                    kb.op(V, lambda: nc.vector.tensor_scalar(cnt[:, 2:3], cnt[:, 1:2], 256.0, None, ALU.mult), reads=['cnt1'], writes=['cnt2'])
                    kb.op(V, lambda: nc.vector.tensor_copy(padb[:], cnt[:, 2:3].to_broadcast([32, 128])), reads=['cnt2'], writes=['padb'])
                    kb.op(PE, lambda: nc.tensor.matmul(psB[:], padb[:], triU[0:32, 0:32], start=True, stop=True), reads=['padb', 'triU'], writes=['psB'])
                    kb.op(PE, lambda: nc.tensor.matmul(psE[:], padb[:], UI[:], start=True, stop=True), reads=['padb', 'UI'], writes=['psE'])
                    kb.op(V, lambda: nc.vector.tensor_copy(base_rep[:], psB[:]), reads=['psB'], writes=['base_rep'])
                    kb.op(V, lambda: nc.vector.tensor_copy(end_rep[:], psE[:]), reads=['psE'], writes=['end_rep'])
                    kb.op(V, lambda: nc.vector.tensor_tensor(cmpb[:], end_rep[:].unsqueeze(1).to_broadcast([128, 68, 32]),
                                                             thr68[:].unsqueeze(2).to_broadcast([128, 68, 32]), ALU.is_le),
                          reads=['end_rep', 'thr68'], writes=['cmpb'])
                    kb.op(V, lambda: nc.vector.reduce_sum(bef[:], cmpb[:], AX.X), reads=['cmpb'], writes=['bef'])
                    kb.op(V, lambda: nc.vector.tensor_scalar(bef[:], bef[:], 31.0, 128.0, ALU.min, ALU.mult), reads=['bef'], writes=['bef'])
                    kb.op(V, lambda: nc.vector.tensor_scalar(bef[:], bef[:], iop[:, 0:1], float(l * 4096), ALU.add, ALU.add), reads=['bef', 'iop'], writes=['bef'])
                    garb = sb("garb", [128, 68], F32, ps2)
                    kb.op(V, lambda: nc.vector.tensor_scalar(garb[:], thr68[:], end_rep[:, 31:32], 1.0e6, ALU.is_ge, ALU.mult),
                          reads=['end_rep', 'thr68'], writes=['garb'])
                    kb.op(V, lambda: nc.vector.tensor_tensor(bef[:], bef[:], garb[:], ALU.add), reads=['bef', 'garb'], writes=['bef'])
                    kb.op(V, lambda: nc.vector.tensor_copy(idxw[:], bef[:]), reads=['bef'], writes=['idxw'])
                kb.barrier()
                with ExitStack() as ps2:
                    vv_ = sb("vv_", [128, 32], F32, ps2)
                    v2_ = sb("v2_", [128, 32], F32, ps2)
                    eq_ = sb("eq_", [128, 32], F32, ps2)
                    dcol = sb("dcol", [128, 4], F32, ps2)
                    Xtok = [sb("Xtok%d" % i, [128, D], BF16, ps2) for i in range(2)]
                    psXt = [pst("psXt%d" % i, [128, 8, 128], BF16, ps2) for i in range(2)]
                    for tc in range(nch):
                        blk = tc // 4
                        s_ = tc % 2
                        kb.op(V, lambda tc=tc: nc.vector.tensor_tensor(vv_[:], Call[:, tc, :], base_rep[:], ALU.add), reads=[], writes=['vv_'])
                        kb.op(V, lambda tc=tc: nc.vector.scalar_tensor_tensor(vv_[:], vv_[:], 1.0, Mall[:, tc, :], ALU.add, ALU.mult), reads=['vv_'], writes=['vv_'])
                        kb.op(V, lambda: nc.vector.reduce_max(dcol[:, 0:1], vv_[:], AX.X), reads=['vv_'], writes=['dc0'])
                        kb.op(V, lambda: nc.vector.tensor_scalar(eq_[:], vv_[:], dcol[:, 0:1], None, ALU.is_equal), reads=['vv_', 'dc0'], writes=['eq_'])
                        kb.op(V, lambda tc=tc: nc.vector.tensor_tensor(v2_[:], eq_[:], gwall[:, tc, :], ALU.mult), reads=['eq_'], writes=['v2_'])
                        kb.op(V, lambda tc=tc: nc.vector.reduce_sum(gAB[:, 0, tc:tc + 1], v2_[:], AX.X), reads=['v2_'], writes=[('gA', tc)])
                        kb.op(V, lambda: nc.vector.tensor_scalar(eq_[:], vv_[:], dcol[:, 0:1], None, ALU.not_equal), reads=['vv_', 'dc0', 'v2_'], writes=['eq_'])
                        kb.op(V, lambda: nc.vector.tensor_tensor(v2_[:], vv_[:], eq_[:], ALU.mult), reads=['eq_', 'vv_'], writes=['v2_'])
                        kb.op(V, lambda: nc.vector.reduce_max(dcol[:, 1:2], v2_[:], AX.X), reads=['v2_'], writes=['dc1'])
                        kb.op(V, lambda: nc.vector.tensor_scalar(eq_[:], v2_[:], dcol[:, 1:2], None, ALU.is_equal), reads=['v2_', 'dc1'], writes=['eq_'])
                        kb.op(V, lambda tc=tc: nc.vector.tensor_tensor(v2_[:], eq_[:], gwall[:, tc, :], ALU.mult), reads=['eq_'], writes=['v2_'])
                        kb.op(V, lambda tc=tc: nc.vector.reduce_sum(gAB[:, 1, tc:tc + 1], v2_[:], AX.X), reads=['v2_'], writes=[('gB', tc)])
                        kb.op(V, lambda: nc.vector.tensor_scalar(dcol[:, 2:4], dcol[:, 0:2], -1.0, None, ALU.add), reads=['dc0', 'dc1'], writes=['dc23'])
                        kb.op(V, lambda tc=tc: nc.vector.tensor_copy(dA[:, tc:tc + 1], dcol[:, 2:3]), reads=['dc23'], writes=[('dA', tc)])
                        kb.op(V, lambda tc=tc: nc.vector.tensor_copy(dB[:, tc:tc + 1], dcol[:, 3:4]), reads=['dc23'], writes=[('dB', tc)])
                        for dc in range(8):
                            kb.op(PE, lambda dc=dc, tc=tc, s_=s_: nc.tensor.transpose(psXt[s_][:, dc, :], hT[:, dc, tc * 128:(tc + 1) * 128], identb[:]),
                                  reads=[hk(dc, blk)], writes=['psXt%d' % s_])
                        kb.op(A, lambda s_=s_: nc.scalar.copy(Xtok[s_][:], psXt[s_][:].rearrange("p a b -> p (a b)")), reads=['psXt%d' % s_], writes=['Xtok%d' % s_])
                        for (dd, dk_) in ((dA, 'dA'), (dB, 'dB')):
                            kb.dma('pool', 'scat%d' % s_, Xs_d[:, :], Xtok[s_][:, :], reads=['Xtok%d' % s_, (dk_, tc)], writes=['Xs'],
                                   indirect=('out', dd[:, tc:tc + 1]))
                kb.barrier()
                with ExitStack() as ps2:
                    GU = [WA[:, 0:2, :], WA[:, 2:4, :]]
                    Dn = [sb("Dn%d" % i, [128, 4096], BF16, ps2) for i in range(2)]
                    Xb = [sb("Xb%d" % i, [128, 2, D], BF16, ps2) for i in range(2)]
                    XT = [sb("XT%d" % i, [128, 2, 8, 128], BF16, ps2) for i in range(2)]
                    sgt = [sb("sgt%d" % i, [128, 512], F32, ps2) for i in range(2)]
                    act = [sb("act%d" % i, [128, 2, 512], BF16, ps2) for i in range(2)]
                    actT = [sb("actT%d" % i, [128, 4, 128], BF16, ps2) for i in range(2)]
                    Yb_ = [sb("Yb%d" % i, [128, D], F32, ps2) for i in range(2)]
                    psXT = pst("psXT", [128, 8, 128], BF16, ps2)
                    psG = [pst("psG%d" % i, [128, 512], F32, ps2) for i in range(2)]
                    psU = [pst("psU%d" % i, [128, 512], F32, ps2) for i in range(2)]
                    psAT = pst("psAT", [128, 4, 256], BF16, ps2)
                    psD = [pst("psD%d" % i, [128, 512], F32, ps2) for i in range(2)]

                    def dmaGU(j):
                        s_ = j % 2
                        kb.dma('pool', 'RWg%d' % s_, GU[s_][:, 0, :], w16_d[0][:, :], reads=['idxw'], writes=['RWg%d' % s_], indirect=('in', idxw[:, j:j + 1], bcw))
                        kb.dma('pool', 'RWu%d' % s_, GU[s_][:, 1, :], w16_d[1][:, :], reads=['idxw'], writes=['RWu%d' % s_], indirect=('in', idxw[:, j:j + 1], bcw))

                    def dmaDn(j):
                        s_ = j % 2
                        kb.dma('pool', 'RWd%d' % s_, Dn[s_][:, :], w16_d[2][:, :], reads=['idxw'], writes=['RWd%d' % s_], indirect=('in', idxw[:, j:j + 1], bcw))

                    def xload(j):
                        s_ = j % 2
                        kb.dma('sp', 'xb%d' % s_, Xb[s_][:], Xs_d[j * 256:(j + 1) * 256, :].rearrange("(u p) d -> p u d", p=128),
                               reads=['Xs'], writes=['Xb%d' % s_])

                    def Tx(j, u):
                        s_ = j % 2
                        for dc in range(8):
                            kb.op(PE, lambda dc=dc: nc.tensor.transpose(psXT[:, dc, :], Xb[s_][:, u, dc * 128:(dc + 1) * 128], identb[:]),
                                  reads=['Xb%d' % s_], writes=['psXT'])
                        kb.op(A, lambda: nc.scalar.copy(XT[s_][:, u], psXT[:]), reads=['psXT'], writes=[('XT', s_, u)])

                    def GUm(j, u):
                        s_ = j % 2
                        wg_ = GU[s_][:, 0, :].rearrange("p (k c) -> p k c", k=8)
                        wu_ = GU[s_][:, 1, :].rearrange("p (k c) -> p k c", k=8)
                        for kc in range(8):
                            kb.op(PE, lambda kc=kc: nc.tensor.matmul(psG[u][:], XT[s_][:, u, kc, :], wg_[:, kc, :], start=(kc == 0), stop=(kc == 7)),
                                  reads=[('XT', s_, u), 'RWg%d' % s_], writes=['psG%d' % u])
                        for kc in range(8):
                            kb.op(PE, lambda kc=kc: nc.tensor.matmul(psU[u][:], XT[s_][:, u, kc, :], wu_[:, kc, :], start=(kc == 0), stop=(kc == 7)),
                                  reads=[('XT', s_, u), 'RWu%d' % s_], writes=['psU%d' % u])
                        kb.op(A, lambda: nc.scalar.activation(sgt[u][:], psG[u][:], AF.Silu), reads=['psG%d' % u], writes=['sgt%d' % u])
                        kb.op(V, lambda: nc.vector.tensor_tensor(act[s_][:, u, :], psU[u][:], sgt[u][:], ALU.mult),
                              reads=['psU%d' % u, 'sgt%d' % u], writes=[('act', s_, u)])

                    def Tact(j, u):
                        s_ = j % 2
                        for hc in range(4):
                            kb.op(PE, lambda hc=hc: nc.tensor.transpose(psAT[:, hc, 0:128], act[s_][:, u, hc * 128:(hc + 1) * 128], identb[:]),
                                  reads=[('act', s_, u)], writes=['psAT'])
                        kb.op(A, lambda: nc.scalar.copy(actT[u][:], psAT[:, :, 0:128]), reads=['psAT'], writes=['actT%d' % u])

                    def Dm(j, u):
                        s_ = j % 2
                        wd_ = Dn[s_][:, :].rearrange("p (k c) -> p k c", k=4)
                        for c2 in range(2):
                            for hc in range(4):
                                kb.op(PE, lambda hc=hc, c2=c2: nc.tensor.matmul(psD[c2][:], actT[u][:, hc, :], wd_[:, hc, c2 * 512:(c2 + 1) * 512],
                                                                               start=(hc == 0), stop=(hc == 3)),
                                      reads=['actT%d' % u, 'RWd%d' % s_], writes=['psD%d' % c2])
                        kb.op(A, lambda: nc.scalar.copy(Yb_[u][:, 0:512], psD[0][:]), reads=['psD0'], writes=['Yb%da' % u])
                        kb.op(V, lambda: nc.vector.tensor_copy(Yb_[u][:, 512:1024], psD[1][:]), reads=['psD1'], writes=['Yb%db' % u])
                        r0 = (j * 2 + u) * 128
                        kb.dma('sp', 'ys%d' % u, Ys_d[r0:r0 + 128, :], Yb_[u][:], reads=['Yb%da' % u, 'Yb%db' % u], writes=['Ys'])

                    if ('pc%d' % l) in kb.lazy:
                        kb.lazy.discard('pc%d' % l)
                    kb._wait('pool', ('dma', 'pc%d' % l), kb.dcnt['pc%d' % l])
                    dmaGU(0)
                    dmaGU(1)
                    dmaDn(0)
                    dmaDn(1)
                    xload(0)
                    xload(1)
                    Tx(0, 0)
                    Tx(0, 1)
                    GUm(0, 0)
                    GUm(0, 1)
                    dmaGU(2)
                    for j in range(NBK):
                        nx = j + 1 < NBK
                        if j + 2 < NBK:
                            xload(j + 2)
                        if nx:
                            Tx(j + 1, 0)
                        Tact(j, 0)
                        if nx:
                            Tx(j + 1, 1)
                        Dm(j, 0)
                        if nx:
                            GUm(j + 1, 0)
                        Tact(j, 1)
                        if nx:
                            GUm(j + 1, 1)
                            if j + 3 < NBK:
                                dmaGU(j + 3)
                        Dm(j, 1)
                        if j + 2 < NBK:
                            dmaDn(j + 2)
                kb.barrier()
                with ExitStack() as ps2:
                    Ya = [sb("Ya%d" % i, [128, D], F32, ps2) for i in range(2)]
                    Yc = [sb("Yc%d" % i, [128, D], F32, ps2) for i in range(2)]
                    ff = sb("ff", [128, D], F32, ps2)
                    psF = [pst("psF%d" % i, [128, 4, 128], F32, ps2) for i in range(2)]
                    for tc in range(nch):
                        blk = tc // 4
                        n = b if blk < 4 else NB
                        s_ = tc % 2
                        kb.dma('pool', 'ga%d' % s_, Ya[s_][:, :], Ys_d[:, :], reads=['Ys'], writes=['Ya%d' % s_], indirect=('in', dA[:, tc:tc + 1]))
                        kb.dma('pool', 'gb%d' % s_, Yc[s_][:, :], Ys_d[:, :], reads=['Ys'], writes=['Yc%d' % s_], indirect=('in', dB[:, tc:tc + 1]))
                        kb.op(A, lambda s_=s_, tc=tc: nc.scalar.activation(ff[:], Ya[s_][:], AF.Identity, scale=gAB[:, 0, tc:tc + 1]),
                              reads=['Ya%d' % s_], writes=['ff'])
                        kb.op(V, lambda s_=s_, tc=tc: nc.vector.scalar_tensor_tensor(ff[:], Yc[s_][:], gAB[:, 1, tc:tc + 1], ff[:], ALU.mult, ALU.add),
                              reads=['Yc%d' % s_, 'ff'], writes=['ff'])
                        for hf in range(2):
                            for d4 in range(4):
                                dc = hf * 4 + d4
                                kb.op(PE, lambda dc=dc, d4=d4, hf=hf: nc.tensor.transpose(psF[hf][:, d4, :], ff[:, dc * 128:(dc + 1) * 128], identf[:]),
                                      reads=['ff'], writes=['psF%d' % hf])
                            for d4 in range(4):
                                dc = hf * 4 + d4
                                g2 = modc(l, 1, dc, n)[2]
                                kb.op(V, lambda dc=dc, d4=d4, hf=hf, g2=g2, tc=tc: nc.vector.scalar_tensor_tensor(
                                    xT[:, dc, tc * 128:(tc + 1) * 128], psF[hf][:, d4, :], g2, xT[:, dc, tc * 128:(tc + 1) * 128], ALU.mult, ALU.add),
                                    reads=['psF%d' % hf], writes=[('xTc', dc, tc)])
            kb.barrier()

        def attention(b):
            with ExitStack() as ps:
                modulate(1, 0, b, 5, ps)
            kb.barrier()
            with ExitStack() as ps:
                kT = sb("akT", [128, 2, T], BF16, ps)
                vv = sb("avv", [128, 18, 256], BF16, ps)
                oT = sb("aoT", [128, 4, S], BF16, ps)
                qTs = [sb("aqT%d" % i, [128, 512], BF16, ps) for i in range(2)]
                PT = [sb("aPT%d" % i, [128, 512], BF16, ps) for i in range(2)]
                sq = sb("asq", [128, 512], BF16, ps)
                rt = sb("art", [128, 512], F32, ps)
                R = sb("aR", [128, 512], F32, ps)
                rA = sb("arA", [128, 512], F32, ps)
                rB = sb("arB", [128, 512], F32, ps)
                rD = sb("arD", [128, 512], F32, ps)
                psS = [pst("apsS%d" % i, [128, 512], F32, ps) for i in range(2)]
                psO = pst("apsO", [128, 512], F32, ps)
                psDn = pst("apsDn", [128, 512], F32, ps)
                ps1 = pst("aps1", [128, 512], F32, ps)
                ps2_ = pst("aps2", [128, 512], F32, ps)
                ps3 = pst("aps3", [128, 512], F32, ps)
                psX = pst("apsX", [128, 512], F32, ps)
                wo_ = WA[:, 2:4, :].rearrange("p a (k c) -> p (a k) c", k=4)
                wload(2, wo_, awo_d.rearrange("(hq p) d -> p hq d", p=128), keys=['W2', 'W3'])
                wflat1 = WA[:, 1, :]
                wk_ = wflat1.rearrange("p (g s k c) -> p g s k c", g=2, s=2, k=8)
                for g in range(2):
                    c0 = 1024 + g * 128
                    wload(1, wk_[:, g, 0], aw_d[:, c0:c0 + 128].rearrange("(kc p) c -> p kc c", p=128))
                    wload(1, wk_[:, g, 1], aw_d[:, ATT_IN + c0:ATT_IN + c0 + 128].rearrange("(kc p) c -> p kc c", p=128))
                wv_ = WA[:, 0, 0:2048].rearrange("p (k c) -> p k c", k=8)
                wload(0, wv_, aw_d[:, 1280:1536].rearrange("(kc p) c -> p kc c", p=128))

                def qk_norm_rope(wsl, w4, tab, gcol, dst, dkeys, blk, hkblk):
                    t0, W = blk_rng(blk)
                    for kc in range(8):
                        kb.op(PE, lambda kc=kc: nc.tensor.matmul(ps1[:, :W], w4[:, 0, kc, :], hT[:, kc, t0:t0 + W], start=(kc == 0), stop=(kc == 7)),
                              reads=[wsl] + hks(hkblk), writes=['aps1'])
                    kb.op(A, lambda: nc.scalar.activation(sq[:, :W], ps1[:, :W], AF.Square), reads=['aps1'], writes=['asq'])
                    if blk < 4:
                        for kc in range(8):
                            kb.op(PE, lambda kc=kc: nc.tensor.matmul(ps2_[:, :W], w4[:, 1, kc, :], hT[:, kc, t0:t0 + W], start=(kc == 0), stop=(kc == 7)),
                                  reads=[wsl] + hks(hkblk), writes=['aps2'])
                    kb.op(PE, lambda: nc.tensor.matmul(ps3[:, :W], onesb[:], sq[:, :W], start=True, stop=True), reads=['asq'], writes=['aps3'])
                    kb.op(A, lambda: nc.scalar.activation(rt[:, :W], ps3[:, :W], AF.Sqrt, bias=epsn[:, 0:1], scale=1.0 / 128), reads=['aps3'], writes=['art'])
                    kb.op(V, lambda: nc.vector.reciprocal(R[:, :W], rt[:, :W]), reads=['art'], writes=['aR'])
                    if blk < 4:
                        nr = W // 64
                        r0 = t0 // 64
                        for (src, dstt, ti, pk, dk_) in ((ps1, rA, 0, 'aps1', 'arA'), (ps2_, rB, 1, 'aps2', 'arB')):
                            kb.op(V, lambda src=src, dstt=dstt, ti=ti: nc.vector.tensor_tensor(
                                dstt[0:64, :W].rearrange("p (a b) -> p a b", b=64), src[0:64, :W].rearrange("p (a b) -> p a b", b=64),
                                tab[0:64, ti, r0:r0 + nr].unsqueeze(2).to_broadcast([64, nr, 64]), ALU.mult),
                                reads=[pk], writes=[dk_ + 'lo'])
                            kb.op(V, lambda src=src, dstt=dstt, ti=ti: nc.vector.tensor_tensor(
                                dstt[64:128, :W].rearrange("p (a b) -> p a b", b=64), src[64:128, :W].rearrange("p (a b) -> p a b", b=64),
                                tab[64:128, ti, :].unsqueeze(1).to_broadcast([64, nr, 64]), ALU.mult),
                                reads=[pk], writes=[dk_ + 'hi'])
                        kb.op(P, lambda: nc.gpsimd.tensor_tensor(rA[:, :W], rA[:, :W], rB[:, :W], ALU.add),
                              reads=['arAlo', 'arAhi', 'arBlo', 'arBhi'], writes=['arAlo', 'arAhi'])
                        kb.op(P, lambda: nc.gpsimd.tensor_tensor(dst, rA[:, :W], R[:, :W], ALU.mult),
                              reads=['arAlo', 'arAhi', 'aR'], writes=dkeys)
                    else:
                        kb.op(V, lambda: nc.vector.scalar_tensor_tensor(dst, ps1[:, :W], gcol, R[:, :W], ALU.mult, ALU.mult),
                              reads=['aps1', 'aR'], writes=dkeys)

                def load_wq(hq):
                    qs = (hq + 1) % 2
                    wq_ = WA[:, 0, qs * 2048:(qs + 1) * 2048].rearrange("p (s k c) -> p s k c", s=2, k=8)
                    qkey = 'WQ%d' % qs
                    deps_extra = ['W0'] if qs == 0 else []
                    kb.dma('pool', qkey, wq_[:, 0], aw_d[:, hq * 128:(hq + 1) * 128].rearrange("(kc p) c -> p kc c", p=128),
                           writes=[qkey] + deps_extra)
                    kb.dma('pool', qkey, wq_[:, 1], aw_d[:, ATT_IN + hq * 128:ATT_IN + (hq + 1) * 128].rearrange("(kc p) c -> p kc c", p=128),
                           writes=[qkey] + deps_extra)

                load_wq(0)
                for g in range(2):
                    for blk in range(5):
                        t0, W = blk_rng(blk)
                        qk_norm_rope('W1', wk_[:, g], atk, gk[:, 0:1], kT[:, g, t0:t0 + W], [('akT', g, blk)], blk, blk)
                for tc in range(18):
                    pv, pvk = (psX, 'apsX') if tc % 2 == 0 else (ps3, 'aps3')
                    for kc in range(8):
                        kb.op(PE, lambda kc=kc, tc=tc, pv=pv: nc.tensor.matmul(pv[:, 0:256], hT[:, kc, tc * 128:(tc + 1) * 128], wv_[:, kc, :],
                                                                              start=(kc == 0), stop=(kc == 7)),
                              reads=['W0'] + hks(tc // 4), writes=[pvk])
                    kb.op(A, lambda tc=tc, pv=pv: nc.scalar.copy(vv[:, tc, :], pv[:, 0:256]), reads=[pvk], writes=[('avv', tc)])
                scale = 128.0 ** -0.5
                for half in range(2):
                    items = [(h4, qb) for h4 in range(4) for qb in range(4)]

                    def qproj(i):
                        h4, qb = items[i]
                        hq = half * 4 + h4
                        qs = (hq + 1) % 2
                        wq_ = WA[:, 0, qs * 2048:(qs + 1) * 2048].rearrange("p (s k c) -> p s k c", s=2, k=8)
                        qkey = 'WQ%d' % qs
                        if qb == 0 and hq + 1 < 8:
                            load_wq(hq + 1)
                        qk_norm_rope(qkey, wq_, atq, gq[:, 0:1], qTs[i % 2][:, :], ['aqT%d' % (i % 2)], qb, qb)

                    qproj(0)
                    for i in range(16):
                        h4, qb = items[i]
                        hq = half * 4 + h4
                        g = hq // 4
                        t0, W = blk_rng(qb)
                        qT = qTs[i % 2]
                        qk_ = 'aqT%d' % (i % 2)
                        if i + 1 < 16:
                            qproj(i + 1)
                        def Sm(sc):
                            pS = psS[sc % 2]
                            kb.op(PE, lambda: nc.tensor.matmul(pS[:], kT[:, g, sc * 128:(sc + 1) * 128], qT[:], start=True, stop=True),
                                  reads=[('akT', g, sc // 4), qk_], writes=['apsS%d' % (sc % 2)])
                            pt = PT[sc % 2]
                            kb.op(A, lambda: nc.scalar.activation(pt[:], pS[:], AF.Exp, scale=scale),
                                  reads=['apsS%d' % (sc % 2)], writes=['aPT%d' % (sc % 2)])

                        def PVm(sc):
                            pt = PT[sc % 2]
                            kb.op(PE, lambda: nc.tensor.matmul(psO[:], vv[:, sc, g * 128:(g + 1) * 128], pt[:], start=(sc == 0), stop=(sc == 17)),
                                  reads=['aPT%d' % (sc % 2), ('avv', sc)], writes=['apsO'])
                            kb.op(PE, lambda: nc.tensor.matmul(psDn[:], onesb[:], pt[:], start=(sc == 0), stop=(sc == 17)),
                                  reads=['aPT%d' % (sc % 2)], writes=['apsDn'])

                        Sm(0)
                        for sc in range(18):
                            if sc + 1 < 18:
                                Sm(sc + 1)
                            PVm(sc)
                        kb.op(V, lambda: nc.vector.reciprocal(rD[:], psDn[:]), reads=['apsDn'], writes=['arD'])
                        kb.op(V, lambda h4=h4, t0=t0: nc.vector.tensor_tensor(oT[:, h4, t0:t0 + 512], psO[:], rD[:], ALU.mult),
                              reads=['apsO', 'arD'], writes=[('aoT', h4, qb)])
                    for qb in range(4):
                        t0, W = blk_rng(qb)
                        for dc in range(8):
                            po, pok = (psX, 'apsX') if dc % 2 == 0 else (ps1, 'aps1')
                            for h4 in range(4):
                                kb.op(PE, lambda h4=h4, dc=dc, po=po: nc.tensor.matmul(po[:], wo_[:, half * 4 + h4, dc * 128:(dc + 1) * 128], oT[:, h4, t0:t0 + 512],
                                                                                      start=(h4 == 0), stop=(h4 == 3)),
                                      reads=['W2', 'W3'] + [('aoT', i, qb) for i in range(4)], writes=[pok])
                            g1 = modc(1, 0, dc, b)[2]
                            kb.op(V, lambda dc=dc, g1=g1, po=po: nc.vector.scalar_tensor_tensor(
                                xT[:, dc, t0:t0 + 512], po[:], g1, xT[:, dc, t0:t0 + 512], ALU.mult, ALU.add),
                                reads=[pok, xk(dc, qb)], writes=[xk(dc, qb)])
            kb.barrier()

        def store(b, final_norm, which_ctx=False):
            with ExitStack() as ps:
                sqb = sb("sqb", [128, 8, 512], BF16, ps)
                rt = sb("rt", [128, 512], F32, ps)
                R = sb("R", [128, 512], F32, ps)
                tmp = sb("ftmp", [128, 512], F32, ps)
                of = sb("of", [128, 8, 512], F32, ps)
                ost = [sb("ost%d" % i, [128, D], F32, ps) for i in range(4)]
                pss = pst("pss", [128, 512], F32, ps)
                ptr = [pst("ptr%d" % i, [128, 4, 128], F32, ps) for i in range(4)]
                blks = [4] if which_ctx else [0, 1, 2, 3]
                oi = 0
                for blk in blks:
                    t0, W = blk_rng(blk)
                    if final_norm:
                        rms_block(blk, sqb, pss, rt, R, 'pss')
                        for dc in range(8):
                            kb.op(V, lambda dc=dc: nc.vector.tensor_tensor(tmp[:, :W], xT[:, dc, t0:t0 + W], R[:, :W], ALU.mult),
                                  reads=[xk(dc, blk), 'R'], writes=['ftmp'])
                            kb.op(A, lambda dc=dc: nc.scalar.activation(of[:, dc, :W], tmp[:, :W], AF.Identity, scale=fg[:, dc:dc + 1]),
                                  reads=['ftmp'], writes=[('of', dc)])
                    for ts in range(W // 128):
                        o_ = ost[oi % 4]
                        okey = 'ost%d' % (oi % 4)
                        for hf in range(2):
                            for d4 in range(4):
                                dc = hf * 4 + d4
                                src = of[:, dc, ts * 128:(ts + 1) * 128] if final_norm else xT[:, dc, t0 + ts * 128:t0 + (ts + 1) * 128]
                                rk = [('of', dc)] if final_norm else [xk(dc, blk)]
                                pi = (oi % 2) * 2 + hf
                                kb.op(PE, lambda src=src, pi=pi, d4=d4: nc.tensor.transpose(ptr[pi][:, d4, :], src, identf[:]),
                                      reads=rk, writes=['ptr%d' % pi])
                            pi = (oi % 2) * 2 + hf
                            if hf == 0:
                                kb.op(A, lambda o_=o_, pi=pi: nc.scalar.copy(o_[:, 0:512], ptr[pi][:].rearrange("p a b -> p (a b)")),
                                      reads=['ptr%d' % pi], writes=[okey + 'a'])
                            else:
                                kb.op(V, lambda o_=o_, pi=pi: nc.vector.tensor_copy(o_[:, 512:1024], ptr[pi][:].rearrange("p a b -> p (a b)")),
                                      reads=['ptr%d' % pi], writes=[okey + 'b'])
                        tok0 = t0 + ts * 128
                        if which_ctx:
                            dst = outc_d[b, tok0 - 2048:tok0 - 2048 + 128, :]
                        else:
                            dst = out_d[b, tok0:tok0 + 128, :]
                        kb.dma('sp', 'o%d' % (oi % 4), dst, o_[:], reads=[okey + 'a', okey + 'b'])
                        oi += 1
            kb.barrier()

        for b in range(NB):
            load_x(b)
            stages = [('ret', lambda: retention(b)), ('moe0', lambda: moe(0, b)),
                      ('att', lambda: attention(b)), ('moe1', lambda: moe(1, b))]
            done_all = True
            for name, fn in stages:
                fn()
                if stop == name:
                    done_all = False
                    break
            store(b, final_norm=done_all)
            if dump_ctx:
                store(b, final_norm=False, which_ctx=True)
        kb.finish()
    return nc


def _rope_tables():
    theta = 10000.0
    p = np.arange(128)
    inv = theta ** (-(np.arange(64, dtype=np.float32)) / 64.0)
    invp = inv[p % 64].astype(np.float32)
    sign = np.where(p < 64, -1.0, 1.0).astype(np.float32)
    rows = np.arange(32, dtype=np.float32)
    cols = np.arange(64, dtype=np.float32)
    ar = (rows[None, :] * invp[:, None]).astype(np.float32)
    ac = (cols[None, :] * invp[:, None]).astype(np.float32)
    ret_tab = np.concatenate([np.cos(ar), sign[:, None] * np.sin(ar), np.cos(ac), sign[:, None] * np.sin(ac)], axis=1).astype(np.float32)
    inv2 = theta ** (-(np.arange(32, dtype=np.float32)) / 32.0)
    invp2 = inv2[p % 32].astype(np.float32)
    sign2 = np.where((p % 64) < 32, -1.0, 1.0).astype(np.float32)
    att_tab = np.zeros((128, 2, 64), np.float32)
    a_r = (rows[None, :] * invp2[:, None]).astype(np.float32)
    a_c = (cols[None, :] * invp2[:, None]).astype(np.float32)
    att_tab[:64, 0, :32] = np.cos(a_r)[:64]
    att_tab[:64, 1, :32] = (sign2[:, None] * np.sin(a_r))[:64]
    att_tab[64:, 0, :] = np.cos(a_c)[64:]
    att_tab[64:, 1, :] = (sign2[:, None] * np.sin(a_c))[64:]
    return ret_tab, att_tab


def _shared_inputs(inp):
    f = lambda a: np.ascontiguousarray(np.asarray(a, dtype=np.float32))
    ret_tab, att_tab = _rope_tables()
    sw_ret = np.concatenate([(np.arange(128) + 64) % 128 + c * 128 for c in range(16)])
    rw = f(inp['ret_w_in'])[0]
    rw_ext = np.concatenate([rw, rw[:, :2048][:, sw_ret]], axis=1)
    pa = np.arange(128)
    sw128 = (pa // 64) * 64 + ((pa % 64) + 32) % 64
    sw_att = np.concatenate([sw128 + c * 128 for c in range(10)])
    aw = f(inp['att_w_qkv'])[0]
    aw_ext = np.concatenate([aw, aw[:, :1280][:, sw_att]], axis=1)
    gqv = f(inp['att_q_gain'])[0]
    gkv = f(inp['att_k_gain'])[0]
    sh = {
        'w_mod': f(inp['w_mod']),
        'bmodT': f(f(inp['b_mod']).reshape(2, 48, 128).transpose(2, 0, 1)),
        'ret_w_in': f(rw_ext),
        'ret_w_out': f(inp['ret_w_out'])[0],
        'lgf': f(inp['ret_log_decay_fwd']).reshape(1, 4),
        'lgb': f(inp['ret_log_decay_bwd']).reshape(1, 4),
        'att_w_qkv': f(aw_ext),
        'att_w_o': f(inp['att_w_o'])[0],
        'gq': f(np.stack([gqv, gqv[sw128]], axis=1)),
        'gk': f(np.stack([gkv, gkv[sw128]], axis=1)),
        'w_router': f(np.concatenate([f(inp['moe_w_group']), f(inp['moe_w_expert'])], axis=2)),
        'b_router': f(np.concatenate([f(inp['moe_b_group']), f(inp['moe_b_expert'])], axis=1)),
        'wg_r': f(f(inp['moe_w_gate']).reshape(2, 32, 8, 128, 512).transpose(0, 1, 3, 2, 4).reshape(2 * 32 * 128, 4096)),
        'wu_r': f(f(inp['moe_w_up']).reshape(2, 32, 8, 128, 512).transpose(0, 1, 3, 2, 4).reshape(2 * 32 * 128, 4096)),
        'wd_r': f(f(inp['moe_w_down']).reshape(2, 32, 4, 128, 1024).transpose(0, 1, 3, 2, 4).reshape(2 * 32 * 128, 4096)),
        'fgT': f(f(inp['final_norm_gain']).reshape(8, 128).T),
        'ret_tab': ret_tab,
        'att_tab': att_tab,
        'ident': np.eye(128, dtype=np.float32),
    }
    return sh


def _core_inputs(inp, sh, b0, nb):
    f = lambda a: np.ascontiguousarray(np.asarray(a, dtype=np.float32))
    cc = np.concatenate([f(inp['c'])[b0:b0 + nb], f(inp['c_ctx'])[None, :]], axis=0)
    m = dict(sh)
    m['x'] = f(inp['x'][b0:b0 + nb])
    m['ctx'] = f(inp['ctx'][b0:b0 + nb])
    m['cT'] = f(cc.reshape(nb + 1, 8, 128).transpose(2, 1, 0))
    return m


_PROG = {}


def kernel(**inputs):
    if 'full' not in _PROG:
        _PROG['full'] = build_program(NB_FULL)
    nc = _PROG['full']
    sh = _shared_inputs(inputs)
    in_maps = [_core_inputs(inputs, sh, c * NB_FULL, NB_FULL) for c in range(NCORES)]
    res = run_bass_kernel_spmd(nc, in_maps, core_ids=list(range(NCORES)))
    out = np.concatenate([np.asarray(r["out"], dtype=np.float32) for r in res.results], axis=0)
    return out
```

```python
import numpy as np
from contextlib import ExitStack
import concourse.bass as bass
import concourse.mybir as mybir
from concourse.bass_utils import run_bass_kernel_spmd

F32 = mybir.dt.float32
BF16 = mybir.dt.bfloat16
AF = mybir.ActivationFunctionType
ALU = mybir.AluOpType
AX = mybir.AxisListType

D = 1024
S = 2048
L = 256
T = S + L
NBLK = 5
NCORES = 8
BATCH = 32
NB_FULL = BATCH // NCORES
RET_IN = 6144
ATT_IN = 1536
BIG = 1.0e30


def blk_rng(blk):
    if blk < 4:
        return blk * 512, 512
    return 2048, 256


class KB:
    def __init__(self, nc, es):
        self.nc = nc
        self.eng = {'pe': nc.tensor, 'dve': nc.vector, 'act': nc.scalar, 'pool': nc.gpsimd, 'sp': nc.sync}
        self.sem = {k: es.enter_context(nc.semaphore("s_" + k)) for k in ('pe', 'dve', 'act', 'pool')}
        self.cnt = {k: 0 for k in self.sem}
        self.seen = {e: {} for e in self.eng}
        self.lastw = {}
        self.readers = {}
        self.dsem = {}
        self.dcnt = {}
        self.es = es
        self.bar_sem = es.enter_context(nc.semaphore("s_bar"))
        self.bar_cnt = 0
        self.lazy = set()
        self.scr = nc.dram_tensor("bar_scr", [2, 64], F32, kind="Internal").ap()
        self.bsrc = es.enter_context(nc.sbuf_tensor("bar_src", [1, 64], F32))
        self.op('dve', lambda: nc.vector.memset(self.bsrc[:], 0.0), writes=['bar_src'])

    def _semobj(self, key):
        if isinstance(key, tuple):
            return self.dsem[key[1]]
        return self.sem[key]

    _skip = None

    def _wait(self, e, key, val):
        if key == 'pe' and e == 'pe':
            return
        if key == self._skip:
            return
        if self.seen[e].get(key, 0) >= val:
            return
        self.eng[e].wait_ge(self._semobj(key), val)
        self.seen[e][key] = val

    def _deps(self, e, reads, writes):
        for r in reads:
            t = self.lastw.get(r)
            if t is not None:
                self._wait(e, t[0], t[1])
        for w in writes:
            t = self.lastw.get(w)
            if t is not None:
                self._wait(e, t[0], t[1])
            rd = self.readers.get(w)
            if rd:
                for k, v in rd.items():
                    self._wait(e, k, v)

    def _commit(self, tok, reads, writes):
        for w in writes:
            self.lastw[w] = tok
            self.readers[w] = {}
        for r in reads:
            self.readers.setdefault(r, {})[tok[0]] = tok[1]

    def op(self, e, fn, reads=(), writes=()):
        self._deps(e, reads, writes)
        ins = fn()
        self.cnt[e] += 1
        ins.then_inc(self.sem[e], 1)
        self._commit((e, self.cnt[e]), reads, writes)

    def dma(self, q, stream, out, in_, reads=(), writes=(), indirect=None, **kw):
        if stream not in self.dsem:
            self.dsem[stream] = self.es.enter_context(self.nc.semaphore("d_" + str(len(self.dsem))))
            self.dcnt[stream] = 0
        self._skip = ('dma', stream)
        self._deps(q, reads, writes)
        self._skip = None
        self.dcnt[stream] += 16
        if indirect is None:
            ins = self.eng[q].dma_start(out=out, in_=in_, **kw)
        elif indirect[0] == 'out':
            ins = self.eng[q].indirect_dma_start(out=out, out_offset=bass.IndirectOffsetOnAxis(ap=indirect[1], axis=0), in_=in_, in_offset=None)
        else:
            if len(indirect) > 2:
                ins = self.eng[q].indirect_dma_start(out=out, out_offset=None, in_=in_, in_offset=bass.IndirectOffsetOnAxis(ap=indirect[1], axis=0),
                                                     bounds_check=indirect[2], oob_is_err=False)
            else:
                ins = self.eng[q].indirect_dma_start(out=out, out_offset=None, in_=in_, in_offset=bass.IndirectOffsetOnAxis(ap=indirect[1], axis=0))
        ins.then_inc(self.dsem[stream], 16)
        self._commit((('dma', stream), self.dcnt[stream]), reads, writes)

    def barrier(self):
        sp = 'sp'
        for k in self.sem:
            if self.cnt[k]:
                self._wait(sp, k, self.cnt[k])
        for s, c in self.dcnt.items():
            if c and s not in self.lazy:
                self._wait(sp, ('dma', s), c)
        self.bar_cnt += 16
        self.eng[sp].dma_start(out=self.scr[1:2, :], in_=self.bsrc[:]).then_inc(self.bar_sem, 16)
        for e in ('pe', 'dve', 'act', 'pool', 'sp'):
            self.eng[e].wait_ge(self.bar_sem, self.bar_cnt)
            for k in self.sem:
                self.seen[e][k] = self.cnt[k]
            for s, c in self.dcnt.items():
                if s not in self.lazy:
                    self.seen[e][('dma', s)] = c
        self.lastw.clear()
        self.readers.clear()

    def finish(self):
        sp = 'sp'
        for k in self.sem:
            if self.cnt[k]:
                self._wait(sp, k, self.cnt[k])
        for s, c in self.dcnt.items():
            if c:
                self._wait(sp, ('dma', s), c)


def build_program(NB=NB_FULL, stop=None, dump_ctx=False):
    nc = bass.Bass("TRN2", target_bir_lowering=False)
    NM = NB + 1

    def din(name, shape):
        return nc.dram_tensor(name, list(shape), F32, kind="ExternalInput").ap()

    x_d = din("x", [NB, S, D])
    ctx_d = din("ctx", [NB, L, D])
    cT_d = din("cT", [128, 8, NM])
    wmod_d = din("w_mod", [2, D, 6 * D])
    bmod_d = din("bmodT", [128, 2, 48])
    rwin_d = din("ret_w_in", [D, RET_IN + 2048])
    rwout_d = din("ret_w_out", [2048, D])
    lgf_d = din("lgf", [1, 4])
    lgb_d = din("lgb", [1, 4])
    aw_d = din("att_w_qkv", [D, ATT_IN + 1280])
    awo_d = din("att_w_o", [D, D])
    gq_d = din("gq", [128, 2])
    gk_d = din("gk", [128, 2])
    wr_d = din("w_router", [2, D, 36])
    br_d = din("b_router", [2, 36])
    wgr_d = din("wg_r", [2 * 32 * 128, 4096])
    wur_d = din("wu_r", [2 * 32 * 128, 4096])
    wdr_d = din("wd_r", [2 * 32 * 128, 4096])
    NSLOT = 50 * 256
    w16_d = [nc.dram_tensor("w16_%d" % i, [2 * 32 * 128, 4096], BF16, kind="Internal").ap() for i in range(3)]
    dec_d = nc.dram_tensor("dec_scr", [4, 128, 6144], BF16, kind="Internal").ap()
    Xs_d = nc.dram_tensor("Xs_scr", [NSLOT, D], BF16, kind="Internal").ap()
    Ys_d = nc.dram_tensor("Ys_scr", [NSLOT, D], F32, kind="Internal").ap()
    fg_d = din("fgT", [128, 8])
    rtab_d = din("ret_tab", [128, 192])
    atab_d = din("att_tab", [128, 2, 64])
    ident_d = din("ident", [128, 128])
    out_d = nc.dram_tensor("out", [NB, S, D], F32, kind="ExternalOutput").ap()
    if dump_ctx:
        outc_d = nc.dram_tensor("outc", [NB, L, D], F32, kind="ExternalOutput").ap()

    with ExitStack() as es:
        kb = KB(nc, es)
        bcw = nc.gpsimd.to_reg(2 * 32 * 128 - 1)
        for l_ in range(2):
            kb.lazy.add('pc%d' % l_)
            for r in range(32):
                r0 = l_ * 4096 + r * 128
                for i_, src_ in enumerate((wgr_d, wur_d, wdr_d)):
                    kb.dma('pool', 'pc%d' % l_, w16_d[i_][r0:r0 + 128, :], src_[r0:r0 + 128, :])
        V, A, P, PE = 'dve', 'act', 'pool', 'pe'

        uid = [0]

        def sb(name, shape, dt, st=es):
            uid[0] += 1
            return st.enter_context(nc.sbuf_tensor("%s_%d" % (name, uid[0]), list(shape), dt))

        def pst(name, shape, dt, st):
            uid[0] += 1
            return st.enter_context(nc.psum_tensor("%s_%d" % (name, uid[0]), list(shape), dt))

        xT = sb("xT", [128, 8, T], F32)
        hT = sb("hT", [128, 8, T], BF16)
        WA = sb("WA", [128, 4, 4096], BF16)
        identf = sb("identf", [128, 128], F32)
        identb = sb("identb", [128, 128], BF16)
        onesb = sb("onesb", [128, 128], BF16)
        mod = sb("mod", [128, 2, 48, NM], F32)
        bmod = sb("bmod", [128, 2, 48], F32)
        rtab = sb("rtab", [128, 192], F32)
        atab = sb("atab", [128, 2, 64], F32)
        atq = sb("atq", [128, 2, 64], F32)
        atk = sb("atk", [128, 2, 64], F32)
        gq = sb("gq_s", [128, 2], F32)
        gk = sb("gk_s", [128, 2], F32)
        lgf = sb("lgf_s", [128, 4], F32)
        lgb = sb("lgb_s", [128, 4], F32)
        nlgb = sb("nlgb_s", [128, 4], F32)
        fg = sb("fg_s", [128, 8], F32)
        triU = sb("triU", [128, 128], F32)
        onesf = sb("onesf", [128, 128], F32)
        UI = sb("UI", [32, 32], F32)
        thr18 = sb("thr18", [32, 18], F32)
        thr68 = sb("thr68", [128, 68], F32)
        iop = sb("iop", [128, 1], F32)
        epsn = sb("epsn", [128, 1], F32)
        epsh = sb("epsh", [128, 1], F32)

        def xk(dc, blk):
            return ('xT', dc, blk)

        def hk(dc, blk):
            return ('hT', dc, blk)

        def xks(blk):
            return [xk(dc, blk) for dc in range(8)]

        def hks(blk):
            return [hk(dc, blk) for dc in range(8)]

        with ExitStack() as ps:
            scT = sb("scT", [128, 8, NM], F32, ps)
            wst = [sb("wst%d" % i, [128, 8, 512], F32, ps) for i in range(2)]
            pm = [pst("pm%d" % i, [128, 4, NM], F32, ps) for i in range(2)]
            kb.dma('sp', 'c0', identf[:], ident_d[:], writes=['identf'])
            kb.dma('sp', 'c1', scT[:], cT_d[:], writes=['scT'])
            kb.dma('sp', 'c2', bmod[:], bmod_d[:], writes=['bmod'])
            kb.dma('sp', 'c3', rtab[:], rtab_d[:], writes=['rtab'])
            kb.dma('sp', 'c4', atab[:], atab_d[:], writes=['atab'])
            kb.dma('sp', 'c5', gq[:], gq_d[:], writes=['gq'])
            kb.dma('sp', 'c6', gk[:], gk_d[:], writes=['gk'])
            kb.dma('sp', 'c7', lgf[:], lgf_d.partition_broadcast(128), writes=['lgf'])
            kb.dma('sp', 'c8', lgb[:], lgb_d.partition_broadcast(128), writes=['lgb'])
            kb.dma('sp', 'c9', fg[:], fg_d[:], writes=['fg'])
            kb.op(V, lambda: nc.vector.tensor_copy(identb[:], identf[:]), reads=['identf'], writes=['identb'])
            kb.op(V, lambda: nc.vector.memset(onesb[:], 1.0), writes=['onesb'])
            kb.op(V, lambda: nc.vector.memset(epsn[:], 1e-6), writes=['epsn'])
            kb.op(V, lambda: nc.vector.memset(onesf[:], 1.0), writes=['onesf'])
            kb.op(P, lambda: nc.gpsimd.iota(triU[:], [[1, 128]], base=0, channel_multiplier=-1, allow_small_or_imprecise_dtypes=True), writes=['triU'])
            kb.op(V, lambda: nc.vector.tensor_scalar(UI[:], triU[0:32, 0:32], 0.0, None, ALU.is_ge), reads=['triU'], writes=['UI'])
            kb.op(V, lambda: nc.vector.tensor_scalar(triU[:], triU[:], 0.0, None, ALU.is_gt), reads=['triU', 'UI'], writes=['triU'])
            kb.op(P, lambda: nc.gpsimd.iota(thr18[:], [[256, 18]], base=0, channel_multiplier=0, allow_small_or_imprecise_dtypes=True), writes=['thr18'])
            kb.op(P, lambda: nc.gpsimd.iota(thr68[:], [[256, 68]], base=0, channel_multiplier=0, allow_small_or_imprecise_dtypes=True), writes=['thr68'])
            kb.op(P, lambda: nc.gpsimd.iota(iop[:], [[0, 1]], base=0, channel_multiplier=1, allow_small_or_imprecise_dtypes=True), writes=['iop'])
            kb.op(V, lambda: nc.vector.memset(epsh[:], 1e-5), writes=['epsh'])
            kb.op(V, lambda: nc.vector.tensor_scalar(nlgb[:], lgb[:], -1.0, None, ALU.mult), reads=['lgb'], writes=['nlgb'])
            for tabo, g_, nm in ((atq, gq, 'atq'), (atk, gk, 'atk')):
                kb.op(V, lambda tabo=tabo, g_=g_: nc.vector.tensor_scalar(tabo[:, 0, :], atab[:, 0, :], g_[:, 0:1], None, ALU.mult),
                      reads=['atab', 'gq', 'gk'], writes=[nm + '0'])
                kb.op(V, lambda tabo=tabo, g_=g_: nc.vector.tensor_scalar(tabo[:, 1, :], atab[:, 1, :], g_[:, 1:2], None, ALU.mult),
                      reads=['atab', 'gq', 'gk'], writes=[nm + '1'])
            kb.op(A, lambda: nc.scalar.activation(scT[:], scT[:], AF.Silu), reads=['scT'], writes=['scT'])
            zt = sb("zt", [128, D], BF16, ps)
            kb.op(V, lambda: nc.vector.memset(zt[:], 0.0), writes=['zt'])
            for r in range(NSLOT // 128):
                kb.dma('sp', 'xz', Xs_d[r * 128:(r + 1) * 128, :], zt[:], reads=['zt'])
            strip = sb("strip_s", [128, 3968], BF16, ps)
            cstrip = sb("cstrip_s", [128, 2176], BF16, ps)
            it0 = sb("it0", [128, 496], F32, ps)
            it1 = sb("it1", [128, 496], F32, ps)
            it2 = sb("it2", [128, 496], F32, ps)
            for h in range(4):
                for pcs in range(8):
                    u0 = pcs * 496
                    kb.op(P, lambda u0=u0: nc.gpsimd.iota(it0[:, :496], [[1, 496]], base=u0 - 1920, channel_multiplier=-1,
                                                          allow_small_or_imprecise_dtypes=True), writes=['it0'])
                    kb.op(V, lambda: nc.vector.tensor_scalar(it1[:, :496], it0[:, :496], 0.0, None, ALU.max), reads=['it0'], writes=['it1'])
                    kb.op(V, lambda: nc.vector.tensor_scalar(it2[:, :496], it0[:, :496], 0.0, None, ALU.min), reads=['it0'], writes=['it2'])
                    kb.op(A, lambda h=h: nc.scalar.activation(it1[:, :496], it1[:, :496], AF.Exp, scale=lgf[:, h:h + 1]), reads=['it1', 'lgf'], writes=['it1'])
                    kb.op(A, lambda h=h: nc.scalar.activation(it2[:, :496], it2[:, :496], AF.Exp, scale=nlgb[:, h:h + 1]), reads=['it2', 'nlgb'], writes=['it2'])
                    kb.op(V, lambda: nc.vector.tensor_tensor(it1[:, :496], it1[:, :496], it2[:, :496], ALU.add), reads=['it1', 'it2'], writes=['it1'])
                    kb.op(V, lambda: nc.vector.tensor_scalar(it2[:, :496], it0[:, :496], 0.0, None, ALU.is_equal), reads=['it0', 'it1'], writes=['it2'])
                    kb.op(V, lambda: nc.vector.tensor_tensor(it1[:, :496], it1[:, :496], it2[:, :496], ALU.add), reads=['it2', 'it1'], writes=['it1'])
                    kb.op(V, lambda u0=u0: nc.vector.tensor_scalar(strip[:, u0:u0 + 496], it1[:, :496], -1.0, 1.0 / 16, ALU.add, ALU.mult),
                          reads=['it1'], writes=['strip'])
                for pcs in range(8):
                    u0 = pcs * 272
                    kb.op(P, lambda u0=u0: nc.gpsimd.iota(it0[:, :272], [[1, 272]], base=u0 + 128, channel_multiplier=-1,
                                                          allow_small_or_imprecise_dtypes=True), writes=['it0'])
                    kb.op(P, lambda u0=u0: nc.gpsimd.iota(it2[:, :272], [[-1, 272]], base=2176 - u0, channel_multiplier=1,
                                                          allow_small_or_imprecise_dtypes=True), writes=['it2'])
                    kb.op(A, lambda h=h: nc.scalar.activation(it0[:, :272], it0[:, :272], AF.Exp, scale=lgf[:, h:h + 1]), reads=['it0', 'lgf'], writes=['it0'])
                    kb.op(A, lambda h=h: nc.scalar.activation(it2[:, :272], it2[:, :272], AF.Exp, scale=lgb[:, h:h + 1]), reads=['it2', 'lgb'], writes=['it2'])
                    kb.op(V, lambda: nc.vector.tensor_tensor(it0[:, :272], it0[:, :272], it2[:, :272], ALU.add), reads=['it0', 'it2'], writes=['it0'])
                    kb.op(V, lambda u0=u0: nc.vector.tensor_scalar(cstrip[:, u0:u0 + 272], it0[:, :272], 1.0 / 16, None, ALU.mult),
                          reads=['it0'], writes=['cstrip'])
                kb.dma('sp', 'dec0', dec_d[h, :, 0:3968], strip[:], reads=['strip'])
                kb.dma('sp', 'dec1', dec_d[h, :, 3968:6144], cstrip[:], reads=['cstrip'])
            pc = 0
            for l in range(2):
                for piece in range(12):
                    s_ = pc % 2
                    kb.dma('sp', 'wst%d' % s_, wst[s_][:],
                           wmod_d[l, :, piece * 512:(piece + 1) * 512].rearrange("(kc p) c -> p kc c", p=128),
                           writes=['wst%d' % s_])
                    for f4 in range(4):
                        for kc in range(8):
                            kb.op(PE, lambda f4=f4, kc=kc, s_=s_: nc.tensor.matmul(
                                pm[s_][:, f4, :], wst[s_][:, kc, f4 * 128:(f4 + 1) * 128], scT[:, kc, :],
                                start=(kc == 0), stop=(kc == 7)),
                                reads=['wst%d' % s_, 'scT'], writes=['pm%d' % s_])
                    for f4 in range(4):
                        fc = piece * 4 + f4
                        is_sc = (fc // 8) in (1, 4)
                        kb.op(V, lambda f4=f4, fc=fc, s_=s_, l=l, is_sc=is_sc: nc.vector.tensor_scalar(
                            mod[:, l, fc, :], pm[s_][:, f4, :], bmod[:, l, fc:fc + 1], 1.0 if is_sc else 0.0,
                            ALU.add, ALU.add),
                            reads=['pm%d' % s_, 'bmod'], writes=[('mod', l, fc)])
                    pc += 1
        kb.barrier()

        def modc(l, which, dc, n):
            base = which * 24
            return (mod[:, l, base + dc, n:n + 1], mod[:, l, base + 8 + dc, n:n + 1], mod[:, l, base + 16 + dc, n:n + 1])

        def rms_block(blk, sqb, pss, rt, R, pskey):
            t0, W = blk_rng(blk)
            for dc in range(8):
                kb.op(A, lambda dc=dc: nc.scalar.activation(sqb[:, dc, :W], xT[:, dc, t0:t0 + W], AF.Square),
                      reads=[xk(dc, blk)], writes=[('sqb', dc)])
            for dc in range(8):
                kb.op(PE, lambda dc=dc: nc.tensor.matmul(pss[:, :W], onesb[:], sqb[:, dc, :W], start=(dc == 0), stop=(dc == 7)),
                      reads=[('sqb', dc)], writes=[pskey])
            kb.op(A, lambda: nc.scalar.activation(rt[:, :W], pss[:, :W], AF.Sqrt, bias=epsn[:, 0:1], scale=1.0 / D),
                  reads=[pskey], writes=['rt'])
            kb.op(V, lambda: nc.vector.reciprocal(R[:, :W], rt[:, :W]), reads=['rt'], writes=['R'])

        def load_x(b):
            with ExitStack() as ps:
                stg = [sb("xstg%d" % i, [128, D], F32, ps) for i in range(4)]
                ptr = [pst("ptr%d" % i, [128, 4, 128], F32, ps) for i in range(4)]
                for tc in range(18):
                    s_ = tc % 4
                    src = x_d[b, tc * 128:(tc + 1) * 128, :] if tc < 16 else ctx_d[b, (tc - 16) * 128:(tc - 15) * 128, :]
                    kb.dma('sp', 'xstg%d' % s_, stg[s_][:], src, writes=['xstg%d' % s_])
                    blk = tc // 4
                    for hf in range(2):
                        pi = (tc % 2) * 2 + hf
                        for d4 in range(4):
                            dc = hf * 4 + d4
                            kb.op(PE, lambda dc=dc, d4=d4, pi=pi, s_=s_: nc.tensor.transpose(
                                ptr[pi][:, d4, :], stg[s_][:, dc * 128:(dc + 1) * 128], identf[:]),
                                reads=['xstg%d' % s_], writes=['ptr%d' % pi])
                        e_ = A if hf == 0 else V
                        f_ = (lambda pi=pi, hf=hf, tc=tc: nc.scalar.copy(xT[:, hf * 4:(hf + 1) * 4, tc * 128:(tc + 1) * 128], ptr[pi][:])) if hf == 0 else \
                             (lambda pi=pi, hf=hf, tc=tc: nc.vector.tensor_copy(xT[:, hf * 4:(hf + 1) * 4, tc * 128:(tc + 1) * 128], ptr[pi][:]))
                        kb.op(e_, f_, reads=['ptr%d' % pi], writes=[('xTl', hf * 4 + d4, tc) for d4 in range(4)])
            kb.barrier()

        def modulate(l, which, b, nblk, ps, router=None):
            sqb = sb("sqb", [128, 8, 512], BF16, ps)
            rt = sb("rt", [128, 512], F32, ps)
            R = sb("R", [128, 512], F32, ps)
            tmp = [sb("mtmp%d" % i, [128, 512], F32, ps) for i in range(2)]
            pss = pst("pss", [128, 512], F32, ps)
            for blk in range(nblk):
                t0, W = blk_rng(blk)
                n = b if blk < 4 else NB
                rms_block(blk, sqb, pss, rt, R, 'pss')
                for dc in range(8):
                    sh, sc, _ = modc(l, which, dc, n)
                    tm = tmp[dc % 2]
                    kb.op(V, lambda dc=dc, tm=tm: nc.vector.tensor_tensor(tm[:, :W], xT[:, dc, t0:t0 + W], R[:, :W], ALU.mult),
                          reads=[xk(dc, blk), 'R'], writes=['mtmp%d' % (dc % 2)])
                    if router is None:
                        kb.op(A, lambda dc=dc, tm=tm, sh=sh, sc=sc: nc.scalar.activation(
                            hT[:, dc, t0:t0 + W], tm[:, :W], AF.Identity, bias=sh, scale=sc),
                            reads=['mtmp%d' % (dc % 2)], writes=[hk(dc, blk)])
                    else:
                        h2f = router['h2f']
                        kb.op(A, lambda dc=dc, tm=tm, sh=sh, sc=sc: nc.scalar.activation(
                            h2f[:, dc, :W], tm[:, :W], AF.Identity, bias=sh, scale=sc),
                            reads=['mtmp%d' % (dc % 2)], writes=[('h2f', dc)])
                        kb.op(P, lambda dc=dc: nc.gpsimd.tensor_copy(hT[:, dc, t0:t0 + W], h2f[:, dc, :W]),
                              reads=[('h2f', dc)], writes=[hk(dc, blk)])
                if router is not None:
                    router['fn'](blk)

        wslot_rr = [0]

        def wload(slot, dst_ap, src_ap, keys=None):
            kb.dma('pool', 'W%d' % slot, dst_ap, src_ap, writes=keys or ['W%d' % slot])

        def retention(b):
            with ExitStack() as ps:
                modulate(0, 0, b, 5, ps)
            kb.barrier()
            with ExitStack() as ps:
                kT = sb("kT", [128, 2, T], BF16, ps)
                qT = sb("qT", [128, 2, 512], BF16, ps)
                vh = sb("vh", [128, 18, 512], BF16, ps)
                strip = sb("strip", [128, 3968], BF16, ps)
                cstrip = sb("cstrip", [128, 2176], BF16, ps)
                PT = [sb("PT%d" % i, [128, 512], BF16, ps) for i in range(2)]
                rA = sb("rA", [128, 512], F32, ps)
                rB = sb("rB", [128, 512], F32, ps)
                zb = [sb("zb%d" % i, [128, 512], BF16, ps) for i in range(2)]
                zT = sb("zT", [128, 4, 512], BF16, ps)
                dg = sb("dg", [128, 4, 128], BF16, ps)
                st6 = sb("st6", [128, 4, 6], F32, ps)
                mv = sb("mv", [128, 4, 2], F32, ps)
                rs = sb("rs", [128, 4], F32, ps)
                psS = [pst("psS%d" % i, [128, 512], F32, ps) for i in range(2)]
                psY = [pst("psY%d" % i, [128, 512], F32, ps) for i in range(4)]
                psM = pst("psM", [128, 512], F32, ps)
                psTb = pst("psTb", [128, 4, 256], BF16, ps)
                psM2 = psTb[:].rearrange("p a b -> p (a b)").bitcast(F32)

                def wslot():
                    s_ = wslot_rr[0] % 4
                    wslot_rr[0] += 1
                    return s_

                c_row, s_row = rtab[:, 0:32], rtab[:, 32:64]
                c_col, s_col = rtab[:, 64:128], rtab[:, 128:192]

                def rope_tabs(j, t0, W):
                    nr = W // 64
                    if j == 0:
                        r0 = t0 // 64
                        return (c_row[:, r0:r0 + nr].unsqueeze(2).to_broadcast([128, nr, 64]),
                                s_row[:, r0:r0 + nr].unsqueeze(2).to_broadcast([128, nr, 64]))
                    return (c_col.unsqueeze(1).to_broadcast([128, nr, 64]),
                            s_col.unsqueeze(1).to_broadcast([128, nr, 64]))

                def v3(ap, W):
                    return ap.rearrange("p (a b) -> p a b", b=64)

                def proj_rope(ws, dstT, dkey, blk, alt=0):
                    t0, W = blk_rng(blk)
                    wv_ = WA[:, ws, :].rearrange("p (s k c) -> p s k c", s=2, k=8)
                    for j in range(2):
                        if alt and j == 1:
                            pA, pAk, pB, pBk = psS[0], 'psS0', psS[1], 'psS1'
                        else:
                            pA, pAk, pB, pBk = psM, 'psM', psM2, 'psTb'
                        for kc in range(8):
                            kb.op(PE, lambda kc=kc, j=j: nc.tensor.matmul(pA[:, :W], wv_[:, 0, kc, j * 128:(j + 1) * 128], hT[:, kc, t0:t0 + W],
                                                                         start=(kc == 0), stop=(kc == 7)),
                                  reads=['W%d' % ws] + hks(blk), writes=[pAk])
                        if blk < 4:
                            for kc in range(8):
                                kb.op(PE, lambda kc=kc, j=j: nc.tensor.matmul(pB[:, :W], wv_[:, 1, kc, j * 128:(j + 1) * 128], hT[:, kc, t0:t0 + W],
                                                                             start=(kc == 0), stop=(kc == 7)),
                                      reads=['W%d' % ws] + hks(blk), writes=[pBk])
                            ct, st_ = rope_tabs(j, t0, W)
                            kb.op(V, lambda ct=ct: nc.vector.tensor_tensor(v3(rA[:, :W], W), v3(pA[:, :W], W), ct, ALU.mult),
                                  reads=[pAk], writes=['rA'])
                            kb.op(V, lambda st_=st_: nc.vector.tensor_tensor(v3(rB[:, :W], W), v3(pB[:, :W], W), st_, ALU.mult),
                                  reads=[pBk], writes=['rB'])
                            kb.op(P, lambda j=j: nc.gpsimd.tensor_tensor(dstT[:, j, t0 - dkey[1]:t0 - dkey[1] + W], rA[:, :W], rB[:, :W], ALU.add),
                                  reads=['rA', 'rB'], writes=[(dkey[0], j, blk)])
                        else:
                            kb.op(A, lambda j=j: nc.scalar.copy(dstT[:, j, t0 - dkey[1]:t0 - dkey[1] + W], pA[:, :W]),
                                  reads=[pAk], writes=[(dkey[0], j, blk)])

                for h in range(4):
                    kb.dma('sp', 'dec0', strip[:], dec_d[h, :, 0:3968], writes=['strip'])
                    kb.dma('sp', 'dec1', cstrip[:], dec_d[h, :, 3968:6144], writes=['cstrip'])
                    ws = wslot()
                    wdst = WA[:, ws, :].rearrange("p (s k c) -> p s k c", s=2, k=8)
                    c0 = 1024 + h * 256
                    wload(ws, wdst[:, 0], rwin_d[:, c0:c0 + 256].rearrange("(kc p) c -> p kc c", p=128))
                    wload(ws, wdst[:, 1], rwin_d[:, RET_IN + c0:RET_IN + c0 + 256].rearrange("(kc p) c -> p kc c", p=128))
                    for blk in range(5):
                        proj_rope(ws, kT, ('kT', 0), blk, alt=1)
                    ws = wslot()
                    wv_ = WA[:, ws, :].rearrange("p (k c) -> p k c", k=8)
                    c0 = 2048 + h * 512
                    wload(ws, wv_, rwin_d[:, c0:c0 + 512].rearrange("(kc p) c -> p kc c", p=128))
                    for tc in range(18):
                        blk = tc // 4
                        pv, pvk = (psM, 'psM') if tc % 2 == 0 else (psM2, 'psTb')
                        for kc in range(8):
                            kb.op(PE, lambda kc=kc, tc=tc, pv=pv: nc.tensor.matmul(pv[:], hT[:, kc, tc * 128:(tc + 1) * 128], wv_[:, kc, :],
                                                                                  start=(kc == 0), stop=(kc == 7)),
                                  reads=['W%d' % ws] + hks(blk), writes=[pvk])
                        if tc % 2 == 0:
                            kb.op(A, lambda tc=tc, pv=pv: nc.scalar.copy(vh[:, tc, :], pv[:]), reads=[pvk], writes=[('vh', tc)])
                        else:
                            kb.op(V, lambda tc=tc, pv=pv: nc.vector.tensor_copy(vh[:, tc, :], pv[:]), reads=[pvk], writes=[('vh', tc)])
                    wsq = wslot()
                    wdq = WA[:, wsq, :].rearrange("p (s k c) -> p s k c", s=2, k=8)
                    c0 = h * 256
                    wload(wsq, wdq[:, 0], rwin_d[:, c0:c0 + 256].rearrange("(kc p) c -> p kc c", p=128))
                    wload(wsq, wdq[:, 1], rwin_d[:, RET_IN + c0:RET_IN + c0 + 256].rearrange("(kc p) c -> p kc c", p=128))
                    wsg = wslot()
                    wg_ = WA[:, wsg, :].rearrange("p (k c) -> p k c", k=8)
                    c0 = 4096 + h * 512
                    wload(wsg, wg_, rwin_d[:, c0:c0 + 512].rearrange("(kc p) c -> p kc c", p=128))
                    wso = wslot()
                    wo_ = WA[:, wso, :].rearrange("p (k c) -> p k c", k=4)
                    wload(wso, wo_, rwout_d[h * 512:(h + 1) * 512, :].rearrange("(hc p) d -> p hc d", p=128))
                    for qb in range(5):
                        t0, W = blk_rng(qb)
                        nts = W // 128
                        proj_rope(wsq, qT, ('qT', t0), qb)
                        scs = list(range(18)) if qb < 4 else [16, 17]
                        def Sm(si):
                            sc = scs[si]
                            pS = psS[si % 2]
                            for j in range(2):
                                kb.op(PE, lambda j=j: nc.tensor.matmul(pS[:, :W], kT[:, j, sc * 128:(sc + 1) * 128], qT[:, j, :W],
                                                                      start=(j == 0), stop=(j == 1)),
                                      reads=[('kT', j, sc // 4), ('qT', 0, qb), ('qT', 1, qb)], writes=['psS%d' % (si % 2)])
                            if sc < 16:
                                off = t0 - sc * 128 + 1920
                                dk, dkey = strip[:, off:off + W], 'strip'
                            elif qb < 4:
                                off = t0 - 128 * (sc - 16) + 128
                                dk, dkey = cstrip[:, off:off + W], 'cstrip'
                            else:
                                off = 1920 - 128 * (sc - 16)
                                dk, dkey = strip[:, off:off + W], 'strip'
                            pt = PT[si % 2]
                            kb.op(V, lambda: nc.vector.tensor_tensor(pt[:, :W], pS[:, :W], dk, ALU.mult),
                                  reads=['psS%d' % (si % 2), dkey], writes=['PT%d' % (si % 2)])

                        def PVm(si):
                            sc = scs[si]
                            pt = PT[si % 2]
                            for ts in range(nts):
                                kb.op(PE, lambda ts=ts: nc.tensor.matmul(
                                    psY[ts][:], pt[:, ts * 128:(ts + 1) * 128], vh[:, sc, :], start=(si == 0), stop=(si == len(scs) - 1)),
                                    reads=['PT%d' % (si % 2), ('vh', sc)], writes=['psY%d' % ts])

                        Sm(0)
                        for si in range(len(scs)):
                            if si + 1 < len(scs):
                                Sm(si + 1)
                            PVm(si)
                        n = b if qb < 4 else NB
                        sgs = [rA, rB]
                        for ts in range(nts):
                            kb.op(V, lambda ts=ts: nc.vector.bn_stats(st6[:, ts, :], psY[ts][:]), reads=['psY%d' % ts], writes=[('st6', ts)])
                            kb.op(V, lambda ts=ts: nc.vector.bn_aggr(mv[:, ts, :], st6[:, ts, :]), reads=[('st6', ts)], writes=[('mv', ts)])
                        kb.op(A, lambda: nc.scalar.activation(rs[:, :nts], mv[:, :nts, 1], AF.Sqrt, bias=epsh[:, 0:1], scale=1.0),
                              reads=[('mv', i) for i in range(nts)], writes=['rs'])
                        kb.op(V, lambda: nc.vector.reciprocal(rs[:, :nts], rs[:, :nts]), reads=['rs'], writes=['rs'])
                        for ts in range(nts):
                            kb.op(P, lambda ts=ts: nc.gpsimd.tensor_scalar(dg[:, ts, :], identb[:], rs[:, ts:ts + 1], None, ALU.mult),
                                  reads=['rs'], writes=[('dg', ts)])

                        def gproj(ts):
                            tk0 = t0 + ts * 128
                            pg = psS[ts % 2]
                            for kc in range(8):
                                kb.op(PE, lambda kc=kc: nc.tensor.matmul(pg[:], hT[:, kc, tk0:tk0 + 128], wg_[:, kc, :], start=(kc == 0), stop=(kc == 7)),
                                      reads=['W%d' % wsg] + hks(qb), writes=['psS%d' % (ts % 2)])
                            sg_ = sgs[ts % 2]
                            sk = 'rA' if ts % 2 == 0 else 'rB'
                            zb_ = zb[ts % 2]
                            kb.op(A, lambda: nc.scalar.activation(sg_[:], pg[:], AF.Silu), reads=['psS%d' % (ts % 2)], writes=[sk])
                            kb.op(V, lambda: nc.vector.scalar_tensor_tensor(zb_[:], psY[ts][:], mv[:, ts, 0:1], sg_[:], ALU.subtract, ALU.mult),
                                  reads=['psY%d' % ts, ('mv', ts), sk], writes=['zb%d' % (ts % 2)])

                        def ztrans(ts):
                            zb_ = zb[ts % 2]
                            pz = psM2.rearrange("p (a b) -> p a b", b=128)
                            for hc in range(4):
                                kb.op(PE, lambda hc=hc: nc.tensor.matmul(pz[:, hc, :], zb_[:, hc * 128:(hc + 1) * 128], dg[:, ts, :], start=True, stop=True),
                                      reads=['zb%d' % (ts % 2), ('dg', ts)], writes=['psTb'])
                            kb.op(A, lambda: nc.scalar.copy(zT[:, :, ts * 128:(ts + 1) * 128], pz[:]), reads=['psTb'], writes=['zT'])

                        gproj(0)
                        for ts in range(nts):
                            if ts + 1 < nts:
                                gproj(ts + 1)
                            ztrans(ts)
                        for dc in range(8):
                            po, pok = (psM, 'psM') if dc % 2 == 0 else (psM2, 'psTb')
                            for hc in range(4):
                                kb.op(PE, lambda hc=hc, dc=dc, po=po: nc.tensor.matmul(po[:, :W], wo_[:, hc, dc * 128:(dc + 1) * 128], zT[:, hc, :W],
                                                                                      start=(hc == 0), stop=(hc == 3)),
                                      reads=['W%d' % wso, 'zT'], writes=[pok])
                            g1 = modc(0, 0, dc, n)[2]
                            kb.op(V, lambda dc=dc, g1=g1, po=po: nc.vector.scalar_tensor_tensor(
                                xT[:, dc, t0:t0 + W], po[:, :W], g1, xT[:, dc, t0:t0 + W], ALU.mult, ALU.add),
                                reads=[pok, xk(dc, qb)], writes=[xk(dc, qb)])
            kb.barrier()

        def moe(l, b):
            nblk = 5 if l == 0 else 4
            nch = 18 if l == 0 else 16
            NBK = nch + 32
            with ExitStack() as ps:
                wr = sb("wr", [128, 8, 36], F32, ps)
                br = sb("br", [128, 36], F32, ps)
                gwall = sb("gwall", [128, 18, 32], F32, ps)
                Mall = sb("Mall", [128, 18, 32], F32, ps)
                Call = sb("Call", [128, 18, 32], F32, ps)
                Macc = sb("Macc", [128, 32], F32, ps)
                base_rep = sb("base_rep", [128, 32], F32, ps)
                idxw = sb("idxw", [128, 68], mybir.dt.int32, ps)
                dA = sb("dA", [128, 18], mybir.dt.int32, ps)
                dB = sb("dB", [128, 18], mybir.dt.int32, ps)
                gAB = sb("gAB", [128, 2, 18], F32, ps)
                kb.dma('sp', 'c0', wr[:], wr_d[l].rearrange("(kc p) n -> p kc n", p=128), writes=['wr'])
                kb.dma('sp', 'c1', br[:], br_d[l:l + 1, :].partition_broadcast(128), writes=['br'])
                kb.op(V, lambda: nc.vector.memset(Macc[:], 0.0), writes=['Macc'])
                with ExitStack() as ps2:
                    h2f = sb("h2f", [128, 8, 512], F32, ps2)
                    Lg = sb("Lg", [128, 4, 36], F32, ps2)
                    EM = sb("EM", [128, 4, 32], F32, ps2)
                    EM2 = sb("EM2", [128, 4, 32], F32, ps2)
                    pe_ = sb("pe_", [128, 4, 32], F32, ps2)
                    gx = sb("gx", [128, 4, 4], F32, ps2)
                    gm = sb("gm", [128, 4, 4], F32, ps2)
                    c4 = sb("c4", [128, 6, 4], F32, ps2)
                    msum = sb("msum", [128, 32], F32, ps2)
                    psL = pst("psL", [128, 4, 36], F32, ps2)
                    psC = pst("psC", [128, 4, 32], F32, ps2)

                    def router_fn(blk):
                        t0, W = blk_rng(blk)
                        nts = W // 128
                        tc0 = t0 // 128
                        gw4 = gwall[:, tc0:tc0 + nts, :]
                        M4 = Mall[:, tc0:tc0 + nts, :]
                        L4 = Lg[:, :nts, :]
                        G4 = L4[:, :, 0:4]
                        E4 = EM[:, :nts, :]
                        E24 = EM2[:, :nts, :]
                        P4 = pe_[:, :nts, :]

                        def bc(col, k):
                            return col.unsqueeze(2).to_broadcast([128, nts, k])

                        for ts in range(nts):
                            for kc in range(8):
                                kb.op(PE, lambda kc=kc, ts=ts: nc.tensor.matmul(psL[:, ts, :], h2f[:, kc, ts * 128:(ts + 1) * 128], wr[:, kc, :],
                                                                               start=(kc == 0), stop=(kc == 7)),
                                      reads=['wr'] + [('h2f', dc) for dc in range(8)], writes=['psL'])
                        kb.op(V, lambda: nc.vector.tensor_tensor(L4, psL[:, :nts, :], br[:].unsqueeze(1).to_broadcast([128, nts, 36]), ALU.add),
                              reads=['psL', 'br'], writes=['Lg'])
                        kb.op(V, lambda: nc.vector.reduce_max(c4[:, 0, :nts], G4, AX.X), reads=['Lg'], writes=['c4a'])
                        kb.op(V, lambda: nc.vector.tensor_tensor(gx[:, :nts, :], G4, bc(c4[:, 0, :nts], 4), ALU.subtract), reads=['Lg', 'c4a'], writes=['gx'])
                        kb.op(A, lambda: nc.scalar.activation(gx[:, :nts, :], gx[:, :nts, :], AF.Exp), reads=['gx'], writes=['gx'])
                        kb.op(V, lambda: nc.vector.reduce_sum(c4[:, 1, :nts], gx[:, :nts, :], AX.X), reads=['gx'], writes=['c4b'])
                        kb.op(V, lambda: nc.vector.tensor_tensor(gm[:, :nts, :], G4, bc(c4[:, 0, :nts], 4), ALU.is_ge), reads=['Lg', 'c4a'], writes=['gm'])
                        kb.op(V, lambda: nc.vector.tensor_scalar(gm[:, :nts, :], gm[:, :nts, :], -1.0, BIG, ALU.add, ALU.mult), reads=['gm'], writes=['gm'])
                        kb.op(V, lambda: nc.vector.tensor_tensor(E4.rearrange("p t (g e) -> p t g e", e=8),
                                                                 L4[:, :, 4:36].rearrange("p t (g e) -> p t g e", e=8),
                                                                 gm[:, :nts, :].unsqueeze(3).to_broadcast([128, nts, 4, 8]), ALU.add),
                              reads=['Lg', 'gm'], writes=['EM'])
                        kb.op(V, lambda: nc.vector.reduce_max(c4[:, 2, :nts], E4, AX.X), reads=['EM'], writes=['c4c'])
                        kb.op(V, lambda: nc.vector.tensor_tensor(E24, E4, bc(c4[:, 2, :nts], 32), ALU.is_ge), reads=['EM', 'c4c'], writes=['EM2'])
                        kb.op(V, lambda: nc.vector.scalar_tensor_tensor(E24, E24, -BIG, E4, ALU.mult, ALU.add), reads=['EM', 'EM2'], writes=['EM2'])
                        kb.op(V, lambda: nc.vector.reduce_max(c4[:, 3, :nts], E24, AX.X), reads=['EM2'], writes=['c4d'])
                        kb.op(V, lambda: nc.vector.tensor_tensor(P4, E4, bc(c4[:, 2, :nts], 32), ALU.subtract), reads=['EM', 'c4c'], writes=['pe_'])
                        kb.op(A, lambda: nc.scalar.activation(P4, P4, AF.Exp), reads=['pe_'], writes=['pe_'])
                        kb.op(V, lambda: nc.vector.tensor_tensor(M4, E4, bc(c4[:, 3, :nts], 32), ALU.is_ge), reads=['EM', 'c4d'], writes=[('M', blk)])
                        kb.op(V, lambda: nc.vector.tensor_tensor(P4, P4, M4, ALU.mult), reads=['pe_', ('M', blk)], writes=['pe_'])
                        kb.op(V, lambda: nc.vector.reduce_sum(c4[:, 4, :nts], P4, AX.X), reads=['pe_'], writes=['c4e'])
                        kb.op(V, lambda: nc.vector.tensor_tensor(c4[:, 4, :nts], c4[:, 4, :nts], c4[:, 1, :nts], ALU.mult), reads=['c4e', 'c4b'], writes=['c4e'])
                        kb.op(V, lambda: nc.vector.reciprocal(c4[:, 5, :nts], c4[:, 4, :nts]), reads=['c4e'], writes=['c4f'])
                        kb.op(V, lambda: nc.vector.tensor_tensor(gw4, P4, bc(c4[:, 5, :nts], 32), ALU.mult), reads=['pe_', 'c4f'], writes=[('gw', blk)])
                        for ts in range(nts):
                            kb.op(PE, lambda ts=ts: nc.tensor.matmul(psC[:, ts, :], triU[:], Mall[:, tc0 + ts, :], start=True, stop=False),
                                  reads=[('M', blk), 'triU'], writes=['psC'])
                            for t2 in range(ts):
                                kb.op(PE, lambda ts=ts, t2=t2: nc.tensor.matmul(psC[:, ts, :], onesf[:], Mall[:, tc0 + t2, :], start=False, stop=False),
                                      reads=[('M', blk)], writes=['psC'])
                            kb.op(PE, lambda ts=ts: nc.tensor.matmul(psC[:, ts, :], onesf[:], Macc[:], start=False, stop=True),
                                  reads=['Macc', 'onesf'], writes=['psC'])
                        kb.op(A, lambda: nc.scalar.copy(Call[:, tc0:tc0 + nts, :], psC[:, :nts, :]), reads=['psC'], writes=[('C', blk)])
                        kb.op(V, lambda: nc.vector.reduce_sum(msum[:], M4.rearrange("p t e -> p e t"), AX.X), reads=[('M', blk)], writes=['msum'])
                        kb.op(V, lambda: nc.vector.tensor_tensor(Macc[:], Macc[:], msum[:], ALU.add), reads=['Macc', 'msum'], writes=['Macc'])

                    modulate(l, 1, b, nblk, ps2, router={'h2f': h2f, 'fn': router_fn})
                    cnt = sb("cnt", [32, 4], F32, ps2)
                    cmp18 = sb("cmp18", [32, 18], F32, ps2)
                    padb = sb("padb", [32, 128], F32, ps2)
                    end_rep = sb("end_rep", [128, 32], F32, ps2)
                    cmpb = sb("cmpb", [128, 68, 32], F32, ps2)
                    bef = sb("bef", [128, 68], F32, ps2)
                    psK = pst("psK", [32, 1], F32, ps2)
                    psB = pst("psB", [128, 32], F32, ps2)
                    psE = pst("psE", [128, 32], F32, ps2)
                    kb.op(PE, lambda: nc.tensor.matmul(psK[:], Macc[:], onesf[:, 0:1], start=True, stop=True), reads=['Macc'], writes=['psK'])
                    kb.op(V, lambda: nc.vector.tensor_copy(cnt[:, 0:1], psK[:]), reads=['psK'], writes=['cnt0'])
                    kb.op(V, lambda: nc.vector.tensor_scalar(cmp18[:], thr18[:], cnt[:, 0:1], None, ALU.is_lt), reads=['cnt0', 'thr18'], writes=['cmp18'])
                    kb.op(V, lambda: nc.vector.reduce_sum(cnt[:, 1:2], cmp18[:], AX.X), reads=['cmp18'], writes=['cnt1'])
                    kb.op(V, lambda: nc.vector.tensor_scalar(cnt[:, 2:3], cnt[:, 1:2], 256.0, None, ALU.mult), reads=['cnt1'], writes=['cnt2'])
                    kb.op(V, lambda: nc.vector.tensor_copy(padb[:], cnt[:, 2:3].to_broadcast([32, 128])), reads=['cnt2'], writes=['padb'])
                    kb.op(PE, lambda: nc.tensor.matmul(psB[:], padb[:], triU[0:32, 0:32], start=True, stop=True), reads=['padb', 'triU'], writes=['psB'])
                    kb.op(PE, lambda: nc.tensor.matmul(psE[:], padb[:], UI[:], start=True, stop=True), reads=['padb', 'UI'], writes=['psE'])
                    kb.op(V, lambda: nc.vector.tensor_copy(base_rep[:], psB[:]), reads=['psB'], writes=['base_rep'])
                    kb.op(V, lambda: nc.vector.tensor_copy(end_rep[:], psE[:]), reads=['psE'], writes=['end_rep'])
                    kb.op(V, lambda: nc.vector.tensor_tensor(cmpb[:], end_rep[:].unsqueeze(1).to_broadcast([128, 68, 32]),
                                                             thr68[:].unsqueeze(2).to_broadcast([128, 68, 32]), ALU.is_le),
                          reads=['end_rep', 'thr68'], writes=['cmpb'])
                    kb.op(V, lambda: nc.vector.reduce_sum(bef[:], cmpb[:], AX.X), reads=['cmpb'], writes=['bef'])
                    kb.op(V, lambda: nc.vector.tensor_scalar(bef[:], bef[:], 31.0, 128.0, ALU.min, ALU.mult), reads=['bef'], writes=['bef'])
                    kb.op(V, lambda: nc.vector.tensor_scalar(bef[:], bef[:], iop[:, 0:1], float(l * 4096), ALU.add, ALU.add), reads=['bef', 'iop'], writes=['bef'])
                    garb = sb("garb", [128, 68], F32, ps2)
                    kb.op(V, lambda: nc.vector.tensor_scalar(garb[:], thr68[:], end_rep[:, 31:32], 1.0e6, ALU.is_ge, ALU.mult),
                          reads=['end_rep', 'thr68'], writes=['garb'])
                    kb.op(V, lambda: nc.vector.tensor_tensor(bef[:], bef[:], garb[:], ALU.add), reads=['bef', 'garb'], writes=['bef'])
                    kb.op(V, lambda: nc.vector.tensor_copy(idxw[:], bef[:]), reads=['bef'], writes=['idxw'])
                kb.barrier()
                with ExitStack() as ps2:
                    vv_ = sb("vv_", [128, 32], F32, ps2)
                    v2_ = sb("v2_", [128, 32], F32, ps2)
                    eq_ = sb("eq_", [128, 32], F32, ps2)
                    dcol = sb("dcol", [128, 4], F32, ps2)
                    Xtok = [sb("Xtok%d" % i, [128, D], BF16, ps2) for i in range(2)]
                    psXt = [pst("psXt%d" % i, [128, 8, 128], BF16, ps2) for i in range(2)]
                    for tc in range(nch):
                        blk = tc // 4
                        s_ = tc % 2
                        kb.op(V, lambda tc=tc: nc.vector.tensor_tensor(vv_[:], Call[:, tc, :], base_rep[:], ALU.add), reads=[], writes=['vv_'])
                        kb.op(V, lambda tc=tc: nc.vector.scalar_tensor_tensor(vv_[:], vv_[:], 1.0, Mall[:, tc, :], ALU.add, ALU.mult), reads=['vv_'], writes=['vv_'])
                        kb.op(V, lambda: nc.vector.reduce_max(dcol[:, 0:1], vv_[:], AX.X), reads=['vv_'], writes=['dc0'])
                        kb.op(V, lambda: nc.vector.tensor_scalar(eq_[:], vv_[:], dcol[:, 0:1], None, ALU.is_equal), reads=['vv_', 'dc0'], writes=['eq_'])
                        kb.op(V, lambda tc=tc: nc.vector.tensor_tensor(v2_[:], eq_[:], gwall[:, tc, :], ALU.mult), reads=['eq_'], writes=['v2_'])
                        kb.op(V, lambda tc=tc: nc.vector.reduce_sum(gAB[:, 0, tc:tc + 1], v2_[:], AX.X), reads=['v2_'], writes=[('gA', tc)])
                        kb.op(V, lambda: nc.vector.tensor_scalar(eq_[:], vv_[:], dcol[:, 0:1], None, ALU.not_equal), reads=['vv_', 'dc0', 'v2_'], writes=['eq_'])
                        kb.op(V, lambda: nc.vector.tensor_tensor(v2_[:], vv_[:], eq_[:], ALU.mult), reads=['eq_', 'vv_'], writes=['v2_'])
                        kb.op(V, lambda: nc.vector.reduce_max(dcol[:, 1:2], v2_[:], AX.X), reads=['v2_'], writes=['dc1'])
                        kb.op(V, lambda: nc.vector.tensor_scalar(eq_[:], v2_[:], dcol[:, 1:2], None, ALU.is_equal), reads=['v2_', 'dc1'], writes=['eq_'])
                        kb.op(V, lambda tc=tc: nc.vector.tensor_tensor(v2_[:], eq_[:], gwall[:, tc, :], ALU.mult), reads=['eq_'], writes=['v2_'])
                        kb.op(V, lambda tc=tc: nc.vector.reduce_sum(gAB[:, 1, tc:tc + 1], v2_[:], AX.X), reads=['v2_'], writes=[('gB', tc)])
                        kb.op(V, lambda: nc.vector.tensor_scalar(dcol[:, 2:4], dcol[:, 0:2], -1.0, None, ALU.add), reads=['dc0', 'dc1'], writes=['dc23'])
                        kb.op(V, lambda tc=tc: nc.vector.tensor_copy(dA[:, tc:tc + 1], dcol[:, 2:3]), reads=['dc23'], writes=[('dA', tc)])
                        kb.op(V, lambda tc=tc: nc.vector.tensor_copy(dB[:, tc:tc + 1], dcol[:, 3:4]), reads=['dc23'], writes=[('dB', tc)])
                        for dc in range(8):
                            kb.op(PE, lambda dc=dc, tc=tc, s_=s_: nc.tensor.transpose(psXt[s_][:, dc, :], hT[:, dc, tc * 128:(tc + 1) * 128], identb[:]),
                                  reads=[hk(dc, blk)], writes=['psXt%d' % s_])
                        kb.op(A, lambda s_=s_: nc.scalar.copy(Xtok[s_][:], psXt[s_][:].rearrange("p a b -> p (a b)")), reads=['psXt%d' % s_], writes=['Xtok%d' % s_])
                        for (dd, dk_) in ((dA, 'dA'), (dB, 'dB')):
                            kb.dma('pool', 'scat%d' % s_, Xs_d[:, :], Xtok[s_][:, :], reads=['Xtok%d' % s_, (dk_, tc)], writes=['Xs'],
                                   indirect=('out', dd[:, tc:tc + 1]))
                kb.barrier()
                with ExitStack() as ps2:
                    GU = [WA[:, 0:2, :], WA[:, 2:4, :]]
                    Dn = [sb("Dn%d" % i, [128, 4096], BF16, ps2) for i in range(2)]
                    Xb = [sb("Xb%d" % i, [128, 2, D], BF16, ps2) for i in range(2)]
                    XT = [sb("XT%d" % i, [128, 2, 8, 128], BF16, ps2) for i in range(2)]
                    sgt = [sb("sgt%d" % i, [128, 512], F32, ps2) for i in range(2)]
                    act = [sb("act%d" % i, [128, 2, 512], BF16, ps2) for i in range(2)]
                    actT = [sb("actT%d" % i, [128, 4, 128], BF16, ps2) for i in range(2)]
                    Yb_ = [sb("Yb%d" % i, [128, D], F32, ps2) for i in range(2)]
                    psXT = pst("psXT", [128, 8, 128], BF16, ps2)
                    psG = [pst("psG%d" % i, [128, 512], F32, ps2) for i in range(2)]
                    psU = [pst("psU%d" % i, [128, 512], F32, ps2) for i in range(2)]
                    psAT = pst("psAT", [128, 4, 256], BF16, ps2)
                    psD = [pst("psD%d" % i, [128, 512], F32, ps2) for i in range(2)]

                    def dmaGU(j):
                        s_ = j % 2
                        kb.dma('pool', 'RWg%d' % s_, GU[s_][:, 0, :], w16_d[0][:, :], reads=['idxw'], writes=['RWg%d' % s_], indirect=('in', idxw[:, j:j + 1], bcw))
                        kb.dma('pool', 'RWu%d' % s_, GU[s_][:, 1, :], w16_d[1][:, :], reads=['idxw'], writes=['RWu%d' % s_], indirect=('in', idxw[:, j:j + 1], bcw))

                    def dmaDn(j):
                        s_ = j % 2
                        kb.dma('pool', 'RWd%d' % s_, Dn[s_][:, :], w16_d[2][:, :], reads=['idxw'], writes=['RWd%d' % s_], indirect=('in', idxw[:, j:j + 1], bcw))

                    def xload(j):
                        s_ = j % 2
                        kb.dma('sp', 'xb%d' % s_, Xb[s_][:], Xs_d[j * 256:(j + 1) * 256, :].rearrange("(u p) d -> p u d", p=128),
                               reads=['Xs'], writes=['Xb%d' % s_])

                    def Tx(j, u):
                        s_ = j % 2
                        for dc in range(8):
                            kb.op(PE, lambda dc=dc: nc.tensor.transpose(psXT[:, dc, :], Xb[s_][:, u, dc * 128:(dc + 1) * 128], identb[:]),
                                  reads=['Xb%d' % s_], writes=['psXT'])
                        kb.op(A, lambda: nc.scalar.copy(XT[s_][:, u], psXT[:]), reads=['psXT'], writes=[('XT', s_, u)])

                    def GUm(j, u):
                        s_ = j % 2
                        wg_ = GU[s_][:, 0, :].rearrange("p (k c) -> p k c", k=8)
                        wu_ = GU[s_][:, 1, :].rearrange("p (k c) -> p k c", k=8)
                        for kc in range(8):
                            kb.op(PE, lambda kc=kc: nc.tensor.matmul(psG[u][:], XT[s_][:, u, kc, :], wg_[:, kc, :], start=(kc == 0), stop=(kc == 7)),
                                  reads=[('XT', s_, u), 'RWg%d' % s_], writes=['psG%d' % u])
                        for kc in range(8):
                            kb.op(PE, lambda kc=kc: nc.tensor.matmul(psU[u][:], XT[s_][:, u, kc, :], wu_[:, kc, :], start=(kc == 0), stop=(kc == 7)),
                                  reads=[('XT', s_, u), 'RWu%d' % s_], writes=['psU%d' % u])
                        kb.op(A, lambda: nc.scalar.activation(sgt[u][:], psG[u][:], AF.Silu), reads=['psG%d' % u], writes=['sgt%d' % u])
                        kb.op(V, lambda: nc.vector.tensor_tensor(act[s_][:, u, :], psU[u][:], sgt[u][:], ALU.mult),
                              reads=['psU%d' % u, 'sgt%d' % u], writes=[('act', s_, u)])

                    def Tact(j, u):
                        s_ = j % 2
                        for hc in range(4):
                            kb.op(PE, lambda hc=hc: nc.tensor.transpose(psAT[:, hc, 0:128], act[s_][:, u, hc * 128:(hc + 1) * 128], identb[:]),
                                  reads=[('act', s_, u)], writes=['psAT'])
                        kb.op(A, lambda: nc.scalar.copy(actT[u][:], psAT[:, :, 0:128]), reads=['psAT'], writes=['actT%d' % u])

                    def Dm(j, u):
                        s_ = j % 2
                        wd_ = Dn[s_][:, :].rearrange("p (k c) -> p k c", k=4)
                        for c2 in range(2):
                            for hc in range(4):
                                kb.op(PE, lambda hc=hc, c2=c2: nc.tensor.matmul(psD[c2][:], actT[u][:, hc, :], wd_[:, hc, c2 * 512:(c2 + 1) * 512],
                                                                               start=(hc == 0), stop=(hc == 3)),
                                      reads=['actT%d' % u, 'RWd%d' % s_], writes=['psD%d' % c2])
                        kb.op(A, lambda: nc.scalar.copy(Yb_[u][:, 0:512], psD[0][:]), reads=['psD0'], writes=['Yb%da' % u])
                        kb.op(V, lambda: nc.vector.tensor_copy(Yb_[u][:, 512:1024], psD[1][:]), reads=['psD1'], writes=['Yb%db' % u])
                        r0 = (j * 2 + u) * 128
                        kb.dma('sp', 'ys%d' % u, Ys_d[r0:r0 + 128, :], Yb_[u][:], reads=['Yb%da' % u, 'Yb%db' % u], writes=['Ys'])

                    if ('pc%d' % l) in kb.lazy:
                        kb.lazy.discard('pc%d' % l)
                    kb._wait('pool', ('dma', 'pc%d' % l), kb.dcnt['pc%d' % l])
                    dmaGU(0)
                    dmaGU(1)
                    dmaDn(0)
                    dmaDn(1)
                    xload(0)
                    xload(1)
                    Tx(0, 0)
                    Tx(0, 1)
                    GUm(0, 0)
                    GUm(0, 1)
                    dmaGU(2)
                    for j in range(NBK):
                        nx = j + 1 < NBK
                        if j + 2 < NBK:
                            xload(j + 2)
                        if nx:
                            Tx(j + 1, 0)
                        Tact(j, 0)
                        if nx:
                            Tx(j + 1, 1)
                        Dm(j, 0)
                        if nx:
                            GUm(j + 1, 0)
                        Tact(j, 1)
                        if nx:
                            GUm(j + 1, 1)
                            if j + 3 < NBK:
                                dmaGU(j + 3)
                        Dm(j, 1)
                        if j + 2 < NBK:
                            dmaDn(j + 2)
                kb.barrier()
                with ExitStack() as ps2:
                    Ya = [sb("Ya%d" % i, [128, D], F32, ps2) for i in range(2)]
                    Yc = [sb("Yc%d" % i, [128, D], F32, ps2) for i in range(2)]
                    ff = sb("ff", [128, D], F32, ps2)
                    psF = [pst("psF%d" % i, [128, 4, 128], F32, ps2) for i in range(2)]
                    for tc in range(nch):
                        blk = tc // 4
                        n = b if blk < 4 else NB
                        s_ = tc % 2
                        kb.dma('pool', 'ga%d' % s_, Ya[s_][:, :], Ys_d[:, :], reads=['Ys'], writes=['Ya%d' % s_], indirect=('in', dA[:, tc:tc + 1]))
                        kb.dma('pool', 'gb%d' % s_, Yc[s_][:, :], Ys_d[:, :], reads=['Ys'], writes=['Yc%d' % s_], indirect=('in', dB[:, tc:tc + 1]))
                        kb.op(A, lambda s_=s_, tc=tc: nc.scalar.activation(ff[:], Ya[s_][:], AF.Identity, scale=gAB[:, 0, tc:tc + 1]),
                              reads=['Ya%d' % s_], writes=['ff'])
                        kb.op(V, lambda s_=s_, tc=tc: nc.vector.scalar_tensor_tensor(ff[:], Yc[s_][:], gAB[:, 1, tc:tc + 1], ff[:], ALU.mult, ALU.add),
                              reads=['Yc%d' % s_, 'ff'], writes=['ff'])
                        for hf in range(2):
                            for d4 in range(4):
                                dc = hf * 4 + d4
                                kb.op(PE, lambda dc=dc, d4=d4, hf=hf: nc.tensor.transpose(psF[hf][:, d4, :], ff[:, dc * 128:(dc + 1) * 128], identf[:]),
                                      reads=['ff'], writes=['psF%d' % hf])
                            for d4 in range(4):
                                dc = hf * 4 + d4
                                g2 = modc(l, 1, dc, n)[2]
                                kb.op(V, lambda dc=dc, d4=d4, hf=hf, g2=g2, tc=tc: nc.vector.scalar_tensor_tensor(
                                    xT[:, dc, tc * 128:(tc + 1) * 128], psF[hf][:, d4, :], g2, xT[:, dc, tc * 128:(tc + 1) * 128], ALU.mult, ALU.add),
                                    reads=['psF%d' % hf], writes=[('xTc', dc, tc)])
            kb.barrier()

        def attention(b):
            with ExitStack() as ps:
                modulate(1, 0, b, 5, ps)
            kb.barrier()
            with ExitStack() as ps:
                kT = sb("akT", [128, 2, T], BF16, ps)
                vv = sb("avv", [128, 18, 256], BF16, ps)
                oT = sb("aoT", [128, 4, S], BF16, ps)
                qTs = [sb("aqT%d" % i, [128, 512], BF16, ps) for i in range(2)]
                PT = [sb("aPT%d" % i, [128, 512], BF16, ps) for i in range(2)]
                sq = sb("asq", [128, 512], BF16, ps)
                rt = sb("art", [128, 512], F32, ps)
                R = sb("aR", [128, 512], F32, ps)
                rA = sb("arA", [128, 512], F32, ps)
                rB = sb("arB", [128, 512], F32, ps)
                rD = sb("arD", [128, 512], F32, ps)
                psS = [pst("apsS%d" % i, [128, 512], F32, ps) for i in range(2)]
                psO = pst("apsO", [128, 512], F32, ps)
                psDn = pst("apsDn", [128, 512], F32, ps)
                ps1 = pst("aps1", [128, 512], F32, ps)
                ps2_ = pst("aps2", [128, 512], F32, ps)
                ps3 = pst("aps3", [128, 512], F32, ps)
                psX = pst("apsX", [128, 512], F32, ps)
                wo_ = WA[:, 2:4, :].rearrange("p a (k c) -> p (a k) c", k=4)
                wload(2, wo_, awo_d.rearrange("(hq p) d -> p hq d", p=128), keys=['W2', 'W3'])
                wflat1 = WA[:, 1, :]
                wk_ = wflat1.rearrange("p (g s k c) -> p g s k c", g=2, s=2, k=8)
                for g in range(2):
                    c0 = 1024 + g * 128
                    wload(1, wk_[:, g, 0], aw_d[:, c0:c0 + 128].rearrange("(kc p) c -> p kc c", p=128))
                    wload(1, wk_[:, g, 1], aw_d[:, ATT_IN + c0:ATT_IN + c0 + 128].rearrange("(kc p) c -> p kc c", p=128))
                wv_ = WA[:, 0, 0:2048].rearrange("p (k c) -> p k c", k=8)
                wload(0, wv_, aw_d[:, 1280:1536].rearrange("(kc p) c -> p kc c", p=128))

                def qk_norm_rope(wsl, w4, tab, gcol, dst, dkeys, blk, hkblk):
                    t0, W = blk_rng(blk)
                    for kc in range(8):
                        kb.op(PE, lambda kc=kc: nc.tensor.matmul(ps1[:, :W], w4[:, 0, kc, :], hT[:, kc, t0:t0 + W], start=(kc == 0), stop=(kc == 7)),
                              reads=[wsl] + hks(hkblk), writes=['aps1'])
                    kb.op(A, lambda: nc.scalar.activation(sq[:, :W], ps1[:, :W], AF.Square), reads=['aps1'], writes=['asq'])
                    if blk < 4:
                        for kc in range(8):
                            kb.op(PE, lambda kc=kc: nc.tensor.matmul(ps2_[:, :W], w4[:, 1, kc, :], hT[:, kc, t0:t0 + W], start=(kc == 0), stop=(kc == 7)),
                                  reads=[wsl] + hks(hkblk), writes=['aps2'])
                    kb.op(PE, lambda: nc.tensor.matmul(ps3[:, :W], onesb[:], sq[:, :W], start=True, stop=True), reads=['asq'], writes=['aps3'])
                    kb.op(A, lambda: nc.scalar.activation(rt[:, :W], ps3[:, :W], AF.Sqrt, bias=epsn[:, 0:1], scale=1.0 / 128), reads=['aps3'], writes=['art'])
                    kb.op(V, lambda: nc.vector.reciprocal(R[:, :W], rt[:, :W]), reads=['art'], writes=['aR'])
                    if blk < 4:
                        nr = W // 64
                        r0 = t0 // 64
                        for (src, dstt, ti, pk, dk_) in ((ps1, rA, 0, 'aps1', 'arA'), (ps2_, rB, 1, 'aps2', 'arB')):
                            kb.op(V, lambda src=src, dstt=dstt, ti=ti: nc.vector.tensor_tensor(
                                dstt[0:64, :W].rearrange("p (a b) -> p a b", b=64), src[0:64, :W].rearrange("p (a b) -> p a b", b=64),
                                tab[0:64, ti, r0:r0 + nr].unsqueeze(2).to_broadcast([64, nr, 64]), ALU.mult),
                                reads=[pk], writes=[dk_ + 'lo'])
                            kb.op(V, lambda src=src, dstt=dstt, ti=ti: nc.vector.tensor_tensor(
                                dstt[64:128, :W].rearrange("p (a b) -> p a b", b=64), src[64:128, :W].rearrange("p (a b) -> p a b", b=64),
                                tab[64:128, ti, :].unsqueeze(1).to_broadcast([64, nr, 64]), ALU.mult),
                                reads=[pk], writes=[dk_ + 'hi'])
                        kb.op(P, lambda: nc.gpsimd.tensor_tensor(rA[:, :W], rA[:, :W], rB[:, :W], ALU.add),
                              reads=['arAlo', 'arAhi', 'arBlo', 'arBhi'], writes=['arAlo', 'arAhi'])
                        kb.op(P, lambda: nc.gpsimd.tensor_tensor(dst, rA[:, :W], R[:, :W], ALU.mult),
                              reads=['arAlo', 'arAhi', 'aR'], writes=dkeys)
                    else:
                        kb.op(V, lambda: nc.vector.scalar_tensor_tensor(dst, ps1[:, :W], gcol, R[:, :W], ALU.mult, ALU.mult),
                              reads=['aps1', 'aR'], writes=dkeys)

                def load_wq(hq):
                    qs = (hq + 1) % 2
                    wq_ = WA[:, 0, qs * 2048:(qs + 1) * 2048].rearrange("p (s k c) -> p s k c", s=2, k=8)
                    qkey = 'WQ%d' % qs
                    deps_extra = ['W0'] if qs == 0 else []
                    kb.dma('pool', qkey, wq_[:, 0], aw_d[:, hq * 128:(hq + 1) * 128].rearrange("(kc p) c -> p kc c", p=128),
                           writes=[qkey] + deps_extra)
                    kb.dma('pool', qkey, wq_[:, 1], aw_d[:, ATT_IN + hq * 128:ATT_IN + (hq + 1) * 128].rearrange("(kc p) c -> p kc c", p=128),
                           writes=[qkey] + deps_extra)

                load_wq(0)
                for g in range(2):
                    for blk in range(5):
                        t0, W = blk_rng(blk)
                        qk_norm_rope('W1', wk_[:, g], atk, gk[:, 0:1], kT[:, g, t0:t0 + W], [('akT', g, blk)], blk, blk)
                for tc in range(18):
                    pv, pvk = (psX, 'apsX') if tc % 2 == 0 else (ps3, 'aps3')
                    for kc in range(8):
                        kb.op(PE, lambda kc=kc, tc=tc, pv=pv: nc.tensor.matmul(pv[:, 0:256], hT[:, kc, tc * 128:(tc + 1) * 128], wv_[:, kc, :],
                                                                              start=(kc == 0), stop=(kc == 7)),
                              reads=['W0'] + hks(tc // 4), writes=[pvk])
                    kb.op(A, lambda tc=tc, pv=pv: nc.scalar.copy(vv[:, tc, :], pv[:, 0:256]), reads=[pvk], writes=[('avv', tc)])
                scale = 128.0 ** -0.5
                for half in range(2):
                    items = [(h4, qb) for h4 in range(4) for qb in range(4)]

                    def qproj(i):
                        h4, qb = items[i]
                        hq = half * 4 + h4
                        qs = (hq + 1) % 2
                        wq_ = WA[:, 0, qs * 2048:(qs + 1) * 2048].rearrange("p (s k c) -> p s k c", s=2, k=8)
                        qkey = 'WQ%d' % qs
                        if qb == 0 and hq + 1 < 8:
                            load_wq(hq + 1)
                        qk_norm_rope(qkey, wq_, atq, gq[:, 0:1], qTs[i % 2][:, :], ['aqT%d' % (i % 2)], qb, qb)

                    qproj(0)
                    for i in range(16):
                        h4, qb = items[i]
                        hq = half * 4 + h4
                        g = hq // 4
                        t0, W = blk_rng(qb)
                        qT = qTs[i % 2]
                        qk_ = 'aqT%d' % (i % 2)
                        if i + 1 < 16:
                            qproj(i + 1)
                        def Sm(sc):
                            pS = psS[sc % 2]
                            kb.op(PE, lambda: nc.tensor.matmul(pS[:], kT[:, g, sc * 128:(sc + 1) * 128], qT[:], start=True, stop=True),
                                  reads=[('akT', g, sc // 4), qk_], writes=['apsS%d' % (sc % 2)])
                            pt = PT[sc % 2]
                            kb.op(A, lambda: nc.scalar.activation(pt[:], pS[:], AF.Exp, scale=scale),
                                  reads=['apsS%d' % (sc % 2)], writes=['aPT%d' % (sc % 2)])

                        def PVm(sc):
                            pt = PT[sc % 2]
                            kb.op(PE, lambda: nc.tensor.matmul(psO[:], vv[:, sc, g * 128:(g + 1) * 128], pt[:], start=(sc == 0), stop=(sc == 17)),
                                  reads=['aPT%d' % (sc % 2), ('avv', sc)], writes=['apsO'])
                            kb.op(PE, lambda: nc.tensor.matmul(psDn[:], onesb[:], pt[:], start=(sc == 0), stop=(sc == 17)),
                                  reads=['aPT%d' % (sc % 2)], writes=['apsDn'])

                        Sm(0)
                        for sc in range(18):
                            if sc + 1 < 18:
                                Sm(sc + 1)
                            PVm(sc)
                        kb.op(V, lambda: nc.vector.reciprocal(rD[:], psDn[:]), reads=['apsDn'], writes=['arD'])
                        kb.op(V, lambda h4=h4, t0=t0: nc.vector.tensor_tensor(oT[:, h4, t0:t0 + 512], psO[:], rD[:], ALU.mult),
                              reads=['apsO', 'arD'], writes=[('aoT', h4, qb)])
                    for qb in range(4):
                        t0, W = blk_rng(qb)
                        for dc in range(8):
                            po, pok = (psX, 'apsX') if dc % 2 == 0 else (ps1, 'aps1')
                            for h4 in range(4):
                                kb.op(PE, lambda h4=h4, dc=dc, po=po: nc.tensor.matmul(po[:], wo_[:, half * 4 + h4, dc * 128:(dc + 1) * 128], oT[:, h4, t0:t0 + 512],
                                                                                      start=(h4 == 0), stop=(h4 == 3)),
                                      reads=['W2', 'W3'] + [('aoT', i, qb) for i in range(4)], writes=[pok])
                            g1 = modc(1, 0, dc, b)[2]
                            kb.op(V, lambda dc=dc, g1=g1, po=po: nc.vector.scalar_tensor_tensor(
                                xT[:, dc, t0:t0 + 512], po[:], g1, xT[:, dc, t0:t0 + 512], ALU.mult, ALU.add),
                                reads=[pok, xk(dc, qb)], writes=[xk(dc, qb)])
            kb.barrier()

        def store(b, final_norm, which_ctx=False):
            with ExitStack() as ps:
                sqb = sb("sqb", [128, 8, 512], BF16, ps)
                rt = sb("rt", [128, 512], F32, ps)
                R = sb("R", [128, 512], F32, ps)
                tmp = sb("ftmp", [128, 512], F32, ps)
                of = sb("of", [128, 8, 512], F32, ps)
                ost = [sb("ost%d" % i, [128, D], F32, ps) for i in range(4)]
                pss = pst("pss", [128, 512], F32, ps)
                ptr = [pst("ptr%d" % i, [128, 4, 128], F32, ps) for i in range(4)]
                blks = [4] if which_ctx else [0, 1, 2, 3]
                oi = 0
                for blk in blks:
                    t0, W = blk_rng(blk)
                    if final_norm:
                        rms_block(blk, sqb, pss, rt, R, 'pss')
                        for dc in range(8):
                            kb.op(V, lambda dc=dc: nc.vector.tensor_tensor(tmp[:, :W], xT[:, dc, t0:t0 + W], R[:, :W], ALU.mult),
                                  reads=[xk(dc, blk), 'R'], writes=['ftmp'])
                            kb.op(A, lambda dc=dc: nc.scalar.activation(of[:, dc, :W], tmp[:, :W], AF.Identity, scale=fg[:, dc:dc + 1]),
                                  reads=['ftmp'], writes=[('of', dc)])
                    for ts in range(W // 128):
                        o_ = ost[oi % 4]
                        okey = 'ost%d' % (oi % 4)
                        for hf in range(2):
                            for d4 in range(4):
                                dc = hf * 4 + d4
                                src = of[:, dc, ts * 128:(ts + 1) * 128] if final_norm else xT[:, dc, t0 + ts * 128:t0 + (ts + 1) * 128]
                                rk = [('of', dc)] if final_norm else [xk(dc, blk)]
                                pi = (oi % 2) * 2 + hf
                                kb.op(PE, lambda src=src, pi=pi, d4=d4: nc.tensor.transpose(ptr[pi][:, d4, :], src, identf[:]),
                                      reads=rk, writes=['ptr%d' % pi])
                            pi = (oi % 2) * 2 + hf
                            if hf == 0:
                                kb.op(A, lambda o_=o_, pi=pi: nc.scalar.copy(o_[:, 0:512], ptr[pi][:].rearrange("p a b -> p (a b)")),
                                      reads=['ptr%d' % pi], writes=[okey + 'a'])
                            else:
                                kb.op(V, lambda o_=o_, pi=pi: nc.vector.tensor_copy(o_[:, 512:1024], ptr[pi][:].rearrange("p a b -> p (a b)")),
                                      reads=['ptr%d' % pi], writes=[okey + 'b'])
                        tok0 = t0 + ts * 128
                        if which_ctx:
                            dst = outc_d[b, tok0 - 2048:tok0 - 2048 + 128, :]
                        else:
                            dst = out_d[b, tok0:tok0 + 128, :]
                        kb.dma('sp', 'o%d' % (oi % 4), dst, o_[:], reads=[okey + 'a', okey + 'b'])
                        oi += 1
            kb.barrier()

        for b in range(NB):
            load_x(b)
            stages = [('ret', lambda: retention(b)), ('moe0', lambda: moe(0, b)),
                      ('att', lambda: attention(b)), ('moe1', lambda: moe(1, b))]
            done_all = True
            for name, fn in stages:
                fn()
                if stop == name:
                    done_all = False
                    break
            store(b, final_norm=done_all)
            if dump_ctx:
                store(b, final_norm=False, which_ctx=True)
        kb.finish()
    return nc


def _rope_tables():
    theta = 10000.0
    p = np.arange(128)
    inv = theta ** (-(np.arange(64, dtype=np.float32)) / 64.0)
    invp = inv[p % 64].astype(np.float32)
    sign = np.where(p < 64, -1.0, 1.0).astype(np.float32)
    rows = np.arange(32, dtype=np.float32)
    cols = np.arange(64, dtype=np.float32)
    ar = (rows[None, :] * invp[:, None]).astype(np.float32)
    ac = (cols[None, :] * invp[:, None]).astype(np.float32)
    ret_tab = np.concatenate([np.cos(ar), sign[:, None] * np.sin(ar), np.cos(ac), sign[:, None] * np.sin(ac)], axis=1).astype(np.float32)
    inv2 = theta ** (-(np.arange(32, dtype=np.float32)) / 32.0)
    invp2 = inv2[p % 32].astype(np.float32)
    sign2 = np.where((p % 64) < 32, -1.0, 1.0).astype(np.float32)
    att_tab = np.zeros((128, 2, 64), np.float32)
    a_r = (rows[None, :] * invp2[:, None]).astype(np.float32)
    a_c = (cols[None, :] * invp2[:, None]).astype(np.float32)
    att_tab[:64, 0, :32] = np.cos(a_r)[:64]
    att_tab[:64, 1, :32] = (sign2[:, None] * np.sin(a_r))[:64]
    att_tab[64:, 0, :] = np.cos(a_c)[64:]
    att_tab[64:, 1, :] = (sign2[:, None] * np.sin(a_c))[64:]
    return ret_tab, att_tab


def _shared_inputs(inp):
    f = lambda a: np.ascontiguousarray(np.asarray(a, dtype=np.float32))
    ret_tab, att_tab = _rope_tables()
    sw_ret = np.concatenate([(np.arange(128) + 64) % 128 + c * 128 for c in range(16)])
    rw = f(inp['ret_w_in'])[0]
    rw_ext = np.concatenate([rw, rw[:, :2048][:, sw_ret]], axis=1)
    pa = np.arange(128)
    sw128 = (pa // 64) * 64 + ((pa % 64) + 32) % 64
    sw_att = np.concatenate([sw128 + c * 128 for c in range(10)])
    aw = f(inp['att_w_qkv'])[0]
    aw_ext = np.concatenate([aw, aw[:, :1280][:, sw_att]], axis=1)
    gqv = f(inp['att_q_gain'])[0]
    gkv = f(inp['att_k_gain'])[0]
    sh = {
        'w_mod': f(inp['w_mod']),
        'bmodT': f(f(inp['b_mod']).reshape(2, 48, 128).transpose(2, 0, 1)),
        'ret_w_in': f(rw_ext),
        'ret_w_out': f(inp['ret_w_out'])[0],
        'lgf': f(inp['ret_log_decay_fwd']).reshape(1, 4),
        'lgb': f(inp['ret_log_decay_bwd']).reshape(1, 4),
        'att_w_qkv': f(aw_ext),
        'att_w_o': f(inp['att_w_o'])[0],
        'gq': f(np.stack([gqv, gqv[sw128]], axis=1)),
        'gk': f(np.stack([gkv, gkv[sw128]], axis=1)),
        'w_router': f(np.concatenate([f(inp['moe_w_group']), f(inp['moe_w_expert'])], axis=2)),
        'b_router': f(np.concatenate([f(inp['moe_b_group']), f(inp['moe_b_expert'])], axis=1)),
        'wg_r': f(f(inp['moe_w_gate']).reshape(2, 32, 8, 128, 512).transpose(0, 1, 3, 2, 4).reshape(2 * 32 * 128, 4096)),
        'wu_r': f(f(inp['moe_w_up']).reshape(2, 32, 8, 128, 512).transpose(0, 1, 3, 2, 4).reshape(2 * 32 * 128, 4096)),
        'wd_r': f(f(inp['moe_w_down']).reshape(2, 32, 4, 128, 1024).transpose(0, 1, 3, 2, 4).reshape(2 * 32 * 128, 4096)),
        'fgT': f(f(inp['final_norm_gain']).reshape(8, 128).T),
        'ret_tab': ret_tab,
        'att_tab': att_tab,
        'ident': np.eye(128, dtype=np.float32),
    }
    return sh


def _core_inputs(inp, sh, b0, nb):
    f = lambda a: np.ascontiguousarray(np.asarray(a, dtype=np.float32))
    cc = np.concatenate([f(inp['c'])[b0:b0 + nb], f(inp['c_ctx'])[None, :]], axis=0)
    m = dict(sh)
    m['x'] = f(inp['x'][b0:b0 + nb])
    m['ctx'] = f(inp['ctx'][b0:b0 + nb])
    m['cT'] = f(cc.reshape(nb + 1, 8, 128).transpose(2, 1, 0))
    return m


_PROG = {}


def kernel(**inputs):
    if 'full' not in _PROG:
        _PROG['full'] = build_program(NB_FULL)
    nc = _PROG['full']
    sh = _shared_inputs(inputs)
    in_maps = [_core_inputs(inputs, sh, c * NB_FULL, NB_FULL) for c in range(NCORES)]
    res = run_bass_kernel_spmd(nc, in_maps, core_ids=list(range(NCORES)))
    out = np.concatenate([np.asarray(r["out"], dtype=np.float32) for r in res.results], axis=0)
    return out
```

```python
import numpy as np
from contextlib import ExitStack
import concourse.bass as bass
import concourse.mybir as mybir
from concourse.bass_utils import run_bass_kernel_spmd

F32 = mybir.dt.float32
BF16 = mybir.dt.bfloat16
AF = mybir.ActivationFunctionType
ALU = mybir.AluOpType
AX = mybir.AxisListType

D = 1024
S = 2048
L = 256
T = S + L
NBLK = 5
NCORES = 8
BATCH = 32
NB_FULL = BATCH // NCORES
RET_IN = 6144
ATT_IN = 1536
BIG = 1.0e30


def blk_rng(blk):
    if blk < 4:
        return blk * 512, 512
    return 2048, 256


class KB:
    def __init__(self, nc, es):
        self.nc = nc
        self.eng = {'pe': nc.tensor, 'dve': nc.vector, 'act': nc.scalar, 'pool': nc.gpsimd, 'sp': nc.sync}
        self.sem = {k: es.enter_context(nc.semaphore("s_" + k)) for k in ('pe', 'dve', 'act', 'pool')}
        self.cnt = {k: 0 for k in self.sem}
        self.seen = {e: {} for e in self.eng}
        self.lastw = {}
        self.readers = {}
        self.dsem = {}
        self.dcnt = {}
        self.es = es
        self.bar_sem = es.enter_context(nc.semaphore("s_bar"))
        self.bar_cnt = 0
        self.lazy = set()
        self.scr = nc.dram_tensor("bar_scr", [2, 64], F32, kind="Internal").ap()
        self.bsrc = es.enter_context(nc.sbuf_tensor("bar_src", [1, 64], F32))
        self.op('dve', lambda: nc.vector.memset(self.bsrc[:], 0.0), writes=['bar_src'])

    def _semobj(self, key):
        if isinstance(key, tuple):
            return self.dsem[key[1]]
        return self.sem[key]

    _skip = None

    def _wait(self, e, key, val):
        if key == 'pe' and e == 'pe':
            return
        if key == self._skip:
            return
        if self.seen[e].get(key, 0) >= val:
            return
        self.eng[e].wait_ge(self._semobj(key), val)
        self.seen[e][key] = val

    def _deps(self, e, reads, writes):
        for r in reads:
            t = self.lastw.get(r)
            if t is not None:
                self._wait(e, t[0], t[1])
        for w in writes:
            t = self.lastw.get(w)
            if t is not None:
                self._wait(e, t[0], t[1])
            rd = self.readers.get(w)
            if rd:
                for k, v in rd.items():
                    self._wait(e, k, v)

    def _commit(self, tok, reads, writes):
        for w in writes:
            self.lastw[w] = tok
            self.readers[w] = {}
        for r in reads:
            self.readers.setdefault(r, {})[tok[0]] = tok[1]

    def op(self, e, fn, reads=(), writes=()):
        self._deps(e, reads, writes)
        ins = fn()
        self.cnt[e] += 1
        ins.then_inc(self.sem[e], 1)
        self._commit((e, self.cnt[e]), reads, writes)

    def dma(self, q, stream, out, in_, reads=(), writes=(), indirect=None, **kw):
        if stream not in self.dsem:
            self.dsem[stream] = self.es.enter_context(self.nc.semaphore("d_" + str(len(self.dsem))))
            self.dcnt[stream] = 0
        self._skip = ('dma', stream)
        self._deps(q, reads, writes)
        self._skip = None
        self.dcnt[stream] += 16
        if indirect is None:
            ins = self.eng[q].dma_start(out=out, in_=in_, **kw)
        elif indirect[0] == 'out':
            ins = self.eng[q].indirect_dma_start(out=out, out_offset=bass.IndirectOffsetOnAxis(ap=indirect[1], axis=0), in_=in_, in_offset=None)
        else:
            if len(indirect) > 2:
                ins = self.eng[q].indirect_dma_start(out=out, out_offset=None, in_=in_, in_offset=bass.IndirectOffsetOnAxis(ap=indirect[1], axis=0),
                                                     bounds_check=indirect[2], oob_is_err=False)
            else:
                ins = self.eng[q].indirect_dma_start(out=out, out_offset=None, in_=in_, in_offset=bass.IndirectOffsetOnAxis(ap=indirect[1], axis=0))
        ins.then_inc(self.dsem[stream], 16)
        self._commit((('dma', stream), self.dcnt[stream]), reads, writes)

    def barrier(self):
        sp = 'sp'
        for k in self.sem:
            if self.cnt[k]:
                self._wait(sp, k, self.cnt[k])
        for s, c in self.dcnt.items():
            if c and s not in self.lazy:
                self._wait(sp, ('dma', s), c)
        self.bar_cnt += 16
        self.eng[sp].dma_start(out=self.scr[1:2, :], in_=self.bsrc[:]).then_inc(self.bar_sem, 16)
        for e in ('pe', 'dve', 'act', 'pool', 'sp'):
            self.eng[e].wait_ge(self.bar_sem, self.bar_cnt)
            for k in self.sem:
                self.seen[e][k] = self.cnt[k]
            for s, c in self.dcnt.items():
                if s not in self.lazy:
                    self.seen[e][('dma', s)] = c
        self.lastw.clear()
        self.readers.clear()

    def finish(self):
        sp = 'sp'
        for k in self.sem:
            if self.cnt[k]:
                self._wait(sp, k, self.cnt[k])
        for s, c in self.dcnt.items():
            if c:
                self._wait(sp, ('dma', s), c)


def build_program(NB=NB_FULL, stop=None, dump_ctx=False):
    nc = bass.Bass("TRN2", target_bir_lowering=False)
    NM = NB + 1

    def din(name, shape):
        return nc.dram_tensor(name, list(shape), F32, kind="ExternalInput").ap()

    x_d = din("x", [NB, S, D])
    ctx_d = din("ctx", [NB, L, D])
    cT_d = din("cT", [128, 8, NM])
    wmod_d = din("w_mod", [2, D, 6 * D])
    bmod_d = din("bmodT", [128, 2, 48])
    rwin_d = din("ret_w_in", [D, RET_IN + 2048])
    rwout_d = din("ret_w_out", [2048, D])
    lgf_d = din("lgf", [1, 4])
    lgb_d = din("lgb", [1, 4])
    aw_d = din("att_w_qkv", [D, ATT_IN + 1280])
    awo_d = din("att_w_o", [D, D])
    gq_d = din("gq", [128, 2])
    gk_d = din("gk", [128, 2])
    wr_d = din("w_router", [2, D, 36])
    br_d = din("b_router", [2, 36])
    wgr_d = din("wg_r", [2 * 32 * 128, 4096])
    wur_d = din("wu_r", [2 * 32 * 128, 4096])
    wdr_d = din("wd_r", [2 * 32 * 128, 4096])
    NSLOT = 50 * 256
    w16_d = [nc.dram_tensor("w16_%d" % i, [2 * 32 * 128, 4096], BF16, kind="Internal").ap() for i in range(3)]
    dec_d = nc.dram_tensor("dec_scr", [4, 128, 6144], BF16, kind="Internal").ap()
    Xs_d = nc.dram_tensor("Xs_scr", [NSLOT, D], BF16, kind="Internal").ap()
    Ys_d = nc.dram_tensor("Ys_scr", [NSLOT, D], F32, kind="Internal").ap()
    fg_d = din("fgT", [128, 8])
    rtab_d = din("ret_tab", [128, 192])
    atab_d = din("att_tab", [128, 2, 64])
    ident_d = din("ident", [128, 128])
    out_d = nc.dram_tensor("out", [NB, S, D], F32, kind="ExternalOutput").ap()
    if dump_ctx:
        outc_d = nc.dram_tensor("outc", [NB, L, D], F32, kind="ExternalOutput").ap()

    with ExitStack() as es:
        kb = KB(nc, es)
        bcw = nc.gpsimd.to_reg(2 * 32 * 128 - 1)
        for l_ in range(2):
            kb.lazy.add('pc%d' % l_)
            for r in range(32):
                r0 = l_ * 4096 + r * 128
                for i_, src_ in enumerate((wgr_d, wur_d, wdr_d)):
                    kb.dma('pool', 'pc%d' % l_, w16_d[i_][r0:r0 + 128, :], src_[r0:r0 + 128, :])
        V, A, P, PE = 'dve', 'act', 'pool', 'pe'

        uid = [0]

        def sb(name, shape, dt, st=es):
            uid[0] += 1
            return st.enter_context(nc.sbuf_tensor("%s_%d" % (name, uid[0]), list(shape), dt))

        def pst(name, shape, dt, st):
            uid[0] += 1
            return st.enter_context(nc.psum_tensor("%s_%d" % (name, uid[0]), list(shape), dt))

        xT = sb("xT", [128, 8, T], F32)
        hT = sb("hT", [128, 8, T], BF16)
        WA = sb("WA", [128, 4, 4096], BF16)
        identf = sb("identf", [128, 128], F32)
        identb = sb("identb", [128, 128], BF16)
        onesb = sb("onesb", [128, 128], BF16)
        mod = sb("mod", [128, 2, 48, NM], F32)
        bmod = sb("bmod", [128, 2, 48], F32)
        rtab = sb("rtab", [128, 192], F32)
        atab = sb("atab", [128, 2, 64], F32)
        atq = sb("atq", [128, 2, 64], F32)
        atk = sb("atk", [128, 2, 64], F32)
        gq = sb("gq_s", [128, 2], F32)
        gk = sb("gk_s", [128, 2], F32)
        lgf = sb("lgf_s", [128, 4], F32)
        lgb = sb("lgb_s", [128, 4], F32)
        nlgb = sb("nlgb_s", [128, 4], F32)
        fg = sb("fg_s", [128, 8], F32)
        triU = sb("triU", [128, 128], F32)
        onesf = sb("onesf", [128, 128], F32)
        UI = sb("UI", [32, 32], F32)
        thr18 = sb("thr18", [32, 18], F32)
        thr68 = sb("thr68", [128, 68], F32)
        iop = sb("iop", [128, 1], F32)
        epsn = sb("epsn", [128, 1], F32)
        epsh = sb("epsh", [128, 1], F32)

        def xk(dc, blk):
            return ('xT', dc, blk)

        def hk(dc, blk):
            return ('hT', dc, blk)

        def xks(blk):
            return [xk(dc, blk) for dc in range(8)]

        def hks(blk):
            return [hk(dc, blk) for dc in range(8)]

        with ExitStack() as ps:
            scT = sb("scT", [128, 8, NM], F32, ps)
            wst = [sb("wst%d" % i, [128, 8, 512], F32, ps) for i in range(2)]
            pm = [pst("pm%d" % i, [128, 4, NM], F32, ps) for i in range(2)]
            kb.dma('sp', 'c0', identf[:], ident_d[:], writes=['identf'])
            kb.dma('sp', 'c1', scT[:], cT_d[:], writes=['scT'])
            kb.dma('sp', 'c2', bmod[:], bmod_d[:], writes=['bmod'])
            kb.dma('sp', 'c3', rtab[:], rtab_d[:], writes=['rtab'])
            kb.dma('sp', 'c4', atab[:], atab_d[:], writes=['atab'])
            kb.dma('sp', 'c5', gq[:], gq_d[:], writes=['gq'])
            kb.dma('sp', 'c6', gk[:], gk_d[:], writes=['gk'])
            kb.dma('sp', 'c7', lgf[:], lgf_d.partition_broadcast(128), writes=['lgf'])
            kb.dma('sp', 'c8', lgb[:], lgb_d.partition_broadcast(128), writes=['lgb'])
            kb.dma('sp', 'c9', fg[:], fg_d[:], writes=['fg'])
            kb.op(V, lambda: nc.vector.tensor_copy(identb[:], identf[:]), reads=['identf'], writes=['identb'])
            kb.op(V, lambda: nc.vector.memset(onesb[:], 1.0), writes=['onesb'])
            kb.op(V, lambda: nc.vector.memset(epsn[:], 1e-6), writes=['epsn'])
            kb.op(V, lambda: nc.vector.memset(onesf[:], 1.0), writes=['onesf'])
            kb.op(P, lambda: nc.gpsimd.iota(triU[:], [[1, 128]], base=0, channel_multiplier=-1, allow_small_or_imprecise_dtypes=True), writes=['triU'])
            kb.op(V, lambda: nc.vector.tensor_scalar(UI[:], triU[0:32, 0:32], 0.0, None, ALU.is_ge), reads=['triU'], writes=['UI'])
            kb.op(V, lambda: nc.vector.tensor_scalar(triU[:], triU[:], 0.0, None, ALU.is_gt), reads=['triU', 'UI'], writes=['triU'])
            kb.op(P, lambda: nc.gpsimd.iota(thr18[:], [[256, 18]], base=0, channel_multiplier=0, allow_small_or_imprecise_dtypes=True), writes=['thr18'])
            kb.op(P, lambda: nc.gpsimd.iota(thr68[:], [[256, 68]], base=0, channel_multiplier=0, allow_small_or_imprecise_dtypes=True), writes=['thr68'])
            kb.op(P, lambda: nc.gpsimd.iota(iop[:], [[0, 1]], base=0, channel_multiplier=1, allow_small_or_imprecise_dtypes=True), writes=['iop'])
            kb.op(V, lambda: nc.vector.memset(epsh[:], 1e-5), writes=['epsh'])
            kb.op(V, lambda: nc.vector.tensor_scalar(nlgb[:], lgb[:], -1.0, None, ALU.mult), reads=['lgb'], writes=['nlgb'])
            for tabo, g_, nm in ((atq, gq, 'atq'), (atk, gk, 'atk')):
                kb.op(V, lambda tabo=tabo, g_=g_: nc.vector.tensor_scalar(tabo[:, 0, :], atab[:, 0, :], g_[:, 0:1], None, ALU.mult),
                      reads=['atab', 'gq', 'gk'], writes=[nm + '0'])
                kb.op(V, lambda tabo=tabo, g_=g_: nc.vector.tensor_scalar(tabo[:, 1, :], atab[:, 1, :], g_[:, 1:2], None, ALU.mult),
                      reads=['atab', 'gq', 'gk'], writes=[nm + '1'])
            kb.op(A, lambda: nc.scalar.activation(scT[:], scT[:], AF.Silu), reads=['scT'], writes=['scT'])
            zt = sb("zt", [128, D], BF16, ps)
            kb.op(V, lambda: nc.vector.memset(zt[:], 0.0), writes=['zt'])
            for r in range(NSLOT // 128):
                kb.dma('sp', 'xz', Xs_d[r * 128:(r + 1) * 128, :], zt[:], reads=['zt'])
            strip = sb("strip_s", [128, 3968], BF16, ps)
            cstrip = sb("cstrip_s", [128, 2176], BF16, ps)
            it0 = sb("it0", [128, 496], F32, ps)
            it1 = sb("it1", [128, 496], F32, ps)
            it2 = sb("it2", [128, 496], F32, ps)
            for h in range(4):
                for pcs in range(8):
                    u0 = pcs * 496
                    kb.op(P, lambda u0=u0: nc.gpsimd.iota(it0[:, :496], [[1, 496]], base=u0 - 1920, channel_multiplier=-1,
                                                          allow_small_or_imprecise_dtypes=True), writes=['it0'])
                    kb.op(V, lambda: nc.vector.tensor_scalar(it1[:, :496], it0[:, :496], 0.0, None, ALU.max), reads=['it0'], writes=['it1'])
                    kb.op(V, lambda: nc.vector.tensor_scalar(it2[:, :496], it0[:, :496], 0.0, None, ALU.min), reads=['it0'], writes=['it2'])
                    kb.op(A, lambda h=h: nc.scalar.activation(it1[:, :496], it1[:, :496], AF.Exp, scale=lgf[:, h:h + 1]), reads=['it1', 'lgf'], writes=['it1'])
                    kb.op(A, lambda h=h: nc.scalar.activation(it2[:, :496], it2[:, :496], AF.Exp, scale=nlgb[:, h:h + 1]), reads=['it2', 'nlgb'], writes=['it2'])
                    kb.op(V, lambda: nc.vector.tensor_tensor(it1[:, :496], it1[:, :496], it2[:, :496], ALU.add), reads=['it1', 'it2'], writes=['it1'])
                    kb.op(V, lambda: nc.vector.tensor_scalar(it2[:, :496], it0[:, :496], 0.0, None, ALU.is_equal), reads=['it0', 'it1'], writes=['it2'])
                    kb.op(V, lambda: nc.vector.tensor_tensor(it1[:, :496], it1[:, :496], it2[:, :496], ALU.add), reads=['it2', 'it1'], writes=['it1'])
                    kb.op(V, lambda u0=u0: nc.vector.tensor_scalar(strip[:, u0:u0 + 496], it1[:, :496], -1.0, 1.0 / 16, ALU.add, ALU.mult),
                          reads=['it1'], writes=['strip'])
                for pcs in range(8):
                    u0 = pcs * 272
                    kb.op(P, lambda u0=u0: nc.gpsimd.iota(it0[:, :272], [[1, 272]], base=u0 + 128, channel_multiplier=-1,
                                                          allow_small_or_imprecise_dtypes=True), writes=['it0'])
                    kb.op(P, lambda u0=u0: nc.gpsimd.iota(it2[:, :272], [[-1, 272]], base=2176 - u0, channel_multiplier=1,
                                                          allow_small_or_imprecise_dtypes=True), writes=['it2'])
                    kb.op(A, lambda h=h: nc.scalar.activation(it0[:, :272], it0[:, :272], AF.Exp, scale=lgf[:, h:h + 1]), reads=['it0', 'lgf'], writes=['it0'])
                    kb.op(A, lambda h=h: nc.scalar.activation(it2[:, :272], it2[:, :272], AF.Exp, scale=lgb[:, h:h + 1]), reads=['it2', 'lgb'], writes=['it2'])
                    kb.op(V, lambda: nc.vector.tensor_tensor(it0[:, :272], it0[:, :272], it2[:, :272], ALU.add), reads=['it0', 'it2'], writes=['it0'])
                    kb.op(V, lambda u0=u0: nc.vector.tensor_scalar(cstrip[:, u0:u0 + 272], it0[:, :272], 1.0 / 16, None, ALU.mult),
                          reads=['it0'], writes=['cstrip'])
                kb.dma('sp', 'dec0', dec_d[h, :, 0:3968], strip[:], reads=['strip'])
                kb.dma('sp', 'dec1', dec_d[h, :, 3968:6144], cstrip[:], reads=['cstrip'])
            pc = 0
            for l in range(2):
                for piece in range(12):
                    s_ = pc % 2
                    kb.dma('sp', 'wst%d' % s_, wst[s_][:],
                           wmod_d[l, :, piece * 512:(piece + 1) * 512].rearrange("(kc p) c -> p kc c", p=128),
                           writes=['wst%d' % s_])
                    for f4 in range(4):
                        for kc in range(8):
                            kb.op(PE, lambda f4=f4, kc=kc, s_=s_: nc.tensor.matmul(
                                pm[s_][:, f4, :], wst[s_][:, kc, f4 * 128:(f4 + 1) * 128], scT[:, kc, :],
                                start=(kc == 0), stop=(kc == 7)),
                                reads=['wst%d' % s_, 'scT'], writes=['pm%d' % s_])
                    for f4 in range(4):
                        fc = piece * 4 + f4
                        is_sc = (fc // 8) in (1, 4)
                        kb.op(V, lambda f4=f4, fc=fc, s_=s_, l=l, is_sc=is_sc: nc.vector.tensor_scalar(
                            mod[:, l, fc, :], pm[s_][:, f4, :], bmod[:, l, fc:fc + 1], 1.0 if is_sc else 0.0,
                            ALU.add, ALU.add),
                            reads=['pm%d' % s_, 'bmod'], writes=[('mod', l, fc)])
                    pc += 1
        kb.barrier()

        def modc(l, which, dc, n):
            base = which * 24
            return (mod[:, l, base + dc, n:n + 1], mod[:, l, base + 8 + dc, n:n + 1], mod[:, l, base + 16 + dc, n:n + 1])

        def rms_block(blk, sqb, pss, rt, R, pskey):
            t0, W = blk_rng(blk)
            for dc in range(8):
                kb.op(A, lambda dc=dc: nc.scalar.activation(sqb[:, dc, :W], xT[:, dc, t0:t0 + W], AF.Square),
                      reads=[xk(dc, blk)], writes=[('sqb', dc)])
            for dc in range(8):
                kb.op(PE, lambda dc=dc: nc.tensor.matmul(pss[:, :W], onesb[:], sqb[:, dc, :W], start=(dc == 0), stop=(dc == 7)),
                      reads=[('sqb', dc)], writes=[pskey])
            kb.op(A, lambda: nc.scalar.activation(rt[:, :W], pss[:, :W], AF.Ln, bias=epsn[:, 0:1], scale=1.0 / D),
                  reads=[pskey], writes=['rt'])
            kb.op(A, lambda: nc.scalar.activation(R[:, :W], rt[:, :W], AF.Exp, scale=-0.5), reads=['rt'], writes=['R'])

        def load_x(b):
            with ExitStack() as ps:
                stg = [sb("xstg%d" % i, [128, D], F32, ps) for i in range(4)]
                ptr = [pst("ptr%d" % i, [128, 4, 128], F32, ps) for i in range(4)]
                for tc in range(18):
                    s_ = tc % 4
                    src = x_d[b, tc * 128:(tc + 1) * 128, :] if tc < 16 else ctx_d[b, (tc - 16) * 128:(tc - 15) * 128, :]
                    kb.dma('sp', 'xstg%d' % s_, stg[s_][:], src, writes=['xstg%d' % s_])
                    blk = tc // 4
                    for hf in range(2):
                        pi = (tc % 2) * 2 + hf
                        for d4 in range(4):
                            dc = hf * 4 + d4
                            kb.op(PE, lambda dc=dc, d4=d4, pi=pi, s_=s_: nc.tensor.transpose(
                                ptr[pi][:, d4, :], stg[s_][:, dc * 128:(dc + 1) * 128], identf[:]),
                                reads=['xstg%d' % s_], writes=['ptr%d' % pi])
                        e_ = A if hf == 0 else V
                        f_ = (lambda pi=pi, hf=hf, tc=tc: nc.scalar.copy(xT[:, hf * 4:(hf + 1) * 4, tc * 128:(tc + 1) * 128], ptr[pi][:])) if hf == 0 else \
                             (lambda pi=pi, hf=hf, tc=tc: nc.vector.tensor_copy(xT[:, hf * 4:(hf + 1) * 4, tc * 128:(tc + 1) * 128], ptr[pi][:]))
                        kb.op(e_, f_, reads=['ptr%d' % pi], writes=[('xTl', hf * 4 + d4, tc) for d4 in range(4)])
            kb.barrier()

        def modulate(l, which, b, nblk, ps, router=None):
            sqb = sb("sqb", [128, 8, 512], BF16, ps)
            rt = sb("rt", [128, 512], F32, ps)
            R = sb("R", [128, 512], F32, ps)
            tmp = [sb("mtmp%d" % i, [128, 512], F32, ps) for i in range(2)]
            pss = pst("pss", [128, 512], F32, ps)
            for blk in range(nblk):
                t0, W = blk_rng(blk)
                n = b if blk < 4 else NB
                rms_block(blk, sqb, pss, rt, R, 'pss')
                for dc in range(8):
                    sh, sc, _ = modc(l, which, dc, n)
                    tm = tmp[dc % 2]
                    kb.op(V, lambda dc=dc, tm=tm: nc.vector.tensor_tensor(tm[:, :W], xT[:, dc, t0:t0 + W], R[:, :W], ALU.mult),
                          reads=[xk(dc, blk), 'R'], writes=['mtmp%d' % (dc % 2)])
                    if router is None:
                        kb.op(A, lambda dc=dc, tm=tm, sh=sh, sc=sc: nc.scalar.activation(
                            hT[:, dc, t0:t0 + W], tm[:, :W], AF.Identity, bias=sh, scale=sc),
                            reads=['mtmp%d' % (dc % 2)], writes=[hk(dc, blk)])
                    else:
                        h2f = router['h2f']
                        kb.op(A, lambda dc=dc, tm=tm, sh=sh, sc=sc: nc.scalar.activation(
                            h2f[:, dc, :W], tm[:, :W], AF.Identity, bias=sh, scale=sc),
                            reads=['mtmp%d' % (dc % 2)], writes=[('h2f', dc)])
                        kb.op(P, lambda dc=dc: nc.gpsimd.tensor_copy(hT[:, dc, t0:t0 + W], h2f[:, dc, :W]),
                              reads=[('h2f', dc)], writes=[hk(dc, blk)])
                if router is not None:
                    router['fn'](blk)

        wslot_rr = [0]

        def wload(slot, dst_ap, src_ap, keys=None):
            kb.dma('pool', 'W%d' % slot, dst_ap, src_ap, writes=keys or ['W%d' % slot])

        def retention(b):
            with ExitStack() as ps:
                modulate(0, 0, b, 5, ps)
            kb.barrier()
            with ExitStack() as ps:
                kT = sb("kT", [128, 2, T], BF16, ps)
                qT = sb("qT", [128, 2, 512], BF16, ps)
                vh = sb("vh", [128, 18, 512], BF16, ps)
                strip = sb("strip", [128, 3968], BF16, ps)
                cstrip = sb("cstrip", [128, 2176], BF16, ps)
                PT = [sb("PT%d" % i, [128, 512], BF16, ps) for i in range(2)]
                rA = sb("rA", [128, 512], F32, ps)
                rB = sb("rB", [128, 512], F32, ps)
                zb = [sb("zb%d" % i, [128, 512], BF16, ps) for i in range(2)]
                zT = sb("zT", [128, 4, 512], BF16, ps)
                dg = sb("dg", [128, 4, 128], BF16, ps)
                st6 = sb("st6", [128, 4, 6], F32, ps)
                mv = sb("mv", [128, 4, 2], F32, ps)
                rs = sb("rs", [128, 4], F32, ps)
                psS = [pst("psS%d" % i, [128, 512], F32, ps) for i in range(2)]
                psY = [pst("psY%d" % i, [128, 512], F32, ps) for i in range(4)]
                psM = pst("psM", [128, 512], F32, ps)
                psTb = pst("psTb", [128, 4, 256], BF16, ps)
                psM2 = psTb[:].rearrange("p a b -> p (a b)").bitcast(F32)

                def wslot():
                    s_ = wslot_rr[0] % 4
                    wslot_rr[0] += 1
                    return s_

                c_row, s_row = rtab[:, 0:32], rtab[:, 32:64]
                c_col, s_col = rtab[:, 64:128], rtab[:, 128:192]

                def rope_tabs(j, t0, W):
                    nr = W // 64
                    if j == 0:
                        r0 = t0 // 64
                        return (c_row[:, r0:r0 + nr].unsqueeze(2).to_broadcast([128, nr, 64]),
                                s_row[:, r0:r0 + nr].unsqueeze(2).to_broadcast([128, nr, 64]))
                    return (c_col.unsqueeze(1).to_broadcast([128, nr, 64]),
                            s_col.unsqueeze(1).to_broadcast([128, nr, 64]))

                def v3(ap, W):
                    return ap.rearrange("p (a b) -> p a b", b=64)

                def proj_rope(ws, dstT, dkey, blk, alt=0):
                    t0, W = blk_rng(blk)
                    wv_ = WA[:, ws, :].rearrange("p (s k c) -> p s k c", s=2, k=8)
                    for j in range(2):
                        if alt and j == 1:
                            pA, pAk, pB, pBk = psS[0], 'psS0', psS[1], 'psS1'
                        else:
                            pA, pAk, pB, pBk = psM, 'psM', psM2, 'psTb'
                        for kc in range(8):
                            kb.op(PE, lambda kc=kc, j=j: nc.tensor.matmul(pA[:, :W], wv_[:, 0, kc, j * 128:(j + 1) * 128], hT[:, kc, t0:t0 + W],
                                                                         start=(kc == 0), stop=(kc == 7)),
                                  reads=['W%d' % ws] + hks(blk), writes=[pAk])
                        if blk < 4:
                            for kc in range(8):
                                kb.op(PE, lambda kc=kc, j=j: nc.tensor.matmul(pB[:, :W], wv_[:, 1, kc, j * 128:(j + 1) * 128], hT[:, kc, t0:t0 + W],
                                                                             start=(kc == 0), stop=(kc == 7)),
                                      reads=['W%d' % ws] + hks(blk), writes=[pBk])
                            ct, st_ = rope_tabs(j, t0, W)
                            kb.op(V, lambda ct=ct: nc.vector.tensor_tensor(v3(rA[:, :W], W), v3(pA[:, :W], W), ct, ALU.mult),
                                  reads=[pAk], writes=['rA'])
                            kb.op(V, lambda st_=st_: nc.vector.tensor_tensor(v3(rB[:, :W], W), v3(pB[:, :W], W), st_, ALU.mult),
                                  reads=[pBk], writes=['rB'])
                            kb.op(V, lambda j=j: nc.vector.tensor_tensor(dstT[:, j, t0 - dkey[1]:t0 - dkey[1] + W], rA[:, :W], rB[:, :W], ALU.add),
                                  reads=['rA', 'rB'], writes=[(dkey[0], j, blk)])
                        else:
                            kb.op(A, lambda j=j: nc.scalar.copy(dstT[:, j, t0 - dkey[1]:t0 - dkey[1] + W], pA[:, :W]),
                                  reads=[pAk], writes=[(dkey[0], j, blk)])

                for h in range(4):
                    kb.dma('sp', 'dec0', strip[:], dec_d[h, :, 0:3968], writes=['strip'])
                    kb.dma('sp', 'dec1', cstrip[:], dec_d[h, :, 3968:6144], writes=['cstrip'])
                    ws = wslot()
                    wdst = WA[:, ws, :].rearrange("p (s k c) -> p s k c", s=2, k=8)
                    c0 = 1024 + h * 256
                    wload(ws, wdst[:, 0], rwin_d[:, c0:c0 + 256].rearrange("(kc p) c -> p kc c", p=128))
                    wload(ws, wdst[:, 1], rwin_d[:, RET_IN + c0:RET_IN + c0 + 256].rearrange("(kc p) c -> p kc c", p=128))
                    for blk in range(5):
                        proj_rope(ws, kT, ('kT', 0), blk, alt=1)
                    ws = wslot()
                    wv_ = WA[:, ws, :].rearrange("p (k c) -> p k c", k=8)
                    c0 = 2048 + h * 512
                    wload(ws, wv_, rwin_d[:, c0:c0 + 512].rearrange("(kc p) c -> p kc c", p=128))
                    for tc in range(18):
                        blk = tc // 4
                        pv, pvk = (psM, 'psM') if tc % 2 == 0 else (psM2, 'psTb')
                        for kc in range(8):
                            kb.op(PE, lambda kc=kc, tc=tc, pv=pv: nc.tensor.matmul(pv[:], hT[:, kc, tc * 128:(tc + 1) * 128], wv_[:, kc, :],
                                                                                  start=(kc == 0), stop=(kc == 7)),
                                  reads=['W%d' % ws] + hks(blk), writes=[pvk])
                        if tc % 2 == 0:
                            kb.op(A, lambda tc=tc, pv=pv: nc.scalar.copy(vh[:, tc, :], pv[:]), reads=[pvk], writes=[('vh', tc)])
                        else:
                            kb.op(V, lambda tc=tc, pv=pv: nc.vector.tensor_copy(vh[:, tc, :], pv[:]), reads=[pvk], writes=[('vh', tc)])
                    wsq = wslot()
                    wdq = WA[:, wsq, :].rearrange("p (s k c) -> p s k c", s=2, k=8)
                    c0 = h * 256
                    wload(wsq, wdq[:, 0], rwin_d[:, c0:c0 + 256].rearrange("(kc p) c -> p kc c", p=128))
                    wload(wsq, wdq[:, 1], rwin_d[:, RET_IN + c0:RET_IN + c0 + 256].rearrange("(kc p) c -> p kc c", p=128))
                    wsg = wslot()
                    wg_ = WA[:, wsg, :].rearrange("p (k c) -> p k c", k=8)
                    c0 = 4096 + h * 512
                    wload(wsg, wg_, rwin_d[:, c0:c0 + 512].rearrange("(kc p) c -> p kc c", p=128))
                    wso = wslot()
                    wo_ = WA[:, wso, :].rearrange("p (k c) -> p k c", k=4)
                    wload(wso, wo_, rwout_d[h * 512:(h + 1) * 512, :].rearrange("(hc p) d -> p hc d", p=128))
                    for qb in range(5):
                        t0, W = blk_rng(qb)
                        nts = W // 128
                        proj_rope(wsq, qT, ('qT', t0), qb)
                        scs = list(range(18)) if qb < 4 else [16, 17]
                        def Sm(si):
                            sc = scs[si]
                            pS = psS[si % 2]
                            for j in range(2):
                                kb.op(PE, lambda j=j: nc.tensor.matmul(pS[:, :W], kT[:, j, sc * 128:(sc + 1) * 128], qT[:, j, :W],
                                                                      start=(j == 0), stop=(j == 1)),
                                      reads=[('kT', j, sc // 4), ('qT', 0, qb), ('qT', 1, qb)], writes=['psS%d' % (si % 2)])
                            if sc < 16:
                                off = t0 - sc * 128 + 1920
                                dk, dkey = strip[:, off:off + W], 'strip'
                            elif qb < 4:
                                off = t0 - 128 * (sc - 16) + 128
                                dk, dkey = cstrip[:, off:off + W], 'cstrip'
                            else:
                                off = 1920 - 128 * (sc - 16)
                                dk, dkey = strip[:, off:off + W], 'strip'
                            pt = PT[si % 2]
                            kb.op(V, lambda: nc.vector.tensor_tensor(pt[:, :W], pS[:, :W], dk, ALU.mult),
                                  reads=['psS%d' % (si % 2), dkey], writes=['PT%d' % (si % 2)])

                        def PVm(si):
                            sc = scs[si]
                            pt = PT[si % 2]
                            for ts in range(nts):
                                kb.op(PE, lambda ts=ts: nc.tensor.matmul(
                                    psY[ts][:], pt[:, ts * 128:(ts + 1) * 128], vh[:, sc, :], start=(si == 0), stop=(si == len(scs) - 1)),
                                    reads=['PT%d' % (si % 2), ('vh', sc)], writes=['psY%d' % ts])

                        Sm(0)
                        for si in range(len(scs)):
                            if si + 1 < len(scs):
                                Sm(si + 1)
                            PVm(si)
                        n = b if qb < 4 else NB
                        sgs = [rA, rB]
                        for ts in range(nts):
                            kb.op(V, lambda ts=ts: nc.vector.bn_stats(st6[:, ts, :], psY[ts][:]), reads=['psY%d' % ts], writes=[('st6', ts)])
                            kb.op(V, lambda ts=ts: nc.vector.bn_aggr(mv[:, ts, :], st6[:, ts, :]), reads=[('st6', ts)], writes=[('mv', ts)])
                        kb.op(A, lambda: nc.scalar.activation(rs[:, :nts], mv[:, :nts, 1], AF.Sqrt, bias=epsh[:, 0:1], scale=1.0),
                              reads=[('mv', i) for i in range(nts)], writes=['rs'])
                        kb.op(V, lambda: nc.vector.reciprocal(rs[:, :nts], rs[:, :nts]), reads=['rs'], writes=['rs'])
                        for ts in range(nts):
                            kb.op(P, lambda ts=ts: nc.gpsimd.tensor_scalar(dg[:, ts, :], identb[:], rs[:, ts:ts + 1], None, ALU.mult),
                                  reads=['rs'], writes=[('dg', ts)])

                        def gproj(ts):
                            tk0 = t0 + ts * 128
                            pg = psS[ts % 2]
                            for kc in range(8):
                                kb.op(PE, lambda kc=kc: nc.tensor.matmul(pg[:], hT[:, kc, tk0:tk0 + 128], wg_[:, kc, :], start=(kc == 0), stop=(kc == 7)),
                                      reads=['W%d' % wsg] + hks(qb), writes=['psS%d' % (ts % 2)])
                            sg_ = sgs[ts % 2]
                            sk = 'rA' if ts % 2 == 0 else 'rB'
                            zb_ = zb[ts % 2]
                            kb.op(A, lambda: nc.scalar.activation(sg_[:], pg[:], AF.Silu), reads=['psS%d' % (ts % 2)], writes=[sk])
                            kb.op(V, lambda: nc.vector.scalar_tensor_tensor(zb_[:], psY[ts][:], mv[:, ts, 0:1], sg_[:], ALU.subtract, ALU.mult),
                                  reads=['psY%d' % ts, ('mv', ts), sk], writes=['zb%d' % (ts % 2)])

                        def ztrans(ts):
                            zb_ = zb[ts % 2]
                            pz = psM2.rearrange("p (a b) -> p a b", b=128)
                            for hc in range(4):
                                kb.op(PE, lambda hc=hc: nc.tensor.matmul(pz[:, hc, :], zb_[:, hc * 128:(hc + 1) * 128], dg[:, ts, :], start=True, stop=True),
                                      reads=['zb%d' % (ts % 2), ('dg', ts)], writes=['psTb'])
                            kb.op(A, lambda: nc.scalar.copy(zT[:, :, ts * 128:(ts + 1) * 128], pz[:]), reads=['psTb'], writes=['zT'])

                        gproj(0)
                        for ts in range(nts):
                            if ts + 1 < nts:
                                gproj(ts + 1)
                            ztrans(ts)
                        for dc in range(8):
                            po, pok = (psM, 'psM') if dc % 2 == 0 else (psM2, 'psTb')
                            for hc in range(4):
                                kb.op(PE, lambda hc=hc, dc=dc, po=po: nc.tensor.matmul(po[:, :W], wo_[:, hc, dc * 128:(dc + 1) * 128], zT[:, hc, :W],
                                                                                      start=(hc == 0), stop=(hc == 3)),
                                      reads=['W%d' % wso, 'zT'], writes=[pok])
                            g1 = modc(0, 0, dc, n)[2]
                            kb.op(V, lambda dc=dc, g1=g1, po=po: nc.vector.scalar_tensor_tensor(
                                xT[:, dc, t0:t0 + W], po[:, :W], g1, xT[:, dc, t0:t0 + W], ALU.mult, ALU.add),
                                reads=[pok, xk(dc, qb)], writes=[xk(dc, qb)])
            kb.barrier()

        def moe(l, b):
            nblk = 5 if l == 0 else 4
            nch = 18 if l == 0 else 16
            NBK = nch + 32
            with ExitStack() as ps:
                wr = sb("wr", [128, 8, 36], F32, ps)
                br = sb("br", [128, 36], F32, ps)
                gwall = sb("gwall", [128, 18, 32], F32, ps)
                Mall = sb("Mall", [128, 18, 32], F32, ps)
                Call = sb("Call", [128, 18, 32], F32, ps)
                Macc = sb("Macc", [128, 32], F32, ps)
                base_rep = sb("base_rep", [128, 32], F32, ps)
                idxw = sb("idxw", [128, 68], mybir.dt.int32, ps)
                dA = sb("dA", [128, 18], mybir.dt.int32, ps)
                dB = sb("dB", [128, 18], mybir.dt.int32, ps)
                gAB = sb("gAB", [128, 2, 18], F32, ps)
                kb.dma('sp', 'c0', wr[:], wr_d[l].rearrange("(kc p) n -> p kc n", p=128), writes=['wr'])
                kb.dma('sp', 'c1', br[:], br_d[l:l + 1, :].partition_broadcast(128), writes=['br'])
                kb.op(V, lambda: nc.vector.memset(Macc[:], 0.0), writes=['Macc'])
                with ExitStack() as ps2:
                    h2f = sb("h2f", [128, 8, 512], F32, ps2)
                    Lg = sb("Lg", [128, 4, 36], F32, ps2)
                    EM = sb("EM", [128, 4, 32], F32, ps2)
                    EM2 = sb("EM2", [128, 4, 32], F32, ps2)
                    pe_ = sb("pe_", [128, 4, 32], F32, ps2)
                    gx = sb("gx", [128, 4, 4], F32, ps2)
                    gm = sb("gm", [128, 4, 4], F32, ps2)
                    c4 = sb("c4", [128, 6, 4], F32, ps2)
                    msum = sb("msum", [128, 32], F32, ps2)
                    psL = pst("psL", [128, 4, 36], F32, ps2)
                    psC = pst("psC", [128, 4, 32], F32, ps2)

                    def router_fn(blk):
                        t0, W = blk_rng(blk)
                        nts = W // 128
                        tc0 = t0 // 128
                        gw4 = gwall[:, tc0:tc0 + nts, :]
                        M4 = Mall[:, tc0:tc0 + nts, :]
                        L4 = Lg[:, :nts, :]
                        G4 = L4[:, :, 0:4]
                        E4 = EM[:, :nts, :]
                        E24 = EM2[:, :nts, :]
                        P4 = pe_[:, :nts, :]

                        def bc(col, k):
                            return col.unsqueeze(2).to_broadcast([128, nts, k])

                        for ts in range(nts):
                            for kc in range(8):
                                kb.op(PE, lambda kc=kc, ts=ts: nc.tensor.matmul(psL[:, ts, :], h2f[:, kc, ts * 128:(ts + 1) * 128], wr[:, kc, :],
                                                                               start=(kc == 0), stop=(kc == 7)),
                                      reads=['wr'] + [('h2f', dc) for dc in range(8)], writes=['psL'])
                        kb.op(V, lambda: nc.vector.tensor_tensor(L4, psL[:, :nts, :], br[:].unsqueeze(1).to_broadcast([128, nts, 36]), ALU.add),
                              reads=['psL', 'br'], writes=['Lg'])
                        kb.op(V, lambda: nc.vector.reduce_max(c4[:, 0, :nts], G4, AX.X), reads=['Lg'], writes=['c4a'])
                        kb.op(V, lambda: nc.vector.tensor_tensor(gx[:, :nts, :], G4, bc(c4[:, 0, :nts], 4), ALU.subtract), reads=['Lg', 'c4a'], writes=['gx'])
                        kb.op(A, lambda: nc.scalar.activation(gx[:, :nts, :], gx[:, :nts, :], AF.Exp), reads=['gx'], writes=['gx'])
                        kb.op(V, lambda: nc.vector.reduce_sum(c4[:, 1, :nts], gx[:, :nts, :], AX.X), reads=['gx'], writes=['c4b'])
                        kb.op(V, lambda: nc.vector.tensor_tensor(gm[:, :nts, :], G4, bc(c4[:, 0, :nts], 4), ALU.is_ge), reads=['Lg', 'c4a'], writes=['gm'])
                        kb.op(V, lambda: nc.vector.tensor_scalar(gm[:, :nts, :], gm[:, :nts, :], -1.0, BIG, ALU.add, ALU.mult), reads=['gm'], writes=['gm'])
                        kb.op(V, lambda: nc.vector.tensor_tensor(E4.rearrange("p t (g e) -> p t g e", e=8),
                                                                 L4[:, :, 4:36].rearrange("p t (g e) -> p t g e", e=8),
                                                                 gm[:, :nts, :].unsqueeze(3).to_broadcast([128, nts, 4, 8]), ALU.add),
                              reads=['Lg', 'gm'], writes=['EM'])
                        kb.op(V, lambda: nc.vector.reduce_max(c4[:, 2, :nts], E4, AX.X), reads=['EM'], writes=['c4c'])
                        kb.op(V, lambda: nc.vector.tensor_tensor(E24, E4, bc(c4[:, 2, :nts], 32), ALU.is_ge), reads=['EM', 'c4c'], writes=['EM2'])
                        kb.op(V, lambda: nc.vector.scalar_tensor_tensor(E24, E24, -BIG, E4, ALU.mult, ALU.add), reads=['EM', 'EM2'], writes=['EM2'])
                        kb.op(V, lambda: nc.vector.reduce_max(c4[:, 3, :nts], E24, AX.X), reads=['EM2'], writes=['c4d'])
                        kb.op(V, lambda: nc.vector.tensor_tensor(P4, E4, bc(c4[:, 2, :nts], 32), ALU.subtract), reads=['EM', 'c4c'], writes=['pe_'])
                        kb.op(A, lambda: nc.scalar.activation(P4, P4, AF.Exp), reads=['pe_'], writes=['pe_'])
                        kb.op(V, lambda: nc.vector.tensor_tensor(M4, E4, bc(c4[:, 3, :nts], 32), ALU.is_ge), reads=['EM', 'c4d'], writes=[('M', blk)])
                        kb.op(V, lambda: nc.vector.tensor_tensor(P4, P4, M4, ALU.mult), reads=['pe_', ('M', blk)], writes=['pe_'])
                        kb.op(V, lambda: nc.vector.reduce_sum(c4[:, 4, :nts], P4, AX.X), reads=['pe_'], writes=['c4e'])
                        kb.op(V, lambda: nc.vector.tensor_tensor(c4[:, 4, :nts], c4[:, 4, :nts], c4[:, 1, :nts], ALU.mult), reads=['c4e', 'c4b'], writes=['c4e'])
                        kb.op(V, lambda: nc.vector.reciprocal(c4[:, 5, :nts], c4[:, 4, :nts]), reads=['c4e'], writes=['c4f'])
                        kb.op(V, lambda: nc.vector.tensor_tensor(gw4, P4, bc(c4[:, 5, :nts], 32), ALU.mult), reads=['pe_', 'c4f'], writes=[('gw', blk)])
                        for ts in range(nts):
                            kb.op(PE, lambda ts=ts: nc.tensor.matmul(psC[:, ts, :], triU[:], Mall[:, tc0 + ts, :], start=True, stop=False),
                                  reads=[('M', blk), 'triU'], writes=['psC'])
                            for t2 in range(ts):
                                kb.op(PE, lambda ts=ts, t2=t2: nc.tensor.matmul(psC[:, ts, :], onesf[:], Mall[:, tc0 + t2, :], start=False, stop=False),
                                      reads=[('M', blk)], writes=['psC'])
                            kb.op(PE, lambda ts=ts: nc.tensor.matmul(psC[:, ts, :], onesf[:], Macc[:], start=False, stop=True),
                                  reads=['Macc', 'onesf'], writes=['psC'])
                        kb.op(A, lambda: nc.scalar.copy(Call[:, tc0:tc0 + nts, :], psC[:, :nts, :]), reads=['psC'], writes=[('C', blk)])
                        kb.op(V, lambda: nc.vector.reduce_sum(msum[:], M4.rearrange("p t e -> p e t"), AX.X), reads=[('M', blk)], writes=['msum'])
                        kb.op(V, lambda: nc.vector.tensor_tensor(Macc[:], Macc[:], msum[:], ALU.add), reads=['Macc', 'msum'], writes=['Macc'])

                    modulate(l, 1, b, nblk, ps2, router={'h2f': h2f, 'fn': router_fn})
                    cnt = sb("cnt", [32, 4], F32, ps2)
                    cmp18 = sb("cmp18", [32, 18], F32, ps2)
                    padb = sb("padb", [32, 128], F32, ps2)
                    end_rep = sb("end_rep", [128, 32], F32, ps2)
                    cmpb = sb("cmpb", [128, 68, 32], F32, ps2)
                    bef = sb("bef", [128, 68], F32, ps2)
                    psK = pst("psK", [32, 1], F32, ps2)
                    psB = pst("psB", [128, 32], F32, ps2)
                    psE = pst("psE", [128, 32], F32, ps2)
                    kb.op(PE, lambda: nc.tensor.matmul(psK[:], Macc[:], onesf[:, 0:1], start=True, stop=True), reads=['Macc'], writes=['psK'])
                    kb.op(V, lambda: nc.vector.tensor_copy(cnt[:, 0:1], psK[:]), reads=['psK'], writes=['cnt0'])
                    kb.op(V, lambda: nc.vector.tensor_scalar(cmp18[:], thr18[:], cnt[:, 0:1], None, ALU.is_lt), reads=['cnt0', 'thr18'], writes=['cmp18'])
                    kb.op(V, lambda: nc.vector.reduce_sum(cnt[:, 1:2], cmp18[:], AX.X), reads=['cmp18'], writes=['cnt1'])
                    kb.op(V, lambda: nc.vector.tensor_scalar(cnt[:, 2:3], cnt[:, 1:2], 256.0, None, ALU.mult), reads=['cnt1'], writes=['cnt2'])
                    kb.op(V, lambda: nc.vector.tensor_copy(padb[:], cnt[:, 2:3].to_broadcast([32, 128])), reads=['cnt2'], writes=['padb'])
                    kb.op(PE, lambda: nc.tensor.matmul(psB[:], padb[:], triU[0:32, 0:32], start=True, stop=True), reads=['padb', 'triU'], writes=['psB'])
                    kb.op(PE, lambda: nc.tensor.matmul(psE[:], padb[:], UI[:], start=True, stop=True), reads=['padb', 'UI'], writes=['psE'])
                    kb.op(V, lambda: nc.vector.tensor_copy(base_rep[:], psB[:]), reads=['psB'], writes=['base_rep'])
                    kb.op(V, lambda: nc.vector.tensor_copy(end_rep[:], psE[:]), reads=['psE'], writes=['end_rep'])
                    kb.op(V, lambda: nc.vector.tensor_tensor(cmpb[:], end_rep[:].unsqueeze(1).to_broadcast([128, 68, 32]),
                                                             thr68[:].unsqueeze(2).to_broadcast([128, 68, 32]), ALU.is_le),
                          reads=['end_rep', 'thr68'], writes=['cmpb'])
                    kb.op(V, lambda: nc.vector.reduce_sum(bef[:], cmpb[:], AX.X), reads=['cmpb'], writes=['bef'])
                    kb.op(V, lambda: nc.vector.tensor_scalar(bef[:], bef[:], 31.0, 128.0, ALU.min, ALU.mult), reads=['bef'], writes=['bef'])
                    kb.op(V, lambda: nc.vector.tensor_scalar(bef[:], bef[:], iop[:, 0:1], float(l * 4096), ALU.add, ALU.add), reads=['bef', 'iop'], writes=['bef'])
                    garb = sb("garb", [128, 68], F32, ps2)
                    kb.op(V, lambda: nc.vector.tensor_scalar(garb[:], thr68[:], end_rep[:, 31:32], 1.0e6, ALU.is_ge, ALU.mult),
                          reads=['end_rep', 'thr68'], writes=['garb'])
                    kb.op(V, lambda: nc.vector.tensor_tensor(bef[:], bef[:], garb[:], ALU.add), reads=['bef', 'garb'], writes=['bef'])
                    kb.op(V, lambda: nc.vector.tensor_copy(idxw[:], bef[:]), reads=['bef'], writes=['idxw'])
                kb.barrier()
                with ExitStack() as ps2:
                    vv_ = sb("vv_", [128, 32], F32, ps2)
                    v2_ = sb("v2_", [128, 32], F32, ps2)
                    eq_ = sb("eq_", [128, 32], F32, ps2)
                    dcol = sb("dcol", [128, 4], F32, ps2)
                    Xtok = [sb("Xtok%d" % i, [128, D], BF16, ps2) for i in range(2)]
                    psXt = [pst("psXt%d" % i, [128, 8, 128], BF16, ps2) for i in range(2)]
                    for tc in range(nch):
                        blk = tc // 4
                        s_ = tc % 2
                        kb.op(V, lambda tc=tc: nc.vector.tensor_tensor(vv_[:], Call[:, tc, :], base_rep[:], ALU.add), reads=[], writes=['vv_'])
                        kb.op(V, lambda tc=tc: nc.vector.scalar_tensor_tensor(vv_[:], vv_[:], 1.0, Mall[:, tc, :], ALU.add, ALU.mult), reads=['vv_'], writes=['vv_'])
                        kb.op(V, lambda: nc.vector.reduce_max(dcol[:, 0:1], vv_[:], AX.X), reads=['vv_'], writes=['dc0'])
                        kb.op(V, lambda: nc.vector.tensor_scalar(eq_[:], vv_[:], dcol[:, 0:1], None, ALU.is_equal), reads=['vv_', 'dc0'], writes=['eq_'])
                        kb.op(V, lambda tc=tc: nc.vector.tensor_tensor(v2_[:], eq_[:], gwall[:, tc, :], ALU.mult), reads=['eq_'], writes=['v2_'])
                        kb.op(V, lambda tc=tc: nc.vector.reduce_sum(gAB[:, 0, tc:tc + 1], v2_[:], AX.X), reads=['v2_'], writes=[('gA', tc)])
                        kb.op(V, lambda: nc.vector.tensor_scalar(eq_[:], vv_[:], dcol[:, 0:1], None, ALU.not_equal), reads=['vv_', 'dc0', 'v2_'], writes=['eq_'])
                        kb.op(V, lambda: nc.vector.tensor_tensor(v2_[:], vv_[:], eq_[:], ALU.mult), reads=['eq_', 'vv_'], writes=['v2_'])
                        kb.op(V, lambda: nc.vector.reduce_max(dcol[:, 1:2], v2_[:], AX.X), reads=['v2_'], writes=['dc1'])
                        kb.op(V, lambda: nc.vector.tensor_scalar(eq_[:], v2_[:], dcol[:, 1:2], None, ALU.is_equal), reads=['v2_', 'dc1'], writes=['eq_'])
                        kb.op(V, lambda tc=tc: nc.vector.tensor_tensor(v2_[:], eq_[:], gwall[:, tc, :], ALU.mult), reads=['eq_'], writes=['v2_'])
                        kb.op(V, lambda tc=tc: nc.vector.reduce_sum(gAB[:, 1, tc:tc + 1], v2_[:], AX.X), reads=['v2_'], writes=[('gB', tc)])
                        kb.op(V, lambda: nc.vector.tensor_scalar(dcol[:, 2:4], dcol[:, 0:2], -1.0, None, ALU.add), reads=['dc0', 'dc1'], writes=['dc23'])
                        kb.op(V, lambda tc=tc: nc.vector.tensor_copy(dA[:, tc:tc + 1], dcol[:, 2:3]), reads=['dc23'], writes=[('dA', tc)])
                        kb.op(V, lambda tc=tc: nc.vector.tensor_copy(dB[:, tc:tc + 1], dcol[:, 3:4]), reads=['dc23'], writes=[('dB', tc)])
                        for dc in range(8):
                            kb.op(PE, lambda dc=dc, tc=tc, s_=s_: nc.tensor.transpose(psXt[s_][:, dc, :], hT[:, dc, tc * 128:(tc + 1) * 128], identb[:]),
                                  reads=[hk(dc, blk)], writes=['psXt%d' % s_])
                        kb.op(A, lambda s_=s_: nc.scalar.copy(Xtok[s_][:], psXt[s_][:].rearrange("p a b -> p (a b)")), reads=['psXt%d' % s_], writes=['Xtok%d' % s_])
                        for (dd, dk_) in ((dA, 'dA'), (dB, 'dB')):
                            kb.dma('pool', 'scat%d' % s_, Xs_d[:, :], Xtok[s_][:, :], reads=['Xtok%d' % s_, (dk_, tc)], writes=['Xs'],
                                   indirect=('out', dd[:, tc:tc + 1]))
                kb.barrier()
                with ExitStack() as ps2:
                    GU = [WA[:, 0:2, :], WA[:, 2:4, :]]
                    Dn = [sb("Dn%d" % i, [128, 4096], BF16, ps2) for i in range(2)]
                    Xb = [sb("Xb%d" % i, [128, 2, D], BF16, ps2) for i in range(2)]
                    XT = [sb("XT%d" % i, [128, 2, 8, 128], BF16, ps2) for i in range(2)]
                    sgt = [sb("sgt%d" % i, [128, 512], F32, ps2) for i in range(2)]
                    act = [sb("act%d" % i, [128, 2, 512], BF16, ps2) for i in range(2)]
                    actT = [sb("actT%d" % i, [128, 4, 128], BF16, ps2) for i in range(2)]
                    Yb_ = [sb("Yb%d" % i, [128, D], F32, ps2) for i in range(2)]
                    psXT = pst("psXT", [128, 8, 128], BF16, ps2)
                    psG = [pst("psG%d" % i, [128, 512], F32, ps2) for i in range(2)]
                    psU = [pst("psU%d" % i, [128, 512], F32, ps2) for i in range(2)]
                    psAT = pst("psAT", [128, 4, 256], BF16, ps2)
                    psD = [pst("psD%d" % i, [128, 512], F32, ps2) for i in range(2)]

                    def dmaGU(j):
                        s_ = j % 2
                        kb.dma('pool', 'RWg%d' % s_, GU[s_][:, 0, :], w16_d[0][:, :], reads=['idxw'], writes=['RWg%d' % s_], indirect=('in', idxw[:, j:j + 1], bcw))
                        kb.dma('pool', 'RWu%d' % s_, GU[s_][:, 1, :], w16_d[1][:, :], reads=['idxw'], writes=['RWu%d' % s_], indirect=('in', idxw[:, j:j + 1], bcw))

                    def dmaDn(j):
                        s_ = j % 2
                        kb.dma('pool', 'RWd%d' % s_, Dn[s_][:, :], w16_d[2][:, :], reads=['idxw'], writes=['RWd%d' % s_], indirect=('in', idxw[:, j:j + 1], bcw))

                    def xload(j):
                        s_ = j % 2
                        kb.dma('sp', 'xb%d' % s_, Xb[s_][:], Xs_d[j * 256:(j + 1) * 256, :].rearrange("(u p) d -> p u d", p=128),
                               reads=['Xs'], writes=['Xb%d' % s_])

                    def Tx(j, u):
                        s_ = j % 2
                        for dc in range(8):
                            kb.op(PE, lambda dc=dc: nc.tensor.transpose(psXT[:, dc, :], Xb[s_][:, u, dc * 128:(dc + 1) * 128], identb[:]),
                                  reads=['Xb%d' % s_], writes=['psXT'])
                        kb.op(A, lambda: nc.scalar.copy(XT[s_][:, u], psXT[:]), reads=['psXT'], writes=[('XT', s_, u)])

                    def GUm(j, u):
                        s_ = j % 2
                        wg_ = GU[s_][:, 0, :].rearrange("p (k c) -> p k c", k=8)
                        wu_ = GU[s_][:, 1, :].rearrange("p (k c) -> p k c", k=8)
                        for kc in range(8):
                            kb.op(PE, lambda kc=kc: nc.tensor.matmul(psG[u][:], XT[s_][:, u, kc, :], wg_[:, kc, :], start=(kc == 0), stop=(kc == 7)),
                                  reads=[('XT', s_, u), 'RWg%d' % s_], writes=['psG%d' % u])
                        for kc in range(8):
                            kb.op(PE, lambda kc=kc: nc.tensor.matmul(psU[u][:], XT[s_][:, u, kc, :], wu_[:, kc, :], start=(kc == 0), stop=(kc == 7)),
                                  reads=[('XT', s_, u), 'RWu%d' % s_], writes=['psU%d' % u])
                        kb.op(A, lambda: nc.scalar.activation(sgt[u][:], psG[u][:], AF.Silu), reads=['psG%d' % u], writes=['sgt%d' % u])
                        kb.op(V, lambda: nc.vector.tensor_tensor(act[s_][:, u, :], psU[u][:], sgt[u][:], ALU.mult),
                              reads=['psU%d' % u, 'sgt%d' % u], writes=[('act', s_, u)])

                    def Tact(j, u):
                        s_ = j % 2
                        for hc in range(4):
                            kb.op(PE, lambda hc=hc: nc.tensor.transpose(psAT[:, hc, 0:128], act[s_][:, u, hc * 128:(hc + 1) * 128], identb[:]),
                                  reads=[('act', s_, u)], writes=['psAT'])
                        kb.op(A, lambda: nc.scalar.copy(actT[u][:], psAT[:, :, 0:128]), reads=['psAT'], writes=['actT%d' % u])

                    def Dm(j, u):
                        s_ = j % 2
                        wd_ = Dn[s_][:, :].rearrange("p (k c) -> p k c", k=4)
                        for c2 in range(2):
                            for hc in range(4):
                                kb.op(PE, lambda hc=hc, c2=c2: nc.tensor.matmul(psD[c2][:], actT[u][:, hc, :], wd_[:, hc, c2 * 512:(c2 + 1) * 512],
                                                                               start=(hc == 0), stop=(hc == 3)),
                                      reads=['actT%d' % u, 'RWd%d' % s_], writes=['psD%d' % c2])
                        kb.op(A, lambda: nc.scalar.copy(Yb_[u][:, 0:512], psD[0][:]), reads=['psD0'], writes=['Yb%da' % u])
                        kb.op(V, lambda: nc.vector.tensor_copy(Yb_[u][:, 512:1024], psD[1][:]), reads=['psD1'], writes=['Yb%db' % u])
                        r0 = (j * 2 + u) * 128
                        kb.dma('sp', 'ys%d' % u, Ys_d[r0:r0 + 128, :], Yb_[u][:], reads=['Yb%da' % u, 'Yb%db' % u], writes=['Ys'])

                    if ('pc%d' % l) in kb.lazy:
                        kb.lazy.discard('pc%d' % l)
                    kb._wait('pool', ('dma', 'pc%d' % l), kb.dcnt['pc%d' % l])
                    dmaGU(0)
                    dmaGU(1)
                    dmaDn(0)
                    dmaDn(1)
                    xload(0)
                    xload(1)
                    Tx(0, 0)
                    Tx(0, 1)
                    GUm(0, 0)
                    GUm(0, 1)
                    dmaGU(2)
                    for j in range(NBK):
                        nx = j + 1 < NBK
                        if j + 2 < NBK:
                            xload(j + 2)
                        if nx:
                            Tx(j + 1, 0)
                        Tact(j, 0)
                        if nx:
                            Tx(j + 1, 1)
                        Dm(j, 0)
                        if nx:
                            GUm(j + 1, 0)
                        Tact(j, 1)
                        if nx:
                            GUm(j + 1, 1)
                            if j + 3 < NBK:
                                dmaGU(j + 3)
                        Dm(j, 1)
                        if j + 2 < NBK:
                            dmaDn(j + 2)
                kb.barrier()
                with ExitStack() as ps2:
                    Ya = [sb("Ya%d" % i, [128, D], F32, ps2) for i in range(2)]
                    Yc = [sb("Yc%d" % i, [128, D], F32, ps2) for i in range(2)]
                    ff = sb("ff", [128, D], F32, ps2)
                    psF = [pst("psF%d" % i, [128, 4, 128], F32, ps2) for i in range(2)]
                    for tc in range(nch):
                        blk = tc // 4
                        n = b if blk < 4 else NB
                        s_ = tc % 2
                        kb.dma('pool', 'ga%d' % s_, Ya[s_][:, :], Ys_d[:, :], reads=['Ys'], writes=['Ya%d' % s_], indirect=('in', dA[:, tc:tc + 1]))
                        kb.dma('pool', 'gb%d' % s_, Yc[s_][:, :], Ys_d[:, :], reads=['Ys'], writes=['Yc%d' % s_], indirect=('in', dB[:, tc:tc + 1]))
                        kb.op(A, lambda s_=s_, tc=tc: nc.scalar.activation(ff[:], Ya[s_][:], AF.Identity, scale=gAB[:, 0, tc:tc + 1]),
                              reads=['Ya%d' % s_], writes=['ff'])
                        kb.op(V, lambda s_=s_, tc=tc: nc.vector.scalar_tensor_tensor(ff[:], Yc[s_][:], gAB[:, 1, tc:tc + 1], ff[:], ALU.mult, ALU.add),
                              reads=['Yc%d' % s_, 'ff'], writes=['ff'])
                        for hf in range(2):
                            for d4 in range(4):
                                dc = hf * 4 + d4
                                kb.op(PE, lambda dc=dc, d4=d4, hf=hf: nc.tensor.transpose(psF[hf][:, d4, :], ff[:, dc * 128:(dc + 1) * 128], identf[:]),
                                      reads=['ff'], writes=['psF%d' % hf])
                            for d4 in range(4):
                                dc = hf * 4 + d4
                                g2 = modc(l, 1, dc, n)[2]
                                kb.op(V, lambda dc=dc, d4=d4, hf=hf, g2=g2, tc=tc: nc.vector.scalar_tensor_tensor(
                                    xT[:, dc, tc * 128:(tc + 1) * 128], psF[hf][:, d4, :], g2, xT[:, dc, tc * 128:(tc + 1) * 128], ALU.mult, ALU.add),
                                    reads=['psF%d' % hf], writes=[('xTc', dc, tc)])
            kb.barrier()

        def attention(b):
            with ExitStack() as ps:
                modulate(1, 0, b, 5, ps)
            kb.barrier()
            with ExitStack() as ps:
                kT = sb("akT", [128, 2, T], BF16, ps)
                vv = sb("avv", [128, 18, 256], BF16, ps)
                oT = sb("aoT", [128, 4, S], BF16, ps)
                qTs = [sb("aqT%d" % i, [128, 512], BF16, ps) for i in range(2)]
                PT = [sb("aPT%d" % i, [128, 512], BF16, ps) for i in range(2)]
                sq = sb("asq", [128, 512], BF16, ps)
                rt = sb("art", [128, 512], F32, ps)
                R = sb("aR", [128, 512], F32, ps)
                rA = sb("arA", [128, 512], F32, ps)
                rB = sb("arB", [128, 512], F32, ps)
                rD = sb("arD", [128, 512], F32, ps)
                psS = [pst("apsS%d" % i, [128, 512], F32, ps) for i in range(2)]
                psO = pst("apsO", [128, 512], F32, ps)
                psDn = pst("apsDn", [128, 512], F32, ps)
                ps1 = pst("aps1", [128, 512], F32, ps)
                ps2_ = pst("aps2", [128, 512], F32, ps)
                ps3 = pst("aps3", [128, 512], F32, ps)
                psX = pst("apsX", [128, 512], F32, ps)
                wo_ = WA[:, 2:4, :].rearrange("p a (k c) -> p (a k) c", k=4)
                wload(2, wo_, awo_d.rearrange("(hq p) d -> p hq d", p=128), keys=['W2', 'W3'])
                wflat1 = WA[:, 1, :]
                wk_ = wflat1.rearrange("p (g s k c) -> p g s k c", g=2, s=2, k=8)
                for g in range(2):
                    c0 = 1024 + g * 128
                    wload(1, wk_[:, g, 0], aw_d[:, c0:c0 + 128].rearrange("(kc p) c -> p kc c", p=128))
                    wload(1, wk_[:, g, 1], aw_d[:, ATT_IN + c0:ATT_IN + c0 + 128].rearrange("(kc p) c -> p kc c", p=128))
                wv_ = WA[:, 0, 0:2048].rearrange("p (k c) -> p k c", k=8)
                wload(0, wv_, aw_d[:, 1280:1536].rearrange("(kc p) c -> p kc c", p=128))

                def qk_norm_rope(wsl, w4, tab, gcol, dst, dkeys, blk, hkblk):
                    t0, W = blk_rng(blk)
                    for kc in range(8):
                        kb.op(PE, lambda kc=kc: nc.tensor.matmul(ps1[:, :W], w4[:, 0, kc, :], hT[:, kc, t0:t0 + W], start=(kc == 0), stop=(kc == 7)),
                              reads=[wsl] + hks(hkblk), writes=['aps1'])
                    kb.op(A, lambda: nc.scalar.activation(sq[:, :W], ps1[:, :W], AF.Square), reads=['aps1'], writes=['asq'])
                    if blk < 4:
                        for kc in range(8):
                            kb.op(PE, lambda kc=kc: nc.tensor.matmul(ps2_[:, :W], w4[:, 1, kc, :], hT[:, kc, t0:t0 + W], start=(kc == 0), stop=(kc == 7)),
                                  reads=[wsl] + hks(hkblk), writes=['aps2'])
                    kb.op(PE, lambda: nc.tensor.matmul(ps3[:, :W], onesb[:], sq[:, :W], start=True, stop=True), reads=['asq'], writes=['aps3'])
                    kb.op(A, lambda: nc.scalar.activation(rt[:, :W], ps3[:, :W], AF.Sqrt, bias=epsn[:, 0:1], scale=1.0 / 128), reads=['aps3'], writes=['art'])
                    kb.op(V, lambda: nc.vector.reciprocal(R[:, :W], rt[:, :W]), reads=['art'], writes=['aR'])
                    if blk < 4:
                        nr = W // 64
                        r0 = t0 // 64
                        for (src, dstt, ti, pk, dk_) in ((ps1, rA, 0, 'aps1', 'arA'), (ps2_, rB, 1, 'aps2', 'arB')):
                            kb.op(V, lambda src=src, dstt=dstt, ti=ti: nc.vector.tensor_tensor(
                                dstt[0:64, :W].rearrange("p (a b) -> p a b", b=64), src[0:64, :W].rearrange("p (a b) -> p a b", b=64),
                                tab[0:64, ti, r0:r0 + nr].unsqueeze(2).to_broadcast([64, nr, 64]), ALU.mult),
                                reads=[pk], writes=[dk_ + 'lo'])
                            kb.op(V, lambda src=src, dstt=dstt, ti=ti: nc.vector.tensor_tensor(
                                dstt[64:128, :W].rearrange("p (a b) -> p a b", b=64), src[64:128, :W].rearrange("p (a b) -> p a b", b=64),
                                tab[64:128, ti, :].unsqueeze(1).to_broadcast([64, nr, 64]), ALU.mult),
                                reads=[pk], writes=[dk_ + 'hi'])
                        kb.op(P, lambda: nc.gpsimd.tensor_tensor(rA[:, :W], rA[:, :W], rB[:, :W], ALU.add),
                              reads=['arAlo', 'arAhi', 'arBlo', 'arBhi'], writes=['arAlo', 'arAhi'])
                        kb.op(P, lambda: nc.gpsimd.tensor_tensor(dst, rA[:, :W], R[:, :W], ALU.mult),
                              reads=['arAlo', 'arAhi', 'aR'], writes=dkeys)
                    else:
                        kb.op(V, lambda: nc.vector.scalar_tensor_tensor(dst, ps1[:, :W], gcol, R[:, :W], ALU.mult, ALU.mult),
                              reads=['aps1', 'aR'], writes=dkeys)

                def load_wq(hq):
                    qs = (hq + 1) % 2
                    wq_ = WA[:, 0, qs * 2048:(qs + 1) * 2048].rearrange("p (s k c) -> p s k c", s=2, k=8)
                    qkey = 'WQ%d' % qs
                    deps_extra = ['W0'] if qs == 0 else []
                    kb.dma('pool', qkey, wq_[:, 0], aw_d[:, hq * 128:(hq + 1) * 128].rearrange("(kc p) c -> p kc c", p=128),
                           writes=[qkey] + deps_extra)
                    kb.dma('pool', qkey, wq_[:, 1], aw_d[:, ATT_IN + hq * 128:ATT_IN + (hq + 1) * 128].rearrange("(kc p) c -> p kc c", p=128),
                           writes=[qkey] + deps_extra)

                load_wq(0)
                for g in range(2):
                    for blk in range(5):
                        t0, W = blk_rng(blk)
                        qk_norm_rope('W1', wk_[:, g], atk, gk[:, 0:1], kT[:, g, t0:t0 + W], [('akT', g, blk)], blk, blk)
                for tc in range(18):
                    pv, pvk = (psX, 'apsX') if tc % 2 == 0 else (ps3, 'aps3')
                    for kc in range(8):
                        kb.op(PE, lambda kc=kc, tc=tc, pv=pv: nc.tensor.matmul(pv[:, 0:256], hT[:, kc, tc * 128:(tc + 1) * 128], wv_[:, kc, :],
                                                                              start=(kc == 0), stop=(kc == 7)),
                              reads=['W0'] + hks(tc // 4), writes=[pvk])
                    kb.op(A, lambda tc=tc, pv=pv: nc.scalar.copy(vv[:, tc, :], pv[:, 0:256]), reads=[pvk], writes=[('avv', tc)])
                scale = 128.0 ** -0.5
                for half in range(2):
                    items = [(h4, qb) for h4 in range(4) for qb in range(4)]

                    def qproj(i):
                        h4, qb = items[i]
                        hq = half * 4 + h4
                        qs = (hq + 1) % 2
                        wq_ = WA[:, 0, qs * 2048:(qs + 1) * 2048].rearrange("p (s k c) -> p s k c", s=2, k=8)
                        qkey = 'WQ%d' % qs
                        if qb == 0 and hq + 1 < 8:
                            load_wq(hq + 1)
                        qk_norm_rope(qkey, wq_, atq, gq[:, 0:1], qTs[i % 2][:, :], ['aqT%d' % (i % 2)], qb, qb)

                    qproj(0)
                    for i in range(16):
                        h4, qb = items[i]
                        hq = half * 4 + h4
                        g = hq // 4
                        t0, W = blk_rng(qb)
                        qT = qTs[i % 2]
                        qk_ = 'aqT%d' % (i % 2)
                        if i + 1 < 16:
                            qproj(i + 1)
                        def Sm(sc):
                            pS = psS[sc % 2]
                            kb.op(PE, lambda: nc.tensor.matmul(pS[:], kT[:, g, sc * 128:(sc + 1) * 128], qT[:], start=True, stop=True),
                                  reads=[('akT', g, sc // 4), qk_], writes=['apsS%d' % (sc % 2)])
                            pt = PT[sc % 2]
                            kb.op(A, lambda: nc.scalar.activation(pt[:], pS[:], AF.Exp, scale=scale),
                                  reads=['apsS%d' % (sc % 2)], writes=['aPT%d' % (sc % 2)])

                        def PVm(sc):
                            pt = PT[sc % 2]
                            kb.op(PE, lambda: nc.tensor.matmul(psO[:], vv[:, sc, g * 128:(g + 1) * 128], pt[:], start=(sc == 0), stop=(sc == 17)),
                                  reads=['aPT%d' % (sc % 2), ('avv', sc)], writes=['apsO'])
                            kb.op(PE, lambda: nc.tensor.matmul(psDn[:], onesb[:], pt[:], start=(sc == 0), stop=(sc == 17)),
                                  reads=['aPT%d' % (sc % 2)], writes=['apsDn'])

                        Sm(0)
                        for sc in range(18):
                            if sc + 1 < 18:
                                Sm(sc + 1)
                            PVm(sc)
                        kb.op(V, lambda: nc.vector.reciprocal(rD[:], psDn[:]), reads=['apsDn'], writes=['arD'])
                        kb.op(V, lambda h4=h4, t0=t0: nc.vector.tensor_tensor(oT[:, h4, t0:t0 + 512], psO[:], rD[:], ALU.mult),
                              reads=['apsO', 'arD'], writes=[('aoT', h4, qb)])
                    for qb in range(4):
                        t0, W = blk_rng(qb)
                        for dc in range(8):
                            po, pok = (psX, 'apsX') if dc % 2 == 0 else (ps1, 'aps1')
                            for h4 in range(4):
                                kb.op(PE, lambda h4=h4, dc=dc, po=po: nc.tensor.matmul(po[:], wo_[:, half * 4 + h4, dc * 128:(dc + 1) * 128], oT[:, h4, t0:t0 + 512],
                                                                                      start=(h4 == 0), stop=(h4 == 3)),
                                      reads=['W2', 'W3'] + [('aoT', i, qb) for i in range(4)], writes=[pok])
                            g1 = modc(1, 0, dc, b)[2]
                            kb.op(V, lambda dc=dc, g1=g1, po=po: nc.vector.scalar_tensor_tensor(
                                xT[:, dc, t0:t0 + 512], po[:], g1, xT[:, dc, t0:t0 + 512], ALU.mult, ALU.add),
                                reads=[pok, xk(dc, qb)], writes=[xk(dc, qb)])
            kb.barrier()

        def store(b, final_norm, which_ctx=False):
            with ExitStack() as ps:
                sqb = sb("sqb", [128, 8, 512], BF16, ps)
                rt = sb("rt", [128, 512], F32, ps)
                R = sb("R", [128, 512], F32, ps)
                tmp = sb("ftmp", [128, 512], F32, ps)
                of = sb("of", [128, 8, 512], F32, ps)
                ost = [sb("ost%d" % i, [128, D], F32, ps) for i in range(4)]
                pss = pst("pss", [128, 512], F32, ps)
                ptr = [pst("ptr%d" % i, [128, 4, 128], F32, ps) for i in range(4)]
                blks = [4] if which_ctx else [0, 1, 2, 3]
                oi = 0
                for blk in blks:
                    t0, W = blk_rng(blk)
                    if final_norm:
                        rms_block(blk, sqb, pss, rt, R, 'pss')
                        for dc in range(8):
                            kb.op(V, lambda dc=dc: nc.vector.tensor_tensor(tmp[:, :W], xT[:, dc, t0:t0 + W], R[:, :W], ALU.mult),
                                  reads=[xk(dc, blk), 'R'], writes=['ftmp'])
                            kb.op(A, lambda dc=dc: nc.scalar.activation(of[:, dc, :W], tmp[:, :W], AF.Identity, scale=fg[:, dc:dc + 1]),
                                  reads=['ftmp'], writes=[('of', dc)])
                    for ts in range(W // 128):
                        o_ = ost[oi % 4]
                        okey = 'ost%d' % (oi % 4)
                        for hf in range(2):
                            for d4 in range(4):
                                dc = hf * 4 + d4
                                src = of[:, dc, ts * 128:(ts + 1) * 128] if final_norm else xT[:, dc, t0 + ts * 128:t0 + (ts + 1) * 128]
                                rk = [('of', dc)] if final_norm else [xk(dc, blk)]
                                pi = (oi % 2) * 2 + hf
                                kb.op(PE, lambda src=src, pi=pi, d4=d4: nc.tensor.transpose(ptr[pi][:, d4, :], src, identf[:]),
                                      reads=rk, writes=['ptr%d' % pi])
                            pi = (oi % 2) * 2 + hf
                            if hf == 0:
                                kb.op(A, lambda o_=o_, pi=pi: nc.scalar.copy(o_[:, 0:512], ptr[pi][:].rearrange("p a b -> p (a b)")),
                                      reads=['ptr%d' % pi], writes=[okey + 'a'])
                            else:
                                kb.op(V, lambda o_=o_, pi=pi: nc.vector.tensor_copy(o_[:, 512:1024], ptr[pi][:].rearrange("p a b -> p (a b)")),
                                      reads=['ptr%d' % pi], writes=[okey + 'b'])
                        tok0 = t0 + ts * 128
                        if which_ctx:
                            dst = outc_d[b, tok0 - 2048:tok0 - 2048 + 128, :]
                        else:
                            dst = out_d[b, tok0:tok0 + 128, :]
                        kb.dma('sp', 'o%d' % (oi % 4), dst, o_[:], reads=[okey + 'a', okey + 'b'])
                        oi += 1
            kb.barrier()

        for b in range(NB):
            load_x(b)
            stages = [('ret', lambda: retention(b)), ('moe0', lambda: moe(0, b)),
                      ('att', lambda: attention(b)), ('moe1', lambda: moe(1, b))]
            done_all = True
            for name, fn in stages:
                fn()
                if stop == name:
                    done_all = False
                    break
            store(b, final_norm=done_all)
            if dump_ctx:
                store(b, final_norm=False, which_ctx=True)
        kb.finish()
    return nc


def _rope_tables():
    theta = 10000.0
    p = np.arange(128)
    inv = theta ** (-(np.arange(64, dtype=np.float32)) / 64.0)
    invp = inv[p % 64].astype(np.float32)
    sign = np.where(p < 64, -1.0, 1.0).astype(np.float32)
    rows = np.arange(32, dtype=np.float32)
    cols = np.arange(64, dtype=np.float32)
    ar = (rows[None, :] * invp[:, None]).astype(np.float32)
    ac = (cols[None, :] * invp[:, None]).astype(np.float32)
    ret_tab = np.concatenate([np.cos(ar), sign[:, None] * np.sin(ar), np.cos(ac), sign[:, None] * np.sin(ac)], axis=1).astype(np.float32)
    inv2 = theta ** (-(np.arange(32, dtype=np.float32)) / 32.0)
    invp2 = inv2[p % 32].astype(np.float32)
    sign2 = np.where((p % 64) < 32, -1.0, 1.0).astype(np.float32)
    att_tab = np.zeros((128, 2, 64), np.float32)
    a_r = (rows[None, :] * invp2[:, None]).astype(np.float32)
    a_c = (cols[None, :] * invp2[:, None]).astype(np.float32)
    att_tab[:64, 0, :32] = np.cos(a_r)[:64]
    att_tab[:64, 1, :32] = (sign2[:, None] * np.sin(a_r))[:64]
    att_tab[64:, 0, :] = np.cos(a_c)[64:]
    att_tab[64:, 1, :] = (sign2[:, None] * np.sin(a_c))[64:]
    return ret_tab, att_tab


def _shared_inputs(inp):
    f = lambda a: np.ascontiguousarray(np.asarray(a, dtype=np.float32))
    ret_tab, att_tab = _rope_tables()
    sw_ret = np.concatenate([(np.arange(128) + 64) % 128 + c * 128 for c in range(16)])
    rw = f(inp['ret_w_in'])[0]
    rw_ext = np.concatenate([rw, rw[:, :2048][:, sw_ret]], axis=1)
    pa = np.arange(128)
    sw128 = (pa // 64) * 64 + ((pa % 64) + 32) % 64
    sw_att = np.concatenate([sw128 + c * 128 for c in range(10)])
    aw = f(inp['att_w_qkv'])[0]
    aw_ext = np.concatenate([aw, aw[:, :1280][:, sw_att]], axis=1)
    gqv = f(inp['att_q_gain'])[0]
    gkv = f(inp['att_k_gain'])[0]
    sh = {
        'w_mod': f(inp['w_mod']),
        'bmodT': f(f(inp['b_mod']).reshape(2, 48, 128).transpose(2, 0, 1)),
        'ret_w_in': f(rw_ext),
        'ret_w_out': f(inp['ret_w_out'])[0],
        'lgf': f(inp['ret_log_decay_fwd']).reshape(1, 4),
        'lgb': f(inp['ret_log_decay_bwd']).reshape(1, 4),
        'att_w_qkv': f(aw_ext),
        'att_w_o': f(inp['att_w_o'])[0],
        'gq': f(np.stack([gqv, gqv[sw128]], axis=1)),
        'gk': f(np.stack([gkv, gkv[sw128]], axis=1)),
        'w_router': f(np.concatenate([f(inp['moe_w_group']), f(inp['moe_w_expert'])], axis=2)),
        'b_router': f(np.concatenate([f(inp['moe_b_group']), f(inp['moe_b_expert'])], axis=1)),
        'wg_r': f(f(inp['moe_w_gate']).reshape(2, 32, 8, 128, 512).transpose(0, 1, 3, 2, 4).reshape(2 * 32 * 128, 4096)),
        'wu_r': f(f(inp['moe_w_up']).reshape(2, 32, 8, 128, 512).transpose(0, 1, 3, 2, 4).reshape(2 * 32 * 128, 4096)),
        'wd_r': f(f(inp['moe_w_down']).reshape(2, 32, 4, 128, 1024).transpose(0, 1, 3, 2, 4).reshape(2 * 32 * 128, 4096)),
        'fgT': f(f(inp['final_norm_gain']).reshape(8, 128).T),
        'ret_tab': ret_tab,
        'att_tab': att_tab,
        'ident': np.eye(128, dtype=np.float32),
    }
    return sh


def _core_inputs(inp, sh, b0, nb):
    f = lambda a: np.ascontiguousarray(np.asarray(a, dtype=np.float32))
    cc = np.concatenate([f(inp['c'])[b0:b0 + nb], f(inp['c_ctx'])[None, :]], axis=0)
    m = dict(sh)
    m['x'] = f(inp['x'][b0:b0 + nb])
    m['ctx'] = f(inp['ctx'][b0:b0 + nb])
    m['cT'] = f(cc.reshape(nb + 1, 8, 128).transpose(2, 1, 0))
    return m


_PROG = {}


def kernel(**inputs):
    if 'full' not in _PROG:
        _PROG['full'] = build_program(NB_FULL)
    nc = _PROG['full']
    sh = _shared_inputs(inputs)
    in_maps = [_core_inputs(inputs, sh, c * NB_FULL, NB_FULL) for c in range(NCORES)]
    res = run_bass_kernel_spmd(nc, in_maps, core_ids=list(range(NCORES)))
    out = np.concatenate([np.asarray(r["out"], dtype=np.float32) for r in res.results], axis=0)
    return out
```
